# Optimizing a Trainium2 kernel written in Bass

```python
import math
import jax, jax.numpy as jnp
from jax import lax
import numpy as np

D_MODEL = 1024
BATCH = 1
SEQ = 16384
DEPTH = 4

GRID_W = 64
CTX_LEN = 256
MIXERS = ('chunk_mlp', 'mlstm', 'rglru', 'fourier')
N_MIXERS = 4
PREFIX_READERS = ('mlstm', 'rglru')
N_MOD = 6
EPS = 1e-6
POS_BASE = 10000.0
CONV_WIDTH = 4
CM_CHUNK = 128
CM_WIDTH = 1024
CM_GROUPS = 4
ML_HEADS = 4
ML_DK = 128
ML_DV = 256
ML_QK = ML_HEADS * ML_DK
ML_V = ML_HEADS * ML_DV
ML_CHUNK = 128
ML_IN = 2 * ML_QK + 2 * ML_V + 4 * ML_HEADS
LRU_WIDTH = 1280
LRU_HEADS = 10
LRU_BLOCK = LRU_WIDTH // LRU_HEADS
LRU_C = 8.0
FN_WIDTH = 1024
FN_GROUPS = 4
MOE_GROUPS = 4
MOE_EXPERTS_PER_GROUP = 8
MOE_EXPERTS = MOE_GROUPS * MOE_EXPERTS_PER_GROUP
MOE_TOPK = 2
MOE_HIDDEN = 512
MOE_BLOCK = 128

kernel_name = 'hybrid_interleaved_diffusion_block'


def _rms_norm(x, g):
    xf = x.astype(jnp.float32)
    y = xf * lax.rsqrt(jnp.mean(xf * xf, axis=-1, keepdims=True) + EPS)
    return (y * g.astype(jnp.float32)).astype(x.dtype)


def _modulate(h, shift, scale):
    return h * (1 + scale) + shift


def _dwconv_centred(x, w, b):
    k_w = w.shape[0]
    left = k_w // 2
    right = k_w - 1 - left
    t = x.shape[1]
    xp = jnp.pad(x, ((0, 0), (left, right), (0, 0)))
    y = xp[:, 0:t] * w[0]
    for j in range(1, k_w):
        y = y + xp[:, j:j + t] * w[j]
    return y + b


def _grid_pos_embedding(n_tok, d):
    rows = n_tok // GRID_W
    r = jnp.repeat(jnp.arange(rows, dtype=jnp.float32), GRID_W)
    col = jnp.tile(jnp.arange(GRID_W, dtype=jnp.float32), rows)
    q = d // 4
    freq = jnp.exp(-math.log(POS_BASE) * jnp.arange(q, dtype=jnp.float32) / q)
    ar = r[:, None] * freq
    ac = col[:, None] * freq
    return jnp.concatenate([jnp.sin(ar), jnp.cos(ar), jnp.sin(ac), jnp.cos(ac)], axis=-1)


def _chunk_mlp(h, w_in, v_norm_g, w_s, b_s, w_out):
    bsz, t, _ = h.shape
    z = jax.nn.gelu(h @ w_in)
    u = z[..., :CM_WIDTH]
    v = _rms_norm(z[..., CM_WIDTH:], v_norm_g)
    vb = v.reshape(bsz, t // CM_CHUNK, CM_CHUNK, CM_GROUPS, CM_WIDTH // CM_GROUPS)
    s = jnp.einsum('gts,bnsgc->bntgc', w_s, vb) + b_s.T[None, None, :, :, None]
    return (u * s.reshape(bsz, t, CM_WIDTH)) @ w_out


def _fourier_mix(h, w_in, w_out):
    bsz, t, _ = h.shape
    z = (h @ w_in).astype(jnp.float32).reshape(bsz, t, FN_GROUPS, FN_WIDTH // FN_GROUPS)
    f = jnp.fft.fftn(z, axes=(1, 3), norm='ortho').real
    return f.reshape(bsz, t, FN_WIDTH).astype(h.dtype) @ w_out


def _heads_to_chunks(a):
    bsz, t, nh, d = a.shape
    return a.reshape(bsz, t // ML_CHUNK, ML_CHUNK, nh, d).transpose(0, 3, 1, 2, 4)


def _gates_to_chunks(a):
    bsz, t, nh = a.shape
    return a.reshape(bsz, t // ML_CHUNK, ML_CHUNK, nh).transpose(0, 3, 1, 2)


def _chunks_to_heads(a):
    bsz, nh, nc, cl, d = a.shape
    return a.transpose(0, 2, 3, 1, 4).reshape(bsz, nc * cl, nh, d)


def _mlstm_chunk_states(k, v, li, lf, state0):
    b = jnp.cumsum(lf, axis=-1)
    g = b[..., -1]
    a = g[..., None] - b + li
    m_loc = jnp.max(a, axis=-1)
    kw = k * jnp.exp(a - m_loc[..., None])[..., None]
    c_loc = jnp.einsum('bhnlk,bhnlv->bhnkv', kw, v)
    n_loc = jnp.sum(kw, axis=-2)

    def step(carry, inp):
        c_prev, n_prev, m_prev = carry
        cl, nl, ml, gl = inp
        m_new = jnp.maximum(gl + m_prev, ml)
        dec = jnp.exp(gl + m_prev - m_new)
        sc = jnp.exp(ml - m_new)
        c_new = dec[..., None, None] * c_prev + sc[..., None, None] * cl
        n_new = dec[..., None] * n_prev + sc[..., None] * nl
        return (c_new, n_new, m_new), (c_prev, n_prev, m_prev)

    xs = (jnp.moveaxis(c_loc, 2, 0), jnp.moveaxis(n_loc, 2, 0),
          jnp.moveaxis(m_loc, 2, 0), jnp.moveaxis(g, 2, 0))
    final, starts = lax.scan(step, state0, xs)
    starts = tuple(jnp.moveaxis(s, 0, 2) for s in starts)
    return starts, final


def _mlstm_chunk_outputs(q, k, v, li, lf, starts):
    c0, n0, m0 = starts
    cl = q.shape[-2]
    b = jnp.cumsum(lf, axis=-1)
    inter = b + m0[..., None]
    past_in_chunk = jnp.tril(jnp.ones((cl, cl), dtype=bool))
    dlog = jnp.where(past_in_chunk, b[..., :, None] - b[..., None, :] + li[..., None, :], -jnp.inf)
    m = jnp.maximum(inter, jnp.max(dlog, axis=-1))
    s = jnp.einsum('bhntk,bhnsk->bhnts', q, k) * jnp.exp(dlog - m[..., None])
    w_inter = jnp.exp(inter - m)
    num = jnp.einsum('bhnts,bhnsv->bhntv', s, v) + w_inter[..., None] * jnp.einsum('bhntk,bhnkv->bhntv', q, c0)
    den = jnp.sum(s, axis=-1) + w_inter * jnp.einsum('bhntk,bhnk->bhnt', q, n0)
    return num / jnp.maximum(jnp.abs(den), jnp.exp(-m))[..., None]


def _mlstm_direction(q, k, v, li, lf, state0, want_out):
    kc, vc = _heads_to_chunks(k), _heads_to_chunks(v)
    lic, lfc = _gates_to_chunks(li), _gates_to_chunks(lf)
    starts, final = _mlstm_chunk_states(kc, vc, lic, lfc, state0)
    if not want_out:
        return None, final
    h = _mlstm_chunk_outputs(_heads_to_chunks(q), kc, vc, lic, lfc, starts)
    return _chunks_to_heads(h), final


def _mlstm_mixer(h_ctx, h_lat, want_ctx_out, w_in, conv_w, conv_b, gate_b, norm_g, w_out):
    f32 = jnp.float32

    def project(h):
        bsz, t, _ = h.shape
        z = h @ w_in
        qk = jax.nn.silu(_dwconv_centred(z[..., :2 * ML_QK], conv_w, conv_b)).astype(f32)
        q = qk[..., :ML_QK].reshape(bsz, t, ML_HEADS, ML_DK) * (ML_DK ** -0.5)
        k = qk[..., ML_QK:].reshape(bsz, t, ML_HEADS, ML_DK)
        v = z[..., 2 * ML_QK:2 * ML_QK + ML_V].astype(f32).reshape(bsz, t, ML_HEADS, ML_DV)
        o = z[..., 2 * ML_QK + ML_V:2 * ML_QK + 2 * ML_V]
        pre = z[..., 2 * ML_QK + 2 * ML_V:].astype(f32).reshape(bsz, t, 2, 2, ML_HEADS) + gate_b.astype(f32)
        li = pre[:, :, :, 0]
        lf = jax.nn.log_sigmoid(pre[:, :, :, 1])
        return q, k, v, o, li, lf

    def readout(h_sum, o):
        bsz, t = o.shape[:2]
        hn = h_sum * lax.rsqrt(jnp.mean(h_sum * h_sum, axis=-1, keepdims=True) + EPS)
        hn = hn.reshape(bsz, t, ML_V) * norm_g.astype(f32)
        return (hn.astype(o.dtype) * jax.nn.sigmoid(o)) @ w_out

    qc, kc, vc, oc, lic, lfc = project(h_ctx)
    ql, kl, vl, ol, lil, lfl = project(h_lat)
    bsz = h_lat.shape[0]
    zero = (jnp.zeros((bsz, ML_HEADS, ML_DK, ML_DV), f32),
            jnp.zeros((bsz, ML_HEADS, ML_DK), f32),
            jnp.zeros((bsz, ML_HEADS), f32))
    ident = lambda a: a
    rev = lambda a: jnp.flip(a, axis=1)
    outs_ctx, outs_lat = [], []
    for d, order in enumerate((ident, rev)):
        hc, state = _mlstm_direction(order(qc), order(kc), order(vc), order(lic[:, :, d]),
                                     order(lfc[:, :, d]), zero, want_ctx_out)
        hl, _ = _mlstm_direction(order(ql), order(kl), order(vl), order(lil[:, :, d]),
                                 order(lfl[:, :, d]), state, True)
        outs_lat.append(order(hl))
        if want_ctx_out:
            outs_ctx.append(order(hc))
    y_lat = readout(outs_lat[0] + outs_lat[1], ol)
    y_ctx = readout(outs_ctx[0] + outs_ctx[1], oc) if want_ctx_out else None
    return y_ctx, y_lat


def _linrec_combine(left, right):
    a_l, b_l = left
    a_r, b_r = right
    return a_l * a_r, a_r * b_l + b_r


def _rglru_mixer(h_ctx, h_lat, want_ctx_out, w_in, conv_w, conv_b, w_a, b_a, w_x, b_x, lam, w_out):
    f32 = jnp.float32

    def branches(h):
        z = h @ w_in
        return (jax.nn.gelu(z[..., :LRU_WIDTH]),
                _dwconv_centred(z[..., LRU_WIDTH:], conv_w, conv_b).astype(f32))

    def block_diag_gate(xr, w, b):
        bsz, t, _ = xr.shape
        y = jnp.einsum('btnc,ncd->btnd', xr.reshape(bsz, t, LRU_HEADS, LRU_BLOCK), w.astype(f32))
        return jax.nn.sigmoid(y.reshape(bsz, t, LRU_WIDTH) + b.astype(f32))

    def recur(xr, d, h0, reverse):
        r = block_diag_gate(xr, w_a[d], b_a[d])
        i = block_diag_gate(xr, w_x[d], b_x[d])
        log_a = -LRU_C * r * jax.nn.softplus(-lam[d].astype(f32))
        a = jnp.exp(log_a)
        u = jnp.sqrt(-jnp.expm1(2.0 * log_a)) * (i * xr)
        a_cum, u_cum = lax.associative_scan(_linrec_combine, (a, u), reverse=reverse, axis=1)
        return a_cum * h0[:, None, :] + u_cum

    g_c, x_c = branches(h_ctx)
    g_l, x_l = branches(h_lat)
    h0 = jnp.zeros((h_lat.shape[0], LRU_WIDTH), f32)
    hl_sum = 0.0
    hc_sum = 0.0
    for d, reverse in ((0, False), (1, True)):
        hc = recur(x_c, d, h0, reverse)
        state = hc[:, 0] if reverse else hc[:, -1]
        hl_sum = hl_sum + recur(x_l, d, state, reverse)
        if want_ctx_out:
            hc_sum = hc_sum + hc
    y_lat = (g_l * hl_sum.astype(g_l.dtype)) @ w_out
    y_ctx = (g_c * hc_sum.astype(g_c.dtype)) @ w_out if want_ctx_out else None
    return y_ctx, y_lat


def _hier_moe(h, rg_w, rg_b, re_w, re_b, w_gate, w_up, w_down):
    n_tok, d = h.shape
    g_logits = (h @ rg_w + rg_b).astype(jnp.float32)
    grp = jnp.argmax(g_logits, axis=-1)
    p_grp = jnp.take_along_axis(jax.nn.softmax(g_logits, axis=-1), grp[:, None], axis=-1)
    e_logits = (h @ re_w + re_b).astype(jnp.float32).reshape(n_tok, MOE_GROUPS, MOE_EXPERTS_PER_GROUP)
    e_in_grp = jnp.take_along_axis(e_logits, grp[:, None, None], axis=1)[:, 0]
    top_val, top_idx = lax.top_k(e_in_grp, MOE_TOPK)
    weight = (jax.nn.softmax(top_val, axis=-1) * p_grp).astype(h.dtype)
    expert = grp[:, None] * MOE_EXPERTS_PER_GROUP + top_idx
    n_asg = n_tok * MOE_TOPK
    flat_e = expert.reshape(n_asg)
    order = jnp.argsort(flat_e)
    se = flat_e[order]
    stok = order // MOE_TOPK
    sw = weight.reshape(n_asg)[order]
    counts = jnp.bincount(flat_e, length=MOE_EXPERTS)
    seg_start = jnp.cumsum(counts) - counts
    padded = (counts + MOE_BLOCK - 1) // MOE_BLOCK * MOE_BLOCK
    pad_end = jnp.cumsum(padded)
    pad_start = pad_end - padded
    dest = pad_start[se] + jnp.arange(n_asg) - seg_start[se]
    n_blocks = -(-(n_asg + MOE_EXPERTS * (MOE_BLOCK - 1)) // MOE_BLOCK)
    buf = jnp.zeros((n_blocks * MOE_BLOCK, d), h.dtype).at[dest].set(h[stok])
    blk_expert = jnp.minimum(jnp.searchsorted(pad_end, jnp.arange(n_blocks) * MOE_BLOCK, side='right'),
                             MOE_EXPERTS - 1)

    def expert_block(args):
        xb, e = args
        return (jax.nn.silu(xb @ w_gate[e]) * (xb @ w_up[e])) @ w_down[e]

    ybuf = lax.map(expert_block, (buf.reshape(n_blocks, MOE_BLOCK, d), blk_expert)).reshape(-1, d)
    return jnp.zeros_like(h).at[stok].add(ybuf[dest] * sw[:, None])


def setup_inputs(seed: int = 0) -> dict:
    key = jax.random.key(seed)
    keys = iter(jax.random.split(key, 64))

    def nrm(shape, scale):
        return jax.random.normal(next(keys), shape, jnp.float32) * scale

    d = D_MODEL
    n_a, n_b, n_c, n_d = (len(range(m, DEPTH, N_MIXERS)) for m in range(N_MIXERS))
    ml_gate_b = nrm((n_b, 2, 2, ML_HEADS), 0.1).at[:, :, 1].add(jnp.linspace(3.0, 6.0, ML_HEADS))
    u = jax.random.uniform(next(keys), (n_c, 2, LRU_WIDTH), jnp.float32, 0.9, 0.999)
    p = u ** (1.0 / LRU_C)
    lru_lambda = jnp.log(p) - jnp.log1p(-p)
    return {
        'x': nrm((BATCH, SEQ, d), 1.0),
        'c': nrm((BATCH, d), 1.0),
        'ctx': nrm((BATCH, CTX_LEN, d), 1.0),
        'c_ctx': nrm((d,), 1.0),
        'ada_w': nrm((DEPTH, d, N_MOD * d), 0.5 * d ** -0.5),
        'ada_b': nrm((DEPTH, N_MOD * d), 0.02),
        'norm_mix_g': 1.0 + nrm((DEPTH, d), 0.1),
        'norm_ffn_g': 1.0 + nrm((DEPTH, d), 0.1),
        'final_norm_g': 1.0 + nrm((d,), 0.1),
        'router_group_w': nrm((DEPTH, d, MOE_GROUPS), d ** -0.5),
        'router_group_b': nrm((DEPTH, MOE_GROUPS), 0.01),
        'router_expert_w': nrm((DEPTH, d, MOE_EXPERTS), d ** -0.5),
        'router_expert_b': nrm((DEPTH, MOE_EXPERTS), 0.01),
        'expert_w_gate': nrm((DEPTH, MOE_EXPERTS, d, MOE_HIDDEN), d ** -0.5),
        'expert_w_up': nrm((DEPTH, MOE_EXPERTS, d, MOE_HIDDEN), d ** -0.5),
        'expert_w_down': nrm((DEPTH, MOE_EXPERTS, MOE_HIDDEN, d), MOE_HIDDEN ** -0.5),
        'cm_w_in': nrm((n_a, d, 2 * CM_WIDTH), d ** -0.5),
        'cm_v_norm_g': 1.0 + nrm((n_a, CM_WIDTH), 0.1),
        'cm_w_s': nrm((n_a, CM_GROUPS, CM_CHUNK, CM_CHUNK), CM_CHUNK ** -0.5),
        'cm_b_s': nrm((n_a, CM_GROUPS, CM_CHUNK), 0.02),
        'cm_w_out': nrm((n_a, CM_WIDTH, d), CM_WIDTH ** -0.5),
        'ml_w_in': nrm((n_b, d, ML_IN), d ** -0.5),
        'ml_conv_w': nrm((n_b, CONV_WIDTH, 2 * ML_QK), CONV_WIDTH ** -0.5),
        'ml_conv_b': nrm((n_b, 2 * ML_QK), 0.02),
        'ml_gate_b': ml_gate_b,
        'ml_norm_g': 1.0 + nrm((n_b, ML_V), 0.1),
        'ml_w_out': nrm((n_b, ML_V, d), ML_V ** -0.5),
        'lru_w_in': nrm((n_c, d, 2 * LRU_WIDTH), d ** -0.5),
        'lru_conv_w': nrm((n_c, CONV_WIDTH, LRU_WIDTH), CONV_WIDTH ** -0.5),
        'lru_conv_b': nrm((n_c, LRU_WIDTH), 0.02),
        'lru_w_a': nrm((n_c, 2, LRU_HEADS, LRU_BLOCK, LRU_BLOCK), LRU_BLOCK ** -0.5),
        'lru_b_a': nrm((n_c, 2, LRU_WIDTH), 0.02),
        'lru_w_x': nrm((n_c, 2, LRU_HEADS, LRU_BLOCK, LRU_BLOCK), LRU_BLOCK ** -0.5),
        'lru_b_x': nrm((n_c, 2, LRU_WIDTH), 0.02),
        'lru_lambda': lru_lambda,
        'lru_w_out': nrm((n_c, LRU_WIDTH, d), LRU_WIDTH ** -0.5),
        'fn_w_in': nrm((n_d, d, FN_WIDTH), d ** -0.5),
        'fn_w_out': nrm((n_d, FN_WIDTH, d), FN_WIDTH ** -0.5),
    }


def reference(x, c, ctx, c_ctx, ada_w, ada_b, norm_mix_g, norm_ffn_g, final_norm_g,
              router_group_w, router_group_b, router_expert_w, router_expert_b,
              expert_w_gate, expert_w_up, expert_w_down,
              cm_w_in, cm_v_norm_g, cm_w_s, cm_b_s, cm_w_out,
              ml_w_in, ml_conv_w, ml_conv_b, ml_gate_b, ml_norm_g, ml_w_out,
              lru_w_in, lru_conv_w, lru_conv_b, lru_w_a, lru_b_a, lru_w_x, lru_b_x, lru_lambda, lru_w_out,
              fn_w_in, fn_w_out):
    bsz, seq, d = x.shape
    n_ctx = ctx.shape[1]
    lat = x + _grid_pos_embedding(seq, d).astype(x.dtype)[None]
    cx = ctx
    readers = [i for i in range(DEPTH) if MIXERS[i % N_MIXERS] in PREFIX_READERS]
    last_reader = readers[-1] if readers else -1
    silu_c = jax.nn.silu(c)
    silu_cc = jax.nn.silu(c_ctx)
    for i in range(DEPTH):
        kind = MIXERS[i % N_MIXERS]
        j = i // N_MIXERS
        ctx_in = i <= last_reader
        ctx_upd = i < last_reader
        mod_l = jnp.split((silu_c @ ada_w[i] + ada_b[i])[:, None, :], N_MOD, axis=-1)
        hl = _modulate(_rms_norm(lat, norm_mix_g[i]), mod_l[0], mod_l[1])
        hc = None
        mod_c = None
        if ctx_in:
            mod_c = jnp.split((silu_cc @ ada_w[i] + ada_b[i])[None, None, :], N_MOD, axis=-1)
            hc = _modulate(_rms_norm(cx, norm_mix_g[i]), mod_c[0], mod_c[1])
        if kind == 'chunk_mlp':
            prm = (cm_w_in[j], cm_v_norm_g[j], cm_w_s[j], cm_b_s[j], cm_w_out[j])
            yl = _chunk_mlp(hl, *prm)
            yc = _chunk_mlp(hc, *prm) if ctx_upd else None
        elif kind == 'fourier':
            yl = _fourier_mix(hl, fn_w_in[j], fn_w_out[j])
            yc = _fourier_mix(hc, fn_w_in[j], fn_w_out[j]) if ctx_upd else None
        elif kind == 'mlstm':
            yc, yl = _mlstm_mixer(hc, hl, ctx_upd, ml_w_in[j], ml_conv_w[j], ml_conv_b[j],
                                  ml_gate_b[j], ml_norm_g[j], ml_w_out[j])
        else:
            yc, yl = _rglru_mixer(hc, hl, ctx_upd, lru_w_in[j], lru_conv_w[j], lru_conv_b[j],
                                  lru_w_a[j], lru_b_a[j], lru_w_x[j], lru_b_x[j], lru_lambda[j],
                                  lru_w_out[j])
        lat = lat + mod_l[2] * yl
        if ctx_upd:
            cx = cx + mod_c[2] * yc
        moe_p = (router_group_w[i], router_group_b[i], router_expert_w[i], router_expert_b[i],
                 expert_w_gate[i], expert_w_up[i], expert_w_down[i])
        hl2 = _modulate(_rms_norm(lat, norm_ffn_g[i]), mod_l[3], mod_l[4])
        if ctx_upd:
            hc2 = _modulate(_rms_norm(cx, norm_ffn_g[i]), mod_c[3], mod_c[4])
            tok = jnp.concatenate([hc2, hl2], axis=1)
            y = _hier_moe(tok.reshape(-1, d), *moe_p).reshape(tok.shape)
            cx = cx + mod_c[5] * y[:, :n_ctx]
            lat = lat + mod_l[5] * y[:, n_ctx:]
        else:
            y = _hier_moe(hl2.reshape(-1, d), *moe_p).reshape(hl2.shape)
            lat = lat + mod_l[5] * y
    return _rms_norm(lat, final_norm_g)
```

```python
import math
from contextlib import ExitStack
import numpy as np
import concourse.bass as bass
import concourse.mybir as mybir
from concourse.bass_utils import run_bass_kernel_spmd
from concourse.ap import AP

F32 = mybir.dt.float32
BF16 = mybir.dt.bfloat16
I32 = mybir.dt.int32
AF = mybir.ActivationFunctionType
ALU = mybir.AluOpType
AX = mybir.AxisListType

D = 1024
SEQ = 16384
NCTX = 256
NT = SEQ + NCTX
DEPTH = 4
EPS = 1e-6
NEXP = 32
HID = 512
P = 128


class Res:
    __slots__ = ("w", "r")

    def __init__(self):
        self.w = None
        self.r = {}


class B:
    NDS = 28

    def __init__(self, nc):
        self.nc = nc
        self.engs = {"sp": nc.sync, "act": nc.scalar, "dve": nc.vector, "pool": nc.gpsimd, "pe": nc.tensor}
        self.csem = {e: nc.alloc_semaphore(name="c_" + e) for e in ("act", "dve", "pool", "pe")}
        self.ccnt = {e: 0 for e in self.csem}
        self.dsem = [nc.alloc_semaphore(name="d%d" % i) for i in range(self.NDS)]
        self.dcnt = [0] * self.NDS
        self.dnext = 0
        self.seen = {e: {} for e in self.engs}
        self.ninst = 0

    def _wait(self, e, tok):
        sem, val = tok
        k = id(sem)
        if self.seen[e].get(k, 0) >= val:
            return
        self.engs[e].wait_ge(sem, val)
        self.seen[e][k] = val
        self.ninst += 1

    def op(self, e, fn, reads=(), writes=(), dma=False, inc=True, ss=False, fence=None):
        toks = []
        for r in reads:
            if r.w is not None:
                toks.append(r.w)
        for w in writes:
            if w.w is not None:
                toks.append(w.w)
            toks.extend(w.r.values())
        own = self.csem.get(e)
        for t in toks:
            if (not dma) and t[0] is own and e == "pe":
                continue
            self._wait(e, t)
        self.ninst += 1
        if dma:
            j = self.dnext
            self.dnext = (j + 1) % self.NDS
            if self.dcnt[j] > 0:
                self._wait(e, (self.dsem[j], 16 * self.dcnt[j]))
            ins = fn()
            self.dcnt[j] += 1
            ins.then_inc(self.dsem[j], 16)
            tok = (self.dsem[j], 16 * self.dcnt[j])
        else:
            ins = fn()
            if not inc:
                return None
            if fence is not None:
                ins = fence()
                self.ninst += 1
            self.ccnt[e] += 1
            ins.then_inc(self.csem[e], 1)
            tok = (self.csem[e], self.ccnt[e])
        for r in reads:
            k = id(tok[0])
            o = r.r.get(k)
            if o is None or o[1] < tok[1]:
                r.r[k] = tok
        for w in writes:
            w.w = tok
            w.r = {}
        return tok

    ROT = 30000

    def barrier(self):
        for e in self.engs:
            self.finish(e)
        for e in list(self.csem):
            if self.ccnt[e] > self.ROT:
                self.epoch = getattr(self, "epoch", 0) + 1
                self.csem[e] = self.nc.alloc_semaphore(name="c_%s_%d" % (e, self.epoch))
                self.ccnt[e] = 0

    def finish(self, e="sp"):
        for j in range(self.NDS):
            if self.dcnt[j]:
                self._wait(e, (self.dsem[j], 16 * self.dcnt[j]))
        for k, s in self.csem.items():
            if self.ccnt[k]:
                self._wait(e, (s, self.ccnt[k]))


class Scope(ExitStack):
    def __init__(self, b):
        super().__init__()
        self._b = b

    def __exit__(self, *a):
        self._b.barrier()
        return super().__exit__(*a)


class T:
    def __init__(self, h, nres=1):
        self.h = h
        self.res = [Res() for _ in range(nres)]

    def __getitem__(self, k):
        return self.h[k]

    @property
    def r(self):
        return self.res[0]


class Pool:
    def __init__(self, tiles):
        self.tiles = tiles
        self.i = 0

    def get(self):
        t = self.tiles[self.i % len(self.tiles)]
        self.i += 1
        return t


def build(layers=(0, 1, 2, 3), in_res=False, dump_res=False, final=True, stage=9, ntl=None, out_res=False):
    nc = bass.Bass("TRN2", target_bir_lowering=False)
    b = B(nc)
    es = ExitStack()

    in_names = []
    b.in_names = in_names

    def dram_in(name, shape, dt=F32):
        in_names.append(name)
        return T(nc.dram_tensor(name, list(shape), dt, kind="ExternalInput").ap())

    def dram_tmp(name, shape, dt=F32):
        return T(nc.dram_tensor(name, list(shape), dt).ap())

    def sb(name, shape, dt=F32, stack=None):
        return T(nc.alloc_sbuf_tensor("s_" + name, list(shape), dt))

    def ps(name, shape, dt=F32):
        return T(nc.alloc_psum_tensor(name, list(shape), dt))

    if not in_res:
        xT = dram_in("xT", [D, SEQ])
        posT = dram_in("posT", [D, SEQ])
        ctxT = dram_in("ctxT", [D, NCTX])
    cT = dram_in("cT", [P, 8, 2])
    ada_w = {l: dram_in("ada_w%d" % l, [D, 6 * D]) for l in layers}
    ada_bT = dram_in("ada_bT", [DEPTH, P, 48])
    nmixT = dram_in("nmixT", [DEPTH, P, 8])
    nffnT = dram_in("nffnT", [DEPTH, P, 8])
    nfinT = dram_in("nfinT", [P, 8])
    rw = dram_in("rw", [DEPTH, D, 36])
    rbrep = dram_in("rbrep", [DEPTH, P, 36])
    if stage >= 5:
        wg = {l: dram_in("wg%d" % l, [NEXP * D, HID]) for l in layers}
        wu = {l: dram_in("wu%d" % l, [NEXP * D, HID]) for l in layers}
        wd = {l: dram_in("wd%d" % l, [NEXP * HID, D]) for l in layers}
    dbg = []
    if stage >= 5:
        wgb = dram_tmp("wgb", [NEXP * D, HID], BF16); wub = dram_tmp("wub", [NEXP * D, HID], BF16); wdb = dram_tmp("wdb", [NEXP * HID, D], BF16)
    if 0 in layers:
        cm_w_in = dram_in("cm_w_in", [D, 2 * D])
        cm_vg_rep = dram_in("cm_vg_rep", [P, D])
        cm_wsT = dram_in("cm_wsT", [P, 4, P])
        cm_bs_rep = dram_in("cm_bs_rep", [P, 4, 512])
        cm_w_out = dram_in("cm_w_out", [D, D])
    if 3 in layers:
        fn_w_in = dram_in("fn_w_in", [D, D]); fn_w_out = dram_in("fn_w_out", [D, D])
        fn_cs = dram_in("fn_cs", [P, 2, 512]); fn_dft = dram_in("fn_dft", [P, 3, P]); fn_tw = dram_in("fn_tw", [P, 2, P])
        fn_uv = dram_tmp("fn_uv", [SEQ, 2048]); fn_b = dram_tmp("fn_b", [P, P, 2048]); fn_r = dram_tmp("fn_r", [SEQ, D])
    if 1 in layers:
        ml_w_in = dram_in("ml_w_in", [D, 3088]); ml_w_out = dram_in("ml_w_out", [D, D])
        ml_gb = dram_in("ml_gb", [4, 4]); ml_cw = dram_in("ml_cw", [P, 8, 4]); ml_cb = dram_in("ml_cb", [P, 8])
        ml_mask = dram_in("ml_mask", [P, 2, P]); ml_ng = dram_in("ml_ng", [P, 8])
        ml_zqk = dram_tmp("ml_zqk", [D, NT]); ml_so = dram_tmp("ml_so", [D, NT], BF16); ml_v = dram_tmp("ml_v", [NT, D], BF16)
        ml_g = dram_tmp("ml_g", [4, 4, NT]); ml_q = dram_tmp("ml_q", [512, NT], BF16); ml_k = dram_tmp("ml_k", [512, NT], BF16)
        ml_hf = dram_tmp("ml_hf", [D, NT]); ml_gated = dram_tmp("ml_gated", [D, NT], BF16)
    LW = 1280
    if 2 in layers:
        lru_w_in = dram_in("lru_w_in", [D, 2 * LW])
        lru_w_out = dram_in("lru_w_out", [LW, D])
        lru_wa = dram_in("lru_wa", [P, 2, 10, P])
        lru_wx = dram_in("lru_wx", [P, 2, 10, P])
        lru_cw = dram_in("lru_cw", [P, 10, 4])
        lru_vec = dram_in("lru_vec", [P, 7, 10])
        lru_g = dram_tmp("lru_g", [LW, NT], BF16)
        lru_zx = dram_tmp("lru_zx", [LW, NT])
        lru_hf = dram_tmp("lru_hf", [LW, NT])
        lru_gated = dram_tmp("lru_gated", [LW, NT], BF16)
    ident_d = dram_in("ident", [P, P])
    ustrict_d = dram_in("ustrict", [P, P])
    iota32_d = dram_in("iota32", [P, 32])
    NBMAX = (2 * NT + NEXP * 127 + 127) // 128
    blkpos_d = dram_in("blkpos", [P, NBMAX])
    piota_d = dram_in("piota", [P, 1])
    sel_d = dram_in("sel", [32, NEXP * P])
    if final:
        outT = T(nc.dram_tensor("outT", [D, SEQ], F32, kind="ExternalOutput").ap())
    if out_res and not dump_res:
        res_out = T(nc.dram_tensor("res_out", [D, NT], F32, kind="ExternalOutput").ap())
    if in_res:
        res_in = dram_in("res_in", [D, NT])
    if dump_res:
        res_out = T(nc.dram_tensor("res_out", [D, NT], F32, kind="ExternalOutput").ap())
        info_out = T(nc.dram_tensor("info_out", [P, NT // P, 8], F32, kind="ExternalOutput").ap())
        dest_out = T(nc.dram_tensor("dest_out", [P, NT // P, 2], I32, kind="ExternalOutput").ap())
        carry_out = T(nc.dram_tensor("carry_out", [P, 32], F32, kind="ExternalOutput").ap())

    resd = dram_tmp("resd", [D, NT])
    h2rows = dram_tmp("h2rows", [NT, D], BF16)
    xbuf = dram_tmp("xbuf", [NBMAX * P, D], BF16)
    ybuf = dram_tmp("ybuf", [NBMAX * P, D], F32)

    def fm(t, c0, n):
        return t[:, c0:c0 + n].rearrange("(k p) t -> p k t", p=P)

    ident = sb("ident", [P, P])
    identb = sb("identb", [P, P], BF16)
    ones = sb("ones", [P, P])
    onesb = sb("onesb", [P, P], BF16)
    ustrict = sb("ustrictb", [P, P], BF16)
    ustrict_f = sb("ustrictf", [P, P])
    iota32 = sb("iota32", [P, 32])
    blkpos = sb("blkpos", [P, NBMAX])
    piota = sb("piota", [P, 1])
    sc = sb("sc", [P, 8, 2])
    modall = sb("modall", [P, DEPTH, 48, 2])
    nmix = sb("nmix", [P, DEPTH, 8])
    nffn = sb("nffn", [P, DEPTH, 8])
    nfin = sb("nfin", [P, 8])
    g1 = sb("g1", [P, 2, 8]); sh1 = sb("sh1", [P, 2, 8]); gt1 = sb("gt1", [P, 2, 8])
    g2 = sb("g2", [P, 2, 8]); sh2 = sb("sh2", [P, 2, 8]); gt2 = sb("gt2", [P, 2, 8])

    def dma(e, out_t, out_ap, in_t, in_ap):
        return b.op(e, lambda: b.engs[e].dma_start(out=out_ap, in_=in_ap), reads=[in_t.r], writes=[out_t.r], dma=True)

    dma("sp", ident, ident[:, :], ident_d, ident_d[:, :])
    dma("sp", ustrict_f, ustrict_f[:, :], ustrict_d, ustrict_d[:, :])
    dma("sp", iota32, iota32[:, :], iota32_d, iota32_d[:, :])
    dma("sp", blkpos, blkpos[:, :], blkpos_d, blkpos_d[:, :])
    dma("sp", piota, piota[:, :], piota_d, piota_d[:, :])
    dma("sp", sc, sc[:, :, :], cT, cT[:, :, :])
    dma("sp", nmix, nmix[:, :, :], nmixT, nmixT[:, :, :].rearrange("l p k -> p l k"))
    dma("sp", nffn, nffn[:, :, :], nffnT, nffnT[:, :, :].rearrange("l p k -> p l k"))
    dma("sp", nfin, nfin[:, :], nfinT, nfinT[:, :])
    V = nc.vector
    A = nc.scalar
    PE = nc.tensor
    G = nc.gpsimd
    b.op("dve", lambda: V.tensor_copy(out=identb[:, :], in_=ident[:, :]), [ident.r], [identb.r])
    b.op("dve", lambda: V.tensor_copy(out=ustrict[:, :], in_=ustrict_f[:, :]), [ustrict_f.r], [ustrict.r])
    b.op("dve", lambda: V.memset(ones[:, :], 1.0), [], [ones.r])
    b.op("dve", lambda: V.memset(onesb[:, :], 1.0), [], [onesb.r])
    b.op("act", lambda: A.activation(out=sc[:, :, :], in_=sc[:, :, :], func=AF.Silu), [sc.r], [sc.r])

    psf = Pool([ps("psf%d" % i, [P, 512]) for i in range(6)])
    psb = Pool([ps("psb%d" % i, [P, 1024], BF16) for i in range(2)])

    with Scope(b) as st:
        awt = T(st.enter_context(nc.sbuf_tensor("awt", [P, 8, 1024], F32)))
        abt = T(st.enter_context(nc.sbuf_tensor("abt", [P, 48], F32)))
        for L in layers:
            dma("sp", abt, abt[:, :], ada_bT, ada_bT[L, :, :])
            for m in range(6):
                dma("sp", awt, awt[:, :, :], ada_w[L],
                    ada_w[L][:, m * D:(m + 1) * D].rearrange("(k p) n -> p k n", p=P))
                pt = psf.get()
                for o in range(8):
                    for k in range(8):
                        last = (o == 7 and k == 7)
                        b.op("pe", lambda o=o, k=k: PE.matmul(pt[:, 2 * o:2 * o + 2], lhsT=awt[:, k, o * P:(o + 1) * P],
                                                               rhs=sc[:, k, :], start=(k == 0), stop=(k == 7)),
                             [awt.r, sc.r], [pt.r], inc=last)
                for j in range(2):
                    b.op("dve", lambda j=j, m=m: V.tensor_tensor(
                        out=modall[:, L, m * 8:(m + 1) * 8, j],
                        in0=pt[:, 0:16].rearrange("p (o j) -> p o j", j=2)[:, :, j],
                        in1=abt[:, m * 8:(m + 1) * 8], op=ALU.add), [pt.r, abt.r], [modall.r])

    def layer_scalars(L):
        for j in range(2):
            b.op("dve", lambda j=j: V.scalar_tensor_tensor(out=g1[:, j, :], in0=modall[:, L, 8:16, j], scalar=1.0,
                                                            in1=nmix[:, L, :], op0=ALU.add, op1=ALU.mult),
                 [modall.r, nmix.r], [g1.r])
            b.op("dve", lambda j=j: V.scalar_tensor_tensor(out=g2[:, j, :], in0=modall[:, L, 32:40, j], scalar=1.0,
                                                            in1=nffn[:, L, :], op0=ALU.add, op1=ALU.mult),
                 [modall.r, nffn.r], [g2.r])
            b.op("dve", lambda j=j: V.tensor_copy(out=sh1[:, j, :], in_=modall[:, L, 0:8, j]), [modall.r], [sh1.r])
            b.op("dve", lambda j=j: V.tensor_copy(out=gt1[:, j, :], in_=modall[:, L, 16:24, j]), [modall.r], [gt1.r])
            b.op("dve", lambda j=j: V.tensor_copy(out=sh2[:, j, :], in_=modall[:, L, 24:32, j]), [modall.r], [sh2.r])
            b.op("dve", lambda j=j: V.tensor_copy(out=gt2[:, j, :], in_=modall[:, L, 40:48, j]), [modall.r], [gt2.r])

    with Scope(b) as st:
        pa = Pool([T(st.enter_context(nc.sbuf_tensor("pa%d" % i, [P, 8, 512], F32))) for i in range(2)])
        pb = Pool([T(st.enter_context(nc.sbuf_tensor("pb%d" % i, [P, 8, 512], F32))) for i in range(2)])
        if in_res:
            for c0 in range(0, NT, 512):
                n = min(512, NT - c0)
                a = pa.get()
                dma("sp", a, a[:, :, :n], res_in, fm(res_in, c0, n))
                dma("sp", resd, fm(resd, c0, n), a, a[:, :, :n])
        else:
            a = pa.get()
            dma("sp", a, a[:, :, :NCTX], ctxT, fm(ctxT, 0, NCTX))
            dma("sp", resd, fm(resd, 0, NCTX), a, a[:, :, :NCTX])
            for c0 in range(0, SEQ, 512):
                a = pa.get(); p2 = pb.get()
                dma("sp", a, a[:, :, :], xT, fm(xT, c0, 512))
                dma("sp", p2, p2[:, :, :], posT, fm(posT, c0, 512))
                b.op("pool", lambda a=a, p2=p2: G.tensor_tensor(out=a[:, :, :], in0=a[:, :, :], in1=p2[:, :, :], op=ALU.add),
                     [a.r, p2.r], [a.r])
                dma("sp", resd, fm(resd, NCTX + c0, 512), a, a[:, :, :])

    def emit_dbg():
        for (nm, t_, ap_, shp, dt_) in dbg:
            o_ = T(nc.dram_tensor("dbg_" + nm, list(shp), dt_, kind="ExternalOutput").ap())
            b.op("pool", lambda o_=o_, ap_=ap_: G.dma_start(out=o_[tuple(slice(None) for _ in shp)], in_=ap_), [t_.r], [o_.r], dma=True)
        del dbg[:]

    def tiles(with_ctx):
        tl = [(0, NCTX, 1)] if with_ctx else []
        tl += [(NCTX + i * 512, 512, 0) for i in range(SEQ // 512)]
        if ntl is not None:
            tl = tl[:ntl]
        return tl

    def rmsnorm_mod(xin, n, j, gsc, shf, tbuf, sqb, out_tiles):
        pst = psf.get()
        for k in range(8):
            b.op("act", lambda k=k: A.activation(out=sqb[:, k, :n], in_=xin[:, k, :n], func=AF.Square), [xin.r], [sqb.r])
        for k in range(8):
            b.op("pe", lambda k=k: PE.matmul(pst[:, :n], lhsT=ones[:, :], rhs=sqb[:, k, :n], start=(k == 0), stop=(k == 7)),
                 [ones.r, sqb.r], [pst.r], inc=(k == 7))
        rstd = sqb
        b.op("act", lambda: A.activation(out=rstd[:, 0, :n], in_=pst[:, :n], func=AF.Sqrt, bias=epsc[:, 0:1], scale=1.0 / D),
             [pst.r, epsc.r], [sqb.r])
        b.op("dve", lambda: V.reciprocal(out=rstd[:, 0, :n], in_=rstd[:, 0, :n]), [sqb.r], [sqb.r])
        for k in range(8):
            b.op("dve", lambda k=k: V.tensor_tensor(out=tbuf[:, k, :n], in0=xin[:, k, :n], in1=rstd[:, 0, :n], op=ALU.mult),
                 [xin.r, sqb.r], [tbuf.r])
        for ot in out_tiles:
            for k in range(8):
                b.op("act", lambda k=k, ot=ot: A.activation(out=ot[:, k, :n], in_=tbuf[:, k, :n], func=AF.Identity,
                                                           bias=shf[:, j, k:k + 1], scale=gsc[:, j, k:k + 1]),
                     [tbuf.r, shf.r, gsc.r], [ot.r])

    epsc = sb("epsc", [P, 1])
    b.op("dve", lambda: V.memset(epsc[:, :], EPS), [], [epsc.r])
    onec = sb("onec", [P, 1])
    b.op("dve", lambda: V.memset(onec[:, :], 1.0), [], [onec.r])

    def gelu_tanh(out_ap, out_t, in_ap, in_t, tmp, n_shape):
        t1, t2 = tmp
        b.op("act", lambda: A.activation(out=t1[0], in_=in_ap, func=AF.Square), [in_t.r], [t1[1].r])
        b.op("dve", lambda: V.tensor_scalar(out=t1[0], in0=t1[0], scalar1=0.044715, scalar2=1.0, op0=ALU.mult, op1=ALU.add),
             [t1[1].r], [t1[1].r])
        b.op("dve", lambda: V.tensor_tensor(out=t1[0], in0=t1[0], in1=in_ap, op=ALU.mult), [t1[1].r, in_t.r], [t1[1].r])
        b.op("act", lambda: A.activation(out=t2[0], in_=t1[0], func=AF.Sigmoid, scale=1.5957691216057308),
             [t1[1].r], [t2[1].r])
        b.op("dve", lambda: V.tensor_tensor(out=out_ap, in0=t2[0], in1=in_ap, op=ALU.mult), [t2[1].r, in_t.r], [out_t.r])

    class Moe:
        pass

    m = Moe()
    m.rwt = sb("rwt", [P, 8, 36]); m.rbt = sb("rbt", [P, 36]); m.carry = sb("carry", [P, 32])
    m.info = sb("rinfo", [P, NT // P, 8])
    m.dest = T(nc.alloc_sbuf_tensor("dest", [P, NT // P, 2], I32))
    m.sm = Pool([sb("rsm%d" % i, [P, 256]) for i in range(2)])
    m.mb = Pool([T(nc.alloc_sbuf_tensor("mb%d" % i, [P, 32], BF16)) for i in range(2)])

    def moe_begin(m, L, ntok):
        m.nch = ntok // P
        m.nb = (2 * ntok + NEXP * 127 + 127) // 128
        dma("sp", m.rwt, m.rwt[:, :, :], rw, rw[L, :, :].rearrange("(k p) n -> p k n", p=P))
        dma("sp", m.rbt, m.rbt[:, :], rbrep, rbrep[L, :, :])
        b.op("dve", lambda: V.memset(m.carry[:, :], 0.0), [], [m.carry.r])

    def moe_route_chunk(m, h2f, off, ch, wt=None):
        s = m.sm.get()
        S = s.h
        pl = psf.get()
        for k in range(8):
            b.op("pe", lambda k=k: PE.matmul(pl[:, 0:36], lhsT=h2f[:, k, off:off + P], rhs=m.rwt[:, k, :],
                                              start=(k == 0), stop=(k == 7)), [h2f.r, m.rwt.r], [pl.r], inc=(k == 7))
        ops = []
        rs = [s.r]
        b.op("dve", lambda: V.tensor_tensor(out=S[:, 0:36], in0=pl[:, 0:36], in1=m.rbt[:, :], op=ALU.add), [pl.r, m.rbt.r], rs)
        b.op("dve", lambda: V.reduce_max(out=S[:, 40:41], in_=S[:, 0:4], axis=AX.X), rs, rs, ss=True)
        b.op("dve", lambda: V.tensor_scalar(out=S[:, 36:40], in0=S[:, 0:4], scalar1=S[:, 40:41], scalar2=None, op0=ALU.is_ge), rs, rs, ss=True)
        b.op("dve", lambda: V.tensor_scalar(out=S[:, 208:212], in0=S[:, 0:4], scalar1=S[:, 40:41], scalar2=None, op0=ALU.subtract), rs, rs, ss=True)
        b.op("act", lambda: A.activation(out=S[:, 208:212], in_=S[:, 208:212], func=AF.Exp), rs, rs, ss=True)
        b.op("dve", lambda: V.reduce_sum(out=S[:, 41:42], in_=S[:, 208:212], axis=AX.X), rs, rs, ss=True)
        b.op("dve", lambda: V.reciprocal(out=S[:, 41:42], in_=S[:, 41:42]), rs, rs)
        b.op("dve", lambda: V.tensor_scalar(out=S[:, 44:52], in0=S[:, 4:12], scalar1=S[:, 36:37], scalar2=None, op0=ALU.mult), rs, rs, ss=True)
        for g in range(1, 4):
            b.op("dve", lambda g=g: V.scalar_tensor_tensor(out=S[:, 44:52], in0=S[:, 4 + 8 * g:12 + 8 * g], scalar=S[:, 36 + g:37 + g],
                                                            in1=S[:, 44:52], op0=ALU.mult, op1=ALU.add), rs, rs, ss=True)
        b.op("dve", lambda: V.reduce_max(out=S[:, 76:77], in_=S[:, 44:52], axis=AX.X), rs, rs, ss=True)
        b.op("dve", lambda: V.tensor_scalar(out=S[:, 52:60], in0=S[:, 44:52], scalar1=S[:, 76:77], scalar2=None, op0=ALU.is_ge), rs, rs, ss=True)
        b.op("dve", lambda: V.scalar_tensor_tensor(out=S[:, 60:68], in0=S[:, 52:60], scalar=-1e30, in1=S[:, 44:52],
                                                    op0=ALU.mult, op1=ALU.add), rs, rs, ss=True)
        b.op("dve", lambda: V.reduce_max(out=S[:, 77:78], in_=S[:, 60:68], axis=AX.X), rs, rs, ss=True)
        b.op("dve", lambda: V.tensor_scalar(out=S[:, 68:76], in0=S[:, 60:68], scalar1=S[:, 77:78], scalar2=None, op0=ALU.is_ge), rs, rs, ss=True)
        b.op("dve", lambda: V.tensor_tensor(out=S[:, 78:79], in0=S[:, 77:78], in1=S[:, 76:77], op=ALU.subtract), rs, rs, ss=True)
        b.op("act", lambda: A.activation(out=S[:, 78:79], in_=S[:, 78:79], func=AF.Exp), rs, rs)
        b.op("dve", lambda: V.tensor_scalar(out=S[:, 79:80], in0=S[:, 78:79], scalar1=1.0, scalar2=None, op0=ALU.add), rs, rs, ss=True)
        b.op("dve", lambda: V.reciprocal(out=S[:, 79:80], in_=S[:, 79:80]), rs, rs, ss=True)
        ir = [m.info.r]
        b.op("dve", lambda: V.tensor_tensor(out=m.info[:, ch, 2:3], in0=S[:, 79:80], in1=S[:, 41:42], op=ALU.mult), rs, ir)
        b.op("dve", lambda: V.tensor_tensor(out=m.info[:, ch, 3:4], in0=m.info[:, ch, 2:3], in1=S[:, 78:79], op=ALU.mult), rs + ir, ir, ss=True)
        for g in range(4):
            b.op("dve", lambda g=g: V.tensor_scalar(out=S[:, 80 + 8 * g:88 + 8 * g], in0=S[:, 52:60], scalar1=S[:, 36 + g:37 + g],
                                                     scalar2=None, op0=ALU.mult), rs, rs, ss=True)
            b.op("dve", lambda g=g: V.tensor_scalar(out=S[:, 112 + 8 * g:120 + 8 * g], in0=S[:, 68:76], scalar1=S[:, 36 + g:37 + g],
                                                     scalar2=None, op0=ALU.mult), rs, rs, ss=True)
        if wt is not None:
            wt_t, wt_ap = wt
            b.op("dve", lambda: V.tensor_scalar(out=S[:, 144:176], in0=S[:, 80:112], scalar1=m.info[:, ch, 2:3], scalar2=None, op0=ALU.mult),
                 rs + ir, rs)
            b.op("dve", lambda: V.scalar_tensor_tensor(out=wt_ap, in0=S[:, 112:144], scalar=m.info[:, ch, 3:4], in1=S[:, 144:176],
                                                        op0=ALU.mult, op1=ALU.add), rs + ir, [wt_t.r])
            return
        mb = m.mb.get()
        b.op("dve", lambda: V.tensor_tensor(out=mb[:, :], in0=S[:, 80:112], in1=S[:, 112:144], op=ALU.add), rs, [mb.r])
        b.op("dve", lambda: V.tensor_tensor(out=S[:, 144:176], in0=S[:, 80:112], in1=iota32[:, :], op=ALU.mult), rs + [iota32.r], rs)
        b.op("dve", lambda: V.reduce_sum(out=m.info[:, ch, 0:1], in_=S[:, 144:176], axis=AX.X), rs, ir, ss=True)
        b.op("dve", lambda: V.tensor_tensor(out=S[:, 144:176], in0=S[:, 112:144], in1=iota32[:, :], op=ALU.mult), rs + [iota32.r], rs)
        b.op("dve", lambda: V.reduce_sum(out=m.info[:, ch, 1:2], in_=S[:, 144:176], axis=AX.X), rs, ir, ss=True)
        pc = psf.get()
        b.op("pe", lambda: PE.matmul(pc[:, 0:32], lhsT=ustrict[:, :], rhs=mb[:, :], start=True, stop=True), [ustrict.r, mb.r], [pc.r], inc=False)
        b.op("pe", lambda: PE.matmul(pc[:, 32:64], lhsT=onesb[:, :], rhs=mb[:, :], start=True, stop=True), [onesb.r, mb.r], [pc.r])
        b.op("dve", lambda: V.tensor_tensor(out=S[:, 176:208], in0=pc[:, 0:32], in1=m.carry[:, :], op=ALU.add), [pc.r, m.carry.r], rs)
        b.op("dve", lambda: V.tensor_tensor(out=m.carry[:, :], in0=pc[:, 32:64], in1=m.carry[:, :], op=ALU.add), [pc.r, m.carry.r], [m.carry.r])
        b.op("dve", lambda: V.tensor_tensor(out=S[:, 144:176], in0=S[:, 80:112], in1=S[:, 176:208], op=ALU.mult), rs, rs, ss=True)
        b.op("dve", lambda: V.reduce_sum(out=m.info[:, ch, 4:5], in_=S[:, 144:176], axis=AX.X), rs, ir, ss=True)
        b.op("dve", lambda: V.tensor_tensor(out=S[:, 144:176], in0=S[:, 112:144], in1=S[:, 176:208], op=ALU.mult), rs, rs, ss=True)
        b.op("dve", lambda: V.reduce_sum(out=m.info[:, ch, 5:6], in_=S[:, 144:176], axis=AX.X), rs, ir, ss=True)

    def moe_rows_chunk(m, h2b, off, tok0, rows):
        pt = psb.get()
        for k in range(8):
            b.op("pe", lambda k=k: PE.transpose(out=pt[:, k * P:(k + 1) * P], in_=h2b[:, k, off:off + P], identity=identb[:, :]),
                 [h2b.r, identb.r], [pt.r], inc=(k == 7))
        b.op("act", lambda: A.copy(out=rows[:, :], in_=pt[:, :]), [pt.r], [rows.r])
        dma("sp", h2rows, h2rows[tok0:tok0 + P, :], rows, rows[:, :])

    def moe_finish(m, L, tl, gt, tok_base):
        nb = m.nb
        with Scope(b) as st:
            def S_(name, shape, dt=F32):
                return T(st.enter_context(nc.sbuf_tensor(name, list(shape), dt)))
            padded = S_("padded", [P, 32]); pend = S_("pend", [P, 32]); pstart = S_("pstart", [P, 32])
            cmp3 = S_("cmp3", [P, nb, 32]); eb = S_("eb", [P, nb]); idxg = S_("idxg", [P, nb], I32); idxd = S_("idxd", [P, nb], I32)
            tmp32 = S_("tmp32", [P, 32]); destf = S_("destf", [P, m.nch, 2])
            b.op("dve", lambda: V.tensor_scalar(out=tmp32[:, :], in0=m.carry[:, :], scalar1=127.0, scalar2=1.0 / 128.0, op0=ALU.add, op1=ALU.mult),
                 [m.carry.r], [tmp32.r])
            b.op("dve", lambda: V.tensor_scalar(out=tmp32[:, :], in0=tmp32[:, :], scalar1=-0.498046875, scalar2=None, op0=ALU.add), [tmp32.r], [tmp32.r])
            b.op("dve", lambda: V.tensor_scalar(out=tmp32[:, :], in0=tmp32[:, :], scalar1=8388608.0, scalar2=None, op0=ALU.add), [tmp32.r], [tmp32.r])
            b.op("dve", lambda: V.tensor_scalar(out=padded[:, :], in0=tmp32[:, :], scalar1=-8388608.0, scalar2=128.0, op0=ALU.add, op1=ALU.mult),
                 [tmp32.r], [padded.r])
            pp = [pend, tmp32]
            b.op("dve", lambda: V.tensor_copy(out=pend[:, :], in_=padded[:, :]), [padded.r], [pend.r])
            cur = 0
            for sh in (1, 2, 4, 8, 16):
                A_, B_ = pp[cur], pp[1 - cur]
                b.op("dve", lambda A_=A_, B_=B_, sh=sh: V.tensor_copy(out=B_[:, 0:sh], in_=A_[:, 0:sh]), [A_.r], [B_.r])
                b.op("dve", lambda A_=A_, B_=B_, sh=sh: V.tensor_tensor(out=B_[:, sh:32], in0=A_[:, sh:32], in1=A_[:, 0:32 - sh], op=ALU.add), [A_.r], [B_.r])
                cur = 1 - cur
            if cur == 1:
                b.op("dve", lambda: V.tensor_copy(out=pend[:, :], in_=tmp32[:, :]), [tmp32.r], [pend.r])
            b.op("dve", lambda: V.tensor_tensor(out=pstart[:, :], in0=pend[:, :], in1=padded[:, :], op=ALU.subtract),
                 [pend.r, padded.r], [pstart.r])
            b.op("dve", lambda: V.tensor_tensor(out=cmp3[:, :, :], in0=pend[:, :].unsqueeze(1).to_broadcast([P, nb, 32]),
                                                 in1=blkpos[:, 0:nb].unsqueeze(2).to_broadcast([P, nb, 32]), op=ALU.is_le),
                 [pend.r, blkpos.r], [cmp3.r])
            b.op("dve", lambda: V.reduce_sum(out=eb[:, :], in_=cmp3[:, :, :], axis=AX.X), [cmp3.r], [eb.r])
            b.op("dve", lambda: V.tensor_scalar(out=eb[:, :], in0=eb[:, :], scalar1=31.0, scalar2=None, op0=ALU.min), [eb.r], [eb.r])
            b.op("dve", lambda: V.tensor_scalar(out=idxg[:, :], in0=eb[:, :], scalar1=float(D), scalar2=piota[:, 0:1], op0=ALU.mult, op1=ALU.add),
                 [eb.r, piota.r], [idxg.r])
            b.op("dve", lambda: V.tensor_scalar(out=idxd[:, :], in0=eb[:, :], scalar1=float(HID), scalar2=piota[:, 0:1], op0=ALU.mult, op1=ALU.add),
                 [eb.r, piota.r], [idxd.r])
            oh = S_("ohd", [P, 32])
            for ch in range(m.nch):
                for kk in range(2):
                    b.op("dve", lambda ch=ch, kk=kk: V.tensor_scalar(out=oh[:, :], in0=iota32[:, :], scalar1=m.info[:, ch, kk:kk + 1],
                                                                      scalar2=None, op0=ALU.is_equal), [iota32.r, m.info.r], [oh.r])
                    b.op("dve", lambda: V.tensor_tensor(out=oh[:, :], in0=oh[:, :], in1=pstart[:, :], op=ALU.mult), [oh.r, pstart.r], [oh.r])
                    b.op("dve", lambda ch=ch, kk=kk: V.reduce_sum(out=destf[:, ch, kk:kk + 1], in_=oh[:, :], axis=AX.X), [oh.r], [destf.r])
            b.op("dve", lambda: V.tensor_tensor(out=destf[:, :, :], in0=destf[:, :, :], in1=m.info[:, 0:m.nch, 4:6], op=ALU.add),
                 [destf.r, m.info.r], [destf.r])
            b.op("dve", lambda: V.tensor_copy(out=m.dest[:, 0:m.nch, :], in_=destf[:, :, :]), [destf.r], [m.dest.r])
            if stage < 4:
                if dump_res:
                    dbg.extend([("padded", padded, padded[:, :], [P, 32], F32), ("pend", pend, pend[:, :], [P, 32], F32),
                                ("pstart", pstart, pstart[:, :], [P, 32], F32), ("destf", destf, destf[:, :, :], [P, m.nch, 2], F32),
                                ("eb", eb, eb[:, :], [P, nb], F32), ("idxg", idxg, idxg[:, :], [P, nb], I32)])
                    emit_dbg()
                    b.barrier()
                return
            rp = Pool([S_("scr%d" % i, [P, D], BF16) for i in range(3)])
            for ch in range(m.nch):
                r_ = rp.get()
                dma("sp", r_, r_[:, :], h2rows, h2rows[ch * P:(ch + 1) * P, :])
                for kk in range(2):
                    b.op("pool", lambda ch=ch, kk=kk, r_=r_: G.indirect_dma_start(
                        out=xbuf[:, :], out_offset=bass.IndirectOffsetOnAxis(ap=m.dest[:, ch, kk:kk + 1], axis=0),
                        in_=r_[:, :], in_offset=None), [r_.r, m.dest.r], [xbuf.r], dma=True)
            if stage < 5:
                return
            wgp = Pool([S_("wgt%d" % i, [P, 8, HID], BF16) for i in range(2)])
            wup = Pool([S_("wut%d" % i, [P, 8, HID], BF16) for i in range(2)])
            wdp = Pool([S_("wdt%d" % i, [P, 4, D], BF16) for i in range(2)])
            xrp = Pool([S_("xr%d" % i, [P, D], BF16) for i in range(2)])
            xtp = Pool([S_("xt%d" % i, [P, 8, P], BF16) for i in range(2)])
            hp = Pool([S_("hh%d" % i, [P, 4, P], BF16) for i in range(2)])
            sgp = Pool([S_("sg%d" % i, [P, P], F32) for i in range(2)])
            yp = Pool([S_("yy%d" % i, [P, D], F32) for i in range(2)])
            wgL = wg[L][:, :]; wuL = wu[L][:, :]; wdL = wd[L][:, :]
            for blk in range(nb):
                wgt = wgp.get(); wut = wup.get(); wdt = wdp.get()
                for k in range(8):
                    b.op("pool", lambda k=k, wgt=wgt, blk=blk: G.indirect_dma_start(
                        out=wgt[:, k, :], out_offset=None, in_=wgL,
                        in_offset=bass.IndirectOffsetOnAxis(ap=idxg[:, blk:blk + 1], axis=0), element_offset=k * P * HID),
                        [wg[L].r, idxg.r], [wgt.r], dma=True)
                    b.op("pool", lambda k=k, wut=wut, blk=blk: G.indirect_dma_start(
                        out=wut[:, k, :], out_offset=None, in_=wuL,
                        in_offset=bass.IndirectOffsetOnAxis(ap=idxg[:, blk:blk + 1], axis=0), element_offset=k * P * HID),
                        [wu[L].r, idxg.r], [wut.r], dma=True)
                for k in range(4):
                    b.op("pool", lambda k=k, wdt=wdt, blk=blk: G.indirect_dma_start(
                        out=wdt[:, k, :], out_offset=None, in_=wdL,
                        in_offset=bass.IndirectOffsetOnAxis(ap=idxd[:, blk:blk + 1], axis=0), element_offset=k * P * D),
                        [wd[L].r, idxd.r], [wdt.r], dma=True)
                xr = xrp.get(); xt = xtp.get()
                dma("sp", xr, xr[:, :], xbuf, xbuf[blk * P:(blk + 1) * P, :])
                pt = psb.get()
                for k in range(8):
                    b.op("pe", lambda k=k: PE.transpose(out=pt[:, k * P:(k + 1) * P], in_=xr[:, k * P:(k + 1) * P], identity=identb[:, :]),
                         [xr.r, identb.r], [pt.r], inc=(k == 7))
                b.op("act", lambda: A.copy(out=xt[:, :, :], in_=pt[:, :].rearrange("p (k s) -> p k s", k=8)), [pt.r], [xt.r])
                hh = hp.get()
                for j in range(4):
                    pg = psf.get()
                    for k in range(8):
                        b.op("pe", lambda k=k, j=j: PE.matmul(pg[:, 0:P], lhsT=wgt[:, k, j * P:(j + 1) * P], rhs=xt[:, k, :],
                                                               start=(k == 0), stop=(k == 7)), [wgt.r, xt.r], [pg.r], inc=False)
                    for k in range(8):
                        b.op("pe", lambda k=k, j=j: PE.matmul(pg[:, P:2 * P], lhsT=wut[:, k, j * P:(j + 1) * P], rhs=xt[:, k, :],
                                                               start=(k == 0), stop=(k == 7)), [wut.r, xt.r], [pg.r], inc=(k == 7))
                    sg = sgp.get()
                    b.op("act", lambda: A.activation(out=sg[:, :], in_=pg[:, 0:P], func=AF.Silu), [pg.r], [sg.r])
                    b.op("dve", lambda j=j: V.tensor_tensor(out=hh[:, j, :], in0=sg[:, :], in1=pg[:, P:2 * P], op=ALU.mult),
                         [sg.r, pg.r], [hh.r])
                yy = yp.get()
                for half in range(2):
                    py = psf.get()
                    for j in range(4):
                        b.op("pe", lambda j=j, half=half: PE.matmul(py[:, :], lhsT=hh[:, j, :], rhs=wdt[:, j, half * 512:(half + 1) * 512],
                                                                     start=(j == 0), stop=(j == 3)), [hh.r, wdt.r], [py.r], inc=(j == 3))
                    if half == 0:
                        b.op("act", lambda: A.copy(out=yy[:, 0:512], in_=py[:, :]), [py.r], [yy.r])
                    else:
                        b.op("dve", lambda: V.tensor_copy(out=yy[:, 512:1024], in_=py[:, :]), [py.r], [yy.r])
                dma("sp", ybuf, ybuf[blk * P:(blk + 1) * P, :], yy, yy[:, :])
        if stage < 6:
            return
        with Scope(b) as st:
            def S_(name, shape, dt=F32):
                return T(st.enter_context(nc.sbuf_tensor(name, list(shape), dt)))
            y0p = Pool([S_("y0_%d" % i, [P, D]) for i in range(2)])
            y1p = Pool([S_("y1_%d" % i, [P, D]) for i in range(2)])
            xp = Pool([S_("xd%d" % i, [P, 8, 512]) for i in range(2)])
            for (c0, n, isctx) in tl:
                xin = xp.get()
                dma("sp", xin, xin[:, :, :n], resd, fm(resd, c0, n))
                for ci in range(n // P):
                    ch = (c0 - tok_base) // P + ci
                    y0 = y0p.get(); y1 = y1p.get()
                    for kk, yt in ((0, y0), (1, y1)):
                        b.op("pool", lambda ch=ch, kk=kk, yt=yt: G.indirect_dma_start(
                            out=yt[:, :], out_offset=None, in_=ybuf[:, :],
                            in_offset=bass.IndirectOffsetOnAxis(ap=m.dest[:, ch, kk:kk + 1], axis=0)),
                            [ybuf.r, m.dest.r], [yt.r], dma=True)
                    b.op("dve", lambda ch=ch, y0=y0: V.tensor_scalar(out=y0[:, :], in0=y0[:, :], scalar1=m.info[:, ch, 2:3], scalar2=None,
                                                                      op0=ALU.mult), [y0.r, m.info.r], [y0.r])
                    b.op("dve", lambda ch=ch, y0=y0, y1=y1: V.scalar_tensor_tensor(out=y0[:, :], in0=y1[:, :], scalar=m.info[:, ch, 3:4],
                                                                                    in1=y0[:, :], op0=ALU.mult, op1=ALU.add),
                         [y0.r, y1.r, m.info.r], [y0.r])
                    for hf in range(2):
                        pt = psf.get()
                        for kq in range(4):
                            k = hf * 4 + kq
                            b.op("pe", lambda k=k, kq=kq, y0=y0, pt=pt: PE.transpose(out=pt[:, kq * P:(kq + 1) * P], in_=y0[:, k * P:(k + 1) * P],
                                                                                     identity=ident[:, :]), [y0.r, ident.r], [pt.r], inc=(kq == 3))
                        for kq in range(4):
                            k = hf * 4 + kq
                            b.op("dve", lambda k=k, kq=kq, pt=pt, ci=ci: V.scalar_tensor_tensor(
                                out=xin[:, k, ci * P:(ci + 1) * P], in0=pt[:, kq * P:(kq + 1) * P], scalar=gt[:, isctx, k:k + 1],
                                in1=xin[:, k, ci * P:(ci + 1) * P], op0=ALU.mult, op1=ALU.add), [pt.r, gt.r, xin.r], [xin.r])
                dma("sp", resd, fm(resd, c0, n), xin, xin[:, :, :n])

    GT = 2

    def moe_dense(L, tl):
        moe_begin(m, L, sum(t[1] for t in tl))
        wgL = wg[L]; wuL = wu[L]; wdL = wd[L]
        with Scope(b) as st:
            def S0(name, shape, dt=F32):
                return T(st.enter_context(nc.sbuf_tensor("%s_L%d" % (name, L), list(shape), dt)))
            cg = Pool([S0("pcg%d" % i, [P, 8, HID], BF16) for i in range(3)])
            cu = Pool([S0("pcu%d" % i, [P, 8, HID], BF16) for i in range(3)])
            cd = Pool([S0("pcd%d" % i, [P, 4, D], BF16) for i in range(3)])
            for e in range(NEXP):
                a_ = cg.get(); u_ = cu.get(); d_ = cd.get()
                b.op("pool", lambda a_=a_: G.dma_start(out=a_[:, :, :], in_=wgL[e * D:(e + 1) * D, :].rearrange("(k p) n -> p k n", p=P)),
                     [wgL.r], [a_.r], dma=True)
                b.op("pool", lambda u_=u_: G.dma_start(out=u_[:, :, :], in_=wuL[e * D:(e + 1) * D, :].rearrange("(k p) n -> p k n", p=P)),
                     [wuL.r], [u_.r], dma=True)
                b.op("pool", lambda d_=d_: G.dma_start(out=d_[:, :, :], in_=wdL[e * HID:(e + 1) * HID, :].rearrange("(k p) n -> p k n", p=P)),
                     [wdL.r], [d_.r], dma=True)
                dma("sp", wgb, wgb[e * D:(e + 1) * D, :].rearrange("(k p) n -> p k n", p=P), a_, a_[:, :, :])
                dma("sp", wub, wub[e * D:(e + 1) * D, :].rearrange("(k p) n -> p k n", p=P), u_, u_[:, :, :])
                dma("sp", wdb, wdb[e * HID:(e + 1) * HID, :].rearrange("(k p) n -> p k n", p=P), d_, d_[:, :, :])
        with Scope(b) as st:
            def S_(name, shape, dt=F32):
                return T(st.enter_context(nc.sbuf_tensor("%s_L%d" % (name, L), list(shape), dt)))
            xin = S_("mx", [P, 8, 512]); sqb = S_("msq", [P, 8, 512]); tb = xin
            h2 = [S_("mh2_%d" % i, [P, 8, 512], BF16) for i in range(GT)]
            yac = [S_("myac%d" % i, [P, 8, 512]) for i in range(GT)]
            wtT = [S_("mwtT%d" % i, [32, 512]) for i in range(GT)]
            wtc = S_("mwtc", [P, 4, 32])
            sel = S_("msel", [32, NEXP * P])
            dma("sp", sel, sel[:, :], sel_d, sel_d[:, :])
            wgp = Pool([S_("mwg%d" % i, [P, 8, HID], BF16) for i in range(2)])
            wup = Pool([S_("mwu%d" % i, [P, 8, HID], BF16) for i in range(2)])
            wdp = Pool([S_("mwd%d" % i, [P, 4, D], BF16) for i in range(2)])
            wrow = S_("mwrow", [P, 512]); sg = S_("msg", [P, 512]); tt = S_("mtt", [P, 512])
            hsp = Pool([S_("mhs%d" % i, [P, 4, 512], BF16) for i in range(2)])
            for g0 in range(0, len(tl), GT):
                grp = tl[g0:g0 + GT]
                for i, (c0, n, isctx) in enumerate(grp):
                    dma("sp", xin, xin[:, :, :n], resd, fm(resd, c0, n))
                    rmsnorm_mod(xin, n, isctx, g2, sh2, tb, sqb, [sqb, h2[i]])
                    for ci in range(n // P):
                        moe_route_chunk(m, sqb, ci * P, c0 // P + ci, wt=(wtc, wtc[:, ci, :]))
                    pT = psf.get()
                    for ci in range(n // P):
                        b.op("pe", lambda ci=ci: PE.transpose(out=pT[0:32, ci * P:(ci + 1) * P], in_=wtc[:, ci, :], identity=ident[:, :]),
                             [wtc.r, ident.r], [pT.r], inc=(ci == n // P - 1))
                    b.op("act", lambda i=i, n=n: A.copy(out=wtT[i][:, :n], in_=pT[0:32, :n]), [pT.r], [wtT[i].r])
                    b.op("pool", lambda i=i: G.memset(yac[i][:, :, :], 0.0), [], [yac[i].r])
                for e in range(NEXP):
                    wgt = wgp.get(); wut = wup.get(); wdt = wdp.get()
                    b.op("pool", lambda: G.dma_start(out=wgt[:, :, :], in_=wgb[e * D:(e + 1) * D, :].rearrange("(k p) n -> p k n", p=P)),
                         [wgb.r], [wgt.r], dma=True)
                    b.op("pool", lambda: G.dma_start(out=wut[:, :, :], in_=wub[e * D:(e + 1) * D, :].rearrange("(k p) n -> p k n", p=P)),
                         [wub.r], [wut.r], dma=True)
                    b.op("pool", lambda: G.dma_start(out=wdt[:, :, :], in_=wdb[e * HID:(e + 1) * HID, :].rearrange("(k p) n -> p k n", p=P)),
                         [wdb.r], [wdt.r], dma=True)
                    for i, (c0, n, isctx) in enumerate(grp):
                        pw = psf.get()
                        b.op("pe", lambda i=i, n=n: PE.matmul(pw[:, :n], lhsT=sel[:, e * P:(e + 1) * P], rhs=wtT[i][:, :n], start=True, stop=True),
                             [sel.r, wtT[i].r], [pw.r])
                        b.op("act", lambda n=n: A.copy(out=wrow[:, :n], in_=pw[:, :n]), [pw.r], [wrow.r])
                        hs = hsp.get()
                        for j in range(4):
                            pg = psf.get(); pu = psf.get()
                            for k in range(8):
                                b.op("pe", lambda k=k, j=j, i=i, n=n: PE.matmul(pg[:, :n], lhsT=wgt[:, k, j * P:(j + 1) * P], rhs=h2[i][:, k, :n],
                                                                               start=(k == 0), stop=(k == 7)), [wgt.r, h2[i].r], [pg.r], inc=(k == 7))
                            for k in range(8):
                                b.op("pe", lambda k=k, j=j, i=i, n=n: PE.matmul(pu[:, :n], lhsT=wut[:, k, j * P:(j + 1) * P], rhs=h2[i][:, k, :n],
                                                                               start=(k == 0), stop=(k == 7)), [wut.r, h2[i].r], [pu.r], inc=(k == 7))
                            b.op("act", lambda n=n: A.activation(out=sg[:, :n], in_=pg[:, :n], func=AF.Silu), [pg.r], [sg.r])
                            b.op("dve", lambda n=n: V.tensor_tensor(out=tt[:, :n], in0=sg[:, :n], in1=pu[:, :n], op=ALU.mult), [sg.r, pu.r], [tt.r])
                            b.op("dve", lambda n=n, j=j: V.tensor_tensor(out=hs[:, j, :n], in0=tt[:, :n], in1=wrow[:, :n], op=ALU.mult),
                                 [tt.r, wrow.r], [hs.r])
                        for o in range(8):
                            py = psf.get()
                            for j in range(4):
                                b.op("pe", lambda o=o, j=j, n=n: PE.matmul(py[:, :n], lhsT=wdt[:, j, o * P:(o + 1) * P], rhs=hs[:, j, :n],
                                                                          start=(j == 0), stop=(j == 3)), [wdt.r, hs.r], [py.r], inc=(j == 3))
                            b.op("dve", lambda o=o, i=i, n=n: V.tensor_tensor(out=yac[i][:, o, :n], in0=yac[i][:, o, :n], in1=py[:, :n], op=ALU.add),
                                 [py.r, yac[i].r], [yac[i].r])
                for i, (c0, n, isctx) in enumerate(grp):
                    dma("sp", xin, xin[:, :, :n], resd, fm(resd, c0, n))
                    for o in range(8):
                        b.op("dve", lambda o=o, i=i, n=n, isctx=isctx: V.scalar_tensor_tensor(
                            out=xin[:, o, :n], in0=yac[i][:, o, :n], scalar=gt2[:, isctx, o:o + 1], in1=xin[:, o, :n],
                            op0=ALU.mult, op1=ALU.add), [yac[i].r, gt2.r, xin.r], [xin.r])
                    dma("sp", resd, fm(resd, c0, n), xin, xin[:, :, :n])

    def layer2(L):
        layer_scalars(L)
        tl = tiles(True)
        with Scope(b) as st:
            def S_(name, shape, dt=F32):
                return T(st.enter_context(nc.sbuf_tensor(name, list(shape), dt)))
            win = S_("lwin", [P, 8, 2 * LW], BF16)
            b.op("pool", lambda: G.dma_start(out=win[:, :, :], in_=lru_w_in[:, :].rearrange("(k p) n -> p k n", p=P)), [lru_w_in.r], [win.r], dma=True)
            xin = S_("lxin", [P, 8, 512]); sqb = S_("lsqb", [P, 8, 512])
            hT = S_("lhT", [P, 8, 512], BF16)
            gsb = S_("lgsb", [P, 10, 512], BF16); zsb = S_("lzsb", [P, 10, 512])
            t1 = S_("lt1", [P, 512]); t2 = S_("lt2", [P, 512])
            for (c0, n, isctx) in tl:
                dma("sp", xin, xin[:, :, :n], resd, fm(resd, c0, n))
                rmsnorm_mod(xin, n, isctx, g1, sh1, xin, sqb, [hT])
                for o in range(20):
                    if o < 10 and isctx:
                        continue
                    pz = psf.get()
                    for k in range(8):
                        b.op("pe", lambda o=o, k=k: PE.matmul(pz[:, :n], lhsT=win[:, k, o * P:(o + 1) * P], rhs=hT[:, k, :n],
                                                               start=(k == 0), stop=(k == 7)), [win.r, hT.r], [pz.r], inc=(k == 7))
                    if o < 10:
                        gelu_tanh(gsb[:, o, :n], gsb, pz[:, :n], pz, ((t1[:, :n], t1), (t2[:, :n], t2)), None)
                    else:
                        b.op("act", lambda o=o: A.copy(out=zsb[:, o - 10, :n], in_=pz[:, :n]), [pz.r], [zsb.r])
                if not isctx:
                    dma("sp", lru_g, lru_g[:, c0:c0 + n].rearrange("(k p) t -> p k t", p=P), gsb, gsb[:, :, :n])
                dma("sp", lru_zx, lru_zx[:, c0:c0 + n].rearrange("(k p) t -> p k t", p=P), zsb, zsb[:, :, :n])
        with Scope(b) as st:
            def S_(name, shape, dt=F32):
                return T(st.enter_context(nc.sbuf_tensor(name, list(shape), dt)))
            wa = S_("lwa", [P, 2, 10, P]); wx = S_("lwx", [P, 2, 10, P]); cw = S_("lcw", [P, 10, 4]); vec = S_("lvec", [P, 7, 10])
            nsp = S_("lnsp", [P, 2, 10])
            dma("sp", wa, wa[:, :, :, :], lru_wa, lru_wa[:, :, :, :]); dma("sp", wx, wx[:, :, :, :], lru_wx, lru_wx[:, :, :, :])
            dma("sp", cw, cw[:, :, :], lru_cw, lru_cw[:, :, :]); dma("sp", vec, vec[:, :, :], lru_vec, lru_vec[:, :, :])
            b.op("act", lambda: A.activation(out=nsp[:, :, :], in_=vec[:, 5:7, :], func=AF.Exp, scale=-1.0), [vec.r], [nsp.r])
            b.op("act", lambda: A.activation(out=nsp[:, :, :], in_=nsp[:, :, :], func=AF.Ln, bias=onec[:, 0:1], scale=1.0), [nsp.r, onec.r], [nsp.r])
            b.op("dve", lambda: V.tensor_scalar(out=nsp[:, :, :], in0=nsp[:, :, :], scalar1=-8.0, scalar2=None, op0=ALU.mult), [nsp.r], [nsp.r])
            zp = Pool([S_("lzh%d" % i, [P, 516]) for i in range(2)])
            xr = S_("lxr", [P, 512]); rr = S_("lrr", [P, 512]); ii = S_("lii", [P, 512]); aa = S_("laa", [P, 512]); uu = S_("luu", [P, 512])
            hh = S_("lhh", [P, 512]); hfp = Pool([S_("lhf%d" % i, [P, 512]) for i in range(2)])
            gtp = Pool([S_("lgt%d" % i, [P, 512], BF16) for i in range(2)]); gout = S_("lgo", [P, 512], BF16)
            carry = S_("lcar", [P, 1])
            lat_tl = [t for t in tl if not t[2]]
            for ct in range(10):
                for d in range(2):
                    order = tl if d == 0 else ([t for t in tl if t[2]] + lat_tl[::-1])
                    b.op("dve", lambda: V.memset(carry[:, :], 0.0), [], [carry.r])
                    for (c0, n, isctx) in order:
                        lo = 0 if isctx else NCTX
                        hi = NCTX if isctx else NT
                        zh = zp.get()
                        a0 = max(c0 - 2, lo); a1 = min(c0 + n + 1, hi)
                        if a0 > c0 - 2 or a1 < c0 + n + 1:
                            b.op("pool", lambda zh=zh: G.memset(zh[:, :], 0.0), [], [zh.r])
                        dma("sp", zh, zh[:, a0 - (c0 - 2):a1 - (c0 - 2)], lru_zx, lru_zx[ct * P:(ct + 1) * P, a0:a1])
                        b.op("dve", lambda zh=zh: V.tensor_scalar(out=xr[:, :n], in0=zh[:, 0:n], scalar1=cw[:, ct, 0:1], scalar2=vec[:, 0, ct:ct + 1],
                                                                  op0=ALU.mult, op1=ALU.add), [zh.r, cw.r, vec.r], [xr.r])
                        for j in range(1, 4):
                            b.op("dve", lambda zh=zh, j=j: V.scalar_tensor_tensor(out=xr[:, :n], in0=zh[:, j:j + n], scalar=cw[:, ct, j:j + 1], in1=xr[:, :n],
                                                                                  op0=ALU.mult, op1=ALU.add), [zh.r, cw.r, xr.r], [xr.r])
                        pr = psf.get(); pi = psf.get()
                        b.op("pe", lambda: PE.matmul(pr[:, :n], lhsT=wa[:, d, ct, :], rhs=xr[:, :n], start=True, stop=True), [wa.r, xr.r], [pr.r])
                        b.op("pe", lambda: PE.matmul(pi[:, :n], lhsT=wx[:, d, ct, :], rhs=xr[:, :n], start=True, stop=True), [wx.r, xr.r], [pi.r])
                        b.op("act", lambda: A.activation(out=rr[:, :n], in_=pr[:, :n], func=AF.Sigmoid, bias=vec[:, 1 + d, ct:ct + 1], scale=1.0),
                             [pr.r, vec.r], [rr.r])
                        b.op("act", lambda: A.activation(out=ii[:, :n], in_=pi[:, :n], func=AF.Sigmoid, bias=vec[:, 3 + d, ct:ct + 1], scale=1.0),
                             [pi.r, vec.r], [ii.r])
                        b.op("act", lambda: A.activation(out=aa[:, :n], in_=rr[:, :n], func=AF.Exp, scale=nsp[:, d, ct:ct + 1]), [rr.r, nsp.r], [aa.r])
                        b.op("dve", lambda: V.tensor_tensor(out=uu[:, :n], in0=aa[:, :n], in1=aa[:, :n], op=ALU.mult), [aa.r], [uu.r])
                        b.op("act", lambda: A.activation(out=uu[:, :n], in_=uu[:, :n], func=AF.Sqrt, bias=onec[:, 0:1], scale=-1.0), [uu.r, onec.r], [uu.r])
                        b.op("dve", lambda: V.tensor_tensor(out=ii[:, :n], in0=ii[:, :n], in1=xr[:, :n], op=ALU.mult), [ii.r, xr.r], [ii.r])
                        b.op("dve", lambda: V.tensor_tensor(out=uu[:, :n], in0=uu[:, :n], in1=ii[:, :n], op=ALU.mult), [uu.r, ii.r], [uu.r])

                        def r2(t):
                            ap = t[:, 0:n]
                            return AP(ap.tensor, ap.offset + (n - 1), [[ap.ap[0][0], P], [-1, n]])
                        if d == 0:
                            hf = hfp.get()
                            b.op("dve", lambda hf=hf: V.tensor_tensor_scan(out=hf[:, :n], data0=aa[:, :n], data1=uu[:, :n], initial=carry[:, 0:1],
                                                                           op0=ALU.mult, op1=ALU.add), [aa.r, uu.r, carry.r], [hf.r])
                            b.op("dve", lambda hf=hf: V.tensor_copy(out=carry[:, :], in_=hf[:, n - 1:n]), [hf.r], [carry.r])
                            if not isctx:
                                dma("sp", lru_hf, lru_hf[ct * P:(ct + 1) * P, c0:c0 + n], hf, hf[:, :n])
                        else:
                            b.op("dve", lambda: V.tensor_tensor_scan(out=r2(hh), data0=r2(aa), data1=r2(uu), initial=carry[:, 0:1],
                                                                      op0=ALU.mult, op1=ALU.add), [aa.r, uu.r, carry.r], [hh.r])
                            b.op("dve", lambda: V.tensor_copy(out=carry[:, :], in_=hh[:, 0:1]), [hh.r], [carry.r])
                            if not isctx:
                                hf = hfp.get(); gt_ = gtp.get()
                                dma("sp", hf, hf[:, :n], lru_hf, lru_hf[ct * P:(ct + 1) * P, c0:c0 + n])
                                dma("sp", gt_, gt_[:, :n], lru_g, lru_g[ct * P:(ct + 1) * P, c0:c0 + n])
                                b.op("dve", lambda hf=hf: V.tensor_tensor(out=hh[:, :n], in0=hh[:, :n], in1=hf[:, :n], op=ALU.add), [hh.r, hf.r], [hh.r])
                                b.op("dve", lambda gt_=gt_: V.tensor_tensor(out=gout[:, :n], in0=hh[:, :n], in1=gt_[:, :n], op=ALU.mult), [hh.r, gt_.r], [gout.r])
                                dma("sp", lru_gated, lru_gated[ct * P:(ct + 1) * P, c0:c0 + n], gout, gout[:, :n])
        with Scope(b) as st:
            def S_(name, shape, dt=F32):
                return T(st.enter_context(nc.sbuf_tensor(name, list(shape), dt)))
            wout = S_("lwout", [P, 10, D], BF16)
            b.op("pool", lambda: G.dma_start(out=wout[:, :, :], in_=lru_w_out[:, :].rearrange("(k p) n -> p k n", p=P)), [lru_w_out.r], [wout.r], dma=True)
            xp = Pool([S_("lcx%d" % i, [P, 8, 512]) for i in range(2)])
            gp = Pool([S_("lcg%d" % i, [P, 10, 512], BF16) for i in range(2)])
            for (c0, n, isctx) in tiles(False):
                xin = xp.get(); gg = gp.get()
                dma("sp", xin, xin[:, :, :n], resd, fm(resd, c0, n))
                dma("sp", gg, gg[:, :, :n], lru_gated, lru_gated[:, c0:c0 + n].rearrange("(k p) t -> p k t", p=P))
                for o in range(8):
                    py = psf.get()
                    for k in range(10):
                        b.op("pe", lambda o=o, k=k: PE.matmul(py[:, :n], lhsT=wout[:, k, o * P:(o + 1) * P], rhs=gg[:, k, :n],
                                                               start=(k == 0), stop=(k == 9)), [wout.r, gg.r], [py.r], inc=(k == 9))
                    b.op("dve", lambda o=o: V.scalar_tensor_tensor(out=xin[:, o, :n], in0=py[:, :n], scalar=gt1[:, 0, o:o + 1],
                                                                    in1=xin[:, o, :n], op0=ALU.mult, op1=ALU.add), [py.r, gt1.r, xin.r], [xin.r])
                dma("sp", resd, fm(resd, c0, n), xin, xin[:, :, :n])
        return tiles(False)

    def layer3(L):
        layer_scalars(L)
        tl = tiles(False)
        with Scope(b) as st:
            def S_(name, shape, dt=F32):
                return T(st.enter_context(nc.sbuf_tensor(name, list(shape), dt)))
            win = S_("fwin", [P, 8, D], BF16)
            b.op("pool", lambda: G.dma_start(out=win[:, :, :], in_=fn_w_in[:, :].rearrange("(k p) n -> p k n", p=P)), [fn_w_in.r], [win.r], dma=True)
            cs = S_("fcs", [P, 2, 512])
            dma("sp", cs, cs[:, :, :], fn_cs, fn_cs[:, :, :])
            xin = S_("fxin", [P, 8, 512]); sqb = S_("fsqb", [P, 8, 512]); hT = S_("fhT", [P, 8, 512], BF16)
            zs = S_("fzs", [P, 8, 512]); uvp = Pool([S_("fuv%d" % i, [P, 2048]) for i in range(2)])
            for (c0, n, isctx) in tl:
                dma("sp", xin, xin[:, :, :n], resd, fm(resd, c0, n))
                rmsnorm_mod(xin, n, 0, g1, sh1, xin, sqb, [hT])
                for o in range(8):
                    pz = psf.get()
                    for k in range(8):
                        b.op("pe", lambda o=o, k=k: PE.matmul(pz[:, :n], lhsT=win[:, k, o * P:(o + 1) * P], rhs=hT[:, k, :n],
                                                               start=(k == 0), stop=(k == 7)), [win.r, hT.r], [pz.r], inc=(k == 7))
                    b.op("act", lambda o=o: A.copy(out=zs[:, o, :n], in_=pz[:, :n]), [pz.r], [zs.r])
                for ci in range(n // P):
                    uv = uvp.get()
                    for g in range(4):
                        pu = psf.get()
                        for kk in range(2):
                            b.op("pe", lambda g=g, kk=kk, ci=ci: PE.matmul(pu[:, :], lhsT=zs[:, 2 * g + kk, ci * P:(ci + 1) * P], rhs=cs[:, kk, :],
                                                                            start=(kk == 0), stop=(kk == 1)), [zs.r, cs.r], [pu.r], inc=(kk == 1))
                        if g % 2 == 0:
                            b.op("act", lambda g=g, uv=uv: A.copy(out=uv[:, g * 512:(g + 1) * 512], in_=pu[:, :]), [pu.r], [uv.r])
                        else:
                            b.op("dve", lambda g=g, uv=uv: V.tensor_copy(out=uv[:, g * 512:(g + 1) * 512], in_=pu[:, :]), [pu.r], [uv.r])
                    t0 = c0 - NCTX + ci * P
                    dma("sp", fn_uv, fn_uv[t0:t0 + P, :], uv, uv[:, :])
        with Scope(b) as st:
            def S_(name, shape, dt=F32):
                return T(st.enter_context(nc.sbuf_tensor(name, list(shape), dt)))
            dft = S_("fdft", [P, 3, P]); tw = S_("ftw", [P, 2, P])
            dma("sp", dft, dft[:, :, :], fn_dft, fn_dft[:, :, :]); dma("sp", tw, tw[:, :, :], fn_tw, fn_tw[:, :, :])
            inp_ = Pool([S_("fin%d" % i, [P, 512]) for i in range(3)])
            outp = Pool([S_("fout%d" % i, [P, 512]) for i in range(3)])
            tmp = S_("ftmp", [P, 512])
            uv3 = fn_uv[:, :].rearrange("(t1 t2) c -> t1 t2 c", t2=P)
            for t2 in range(P):
                for g in range(4):
                    xi = inp_.get(); bo = outp.get()
                    dma("sp", xi, xi[:, :], fn_uv, uv3[:, t2, g * 512:(g + 1) * 512])
                    pa = psf.get()
                    b.op("pe", lambda: PE.matmul(pa[:, 0:256], lhsT=dft[:, 0, :], rhs=xi[:, 0:256], start=True, stop=False), [dft.r, xi.r], [pa.r], inc=False)
                    b.op("pe", lambda: PE.matmul(pa[:, 0:256], lhsT=dft[:, 2, :], rhs=xi[:, 256:512], start=False, stop=True), [dft.r, xi.r], [pa.r], inc=False)
                    b.op("pe", lambda: PE.matmul(pa[:, 256:512], lhsT=dft[:, 1, :], rhs=xi[:, 0:256], start=True, stop=False), [dft.r, xi.r], [pa.r], inc=False)
                    b.op("pe", lambda: PE.matmul(pa[:, 256:512], lhsT=dft[:, 0, :], rhs=xi[:, 256:512], start=False, stop=True), [dft.r, xi.r], [pa.r])
                    b.op("dve", lambda: V.tensor_scalar(out=tmp[:, 0:256], in0=pa[:, 256:512], scalar1=tw[:, 1, t2:t2 + 1], scalar2=None, op0=ALU.mult),
                         [pa.r, tw.r], [tmp.r])
                    b.op("dve", lambda: V.scalar_tensor_tensor(out=bo[:, 0:256], in0=pa[:, 0:256], scalar=tw[:, 0, t2:t2 + 1], in1=tmp[:, 0:256],
                                                                op0=ALU.mult, op1=ALU.subtract), [pa.r, tw.r, tmp.r], [bo.r])
                    b.op("dve", lambda: V.tensor_scalar(out=tmp[:, 256:512], in0=pa[:, 256:512], scalar1=tw[:, 0, t2:t2 + 1], scalar2=None, op0=ALU.mult),
                         [pa.r, tw.r], [tmp.r])
                    b.op("dve", lambda: V.scalar_tensor_tensor(out=bo[:, 256:512], in0=pa[:, 0:256], scalar=tw[:, 1, t2:t2 + 1], in1=tmp[:, 256:512],
                                                                op0=ALU.mult, op1=ALU.add), [pa.r, tw.r, tmp.r], [bo.r])
                    dma("sp", fn_b, fn_b[t2, :, g * 512:(g + 1) * 512], bo, bo[:, :])
            rp = Pool([S_("frr%d" % i, [P, D]) for i in range(2)])
            r3 = fn_r[:, :].rearrange("(k2 k1) c -> k2 k1 c", k1=P)
            for k1 in range(P):
                ro = rp.get()
                for g in range(4):
                    xi = inp_.get()
                    dma("sp", xi, xi[:, :], fn_b, fn_b[:, k1, g * 512:(g + 1) * 512])
                    pr = psf.get()
                    b.op("pe", lambda: PE.matmul(pr[:, 0:256], lhsT=dft[:, 0, :], rhs=xi[:, 0:256], start=True, stop=False), [dft.r, xi.r], [pr.r], inc=False)
                    b.op("pe", lambda: PE.matmul(pr[:, 0:256], lhsT=dft[:, 2, :], rhs=xi[:, 256:512], start=False, stop=True), [dft.r, xi.r], [pr.r])
                    b.op("act", lambda g=g, ro=ro: A.activation(out=ro[:, g * 256:(g + 1) * 256], in_=pr[:, 0:256], func=AF.Copy, scale=1.0 / 2048.0),
                         [pr.r], [ro.r])
                dma("sp", fn_r, r3[:, k1, :], ro, ro[:, :])
        with Scope(b) as st:
            def S_(name, shape, dt=F32):
                return T(st.enter_context(nc.sbuf_tensor(name, list(shape), dt)))
            wout = S_("fwout", [P, 8, D], BF16)
            b.op("pool", lambda: G.dma_start(out=wout[:, :, :], in_=fn_w_out[:, :].rearrange("(k p) n -> p k n", p=P)), [fn_w_out.r], [wout.r], dma=True)
            xp = Pool([S_("fdx%d" % i, [P, 8, 512]) for i in range(2)])
            rtp = Pool([S_("frt%d" % i, [P, D]) for i in range(2)])
            rT = S_("frT", [P, 8, 512], BF16)
            for (c0, n, isctx) in tl:
                xin = xp.get()
                dma("sp", xin, xin[:, :, :n], resd, fm(resd, c0, n))
                for ci in range(n // P):
                    rt = rtp.get()
                    t0 = c0 - NCTX + ci * P
                    dma("sp", rt, rt[:, :], fn_r, fn_r[t0:t0 + P, :])
                    for hf in range(2):
                        pt = psf.get()
                        for kq in range(4):
                            k = hf * 4 + kq
                            b.op("pe", lambda k=k, kq=kq, rt=rt: PE.transpose(out=pt[:, kq * P:(kq + 1) * P], in_=rt[:, k * P:(k + 1) * P], identity=ident[:, :]),
                                 [rt.r, ident.r], [pt.r], inc=(kq == 3))
                        b.op("act", lambda hf=hf, ci=ci: A.copy(out=rT[:, hf * 4:(hf + 1) * 4, ci * P:(ci + 1) * P],
                                                                 in_=pt[:, :].rearrange("p (k t) -> p k t", k=4)), [pt.r], [rT.r])
                for o in range(8):
                    py = psf.get()
                    for k in range(8):
                        b.op("pe", lambda o=o, k=k: PE.matmul(py[:, :n], lhsT=wout[:, k, o * P:(o + 1) * P], rhs=rT[:, k, :n],
                                                               start=(k == 0), stop=(k == 7)), [wout.r, rT.r], [py.r], inc=(k == 7))
                    b.op("dve", lambda o=o: V.scalar_tensor_tensor(out=xin[:, o, :n], in0=py[:, :n], scalar=gt1[:, 0, o:o + 1],
                                                                    in1=xin[:, o, :n], op0=ALU.mult, op1=ALU.add), [py.r, gt1.r, xin.r], [xin.r])
                dma("sp", resd, fm(resd, c0, n), xin, xin[:, :, :n])
        return tl

    def layer1(L):
        layer_scalars(L)
        tl = tiles(True)
        MLI = 3088
        with Scope(b) as st:
            def S_(name, shape, dt=F32):
                return T(st.enter_context(nc.sbuf_tensor(name, list(shape), dt)))
            win = S_("mwin", [P, 8, MLI], BF16)
            b.op("pool", lambda: G.dma_start(out=win[:, :, :], in_=ml_w_in[:, :].rearrange("(k p) n -> p k n", p=P)), [ml_w_in.r], [win.r], dma=True)
            gb = S_("mgb", [4, 4]); ngb = S_("mngb", [4, 4])
            dma("sp", gb, gb[:, :], ml_gb, ml_gb[:, :])
            b.op("dve", lambda: V.tensor_scalar(out=ngb[:, :], in0=gb[:, :], scalar1=-1.0, scalar2=None, op0=ALU.mult), [gb.r], [ngb.r])
            xin = S_("mxin", [P, 8, 512]); sqb = S_("msqb", [P, 8, 512]); hT = S_("mhT", [P, 8, 512], BF16)
            zq = S_("mzq", [P, 8, 512]); so = S_("mso", [P, 8, 512], BF16)
            vp = Pool([S_("mvv%d" % i, [P, D], BF16) for i in range(2)])
            gr = S_("mgr", [4, 4, 512])
            for (c0, n, isctx) in tl:
                dma("sp", xin, xin[:, :, :n], resd, fm(resd, c0, n))
                rmsnorm_mod(xin, n, isctx, g1, sh1, xin, sqb, [hT])
                for o in range(8):
                    pz = psf.get()
                    for k in range(8):
                        b.op("pe", lambda o=o, k=k: PE.matmul(pz[:, :n], lhsT=win[:, k, o * P:(o + 1) * P], rhs=hT[:, k, :n],
                                                               start=(k == 0), stop=(k == 7)), [win.r, hT.r], [pz.r], inc=(k == 7))
                    b.op("act", lambda o=o: A.copy(out=zq[:, o, :n], in_=pz[:, :n]), [pz.r], [zq.r])
                dma("sp", ml_zqk, fm(ml_zqk, c0, n), zq, zq[:, :, :n])
                for o in range(8):
                    pz = psf.get()
                    for k in range(8):
                        b.op("pe", lambda o=o, k=k: PE.matmul(pz[:, :n], lhsT=win[:, k, 2048 + o * P:2048 + (o + 1) * P], rhs=hT[:, k, :n],
                                                               start=(k == 0), stop=(k == 7)), [win.r, hT.r], [pz.r], inc=(k == 7))
                    b.op("act", lambda o=o: A.activation(out=so[:, o, :n], in_=pz[:, :n], func=AF.Sigmoid), [pz.r], [so.r])
                dma("sp", ml_so, fm(ml_so, c0, n), so, so[:, :, :n])
                for ci in range(n // P):
                    vv = vp.get()
                    for hf in range(2):
                        pv = psf.get()
                        for k in range(8):
                            b.op("pe", lambda k=k, hf=hf, ci=ci: PE.matmul(pv[:, :], lhsT=hT[:, k, ci * P:(ci + 1) * P],
                                                                            rhs=win[:, k, 1024 + hf * 512:1024 + (hf + 1) * 512],
                                                                            start=(k == 0), stop=(k == 7)), [win.r, hT.r], [pv.r], inc=(k == 7))
                        b.op("act", lambda hf=hf, vv=vv: A.copy(out=vv[:, hf * 512:(hf + 1) * 512], in_=pv[:, :]), [pv.r], [vv.r])
                    dma("sp", ml_v, ml_v[c0 + ci * P:c0 + (ci + 1) * P, :], vv, vv[:, :])
                for q in range(4):
                    pgt = psf.get()
                    for k in range(8):
                        b.op("pe", lambda q=q, k=k: PE.matmul(pgt[0:4, :n], lhsT=win[:, k, 3072 + 4 * q:3072 + 4 * q + 4], rhs=hT[:, k, :n],
                                                               start=(k == 0), stop=(k == 7)), [win.r, hT.r], [pgt.r], inc=(k == 7))
                    if q % 2 == 0:
                        b.op("act", lambda q=q: A.activation(out=gr[:, q, :n], in_=pgt[0:4, :n], func=AF.Identity, bias=gb[:, q:q + 1], scale=1.0),
                             [pgt.r, gb.r], [gr.r])
                    else:
                        b.op("act", lambda q=q: A.activation(out=gr[:, q, :n], in_=pgt[0:4, :n], func=AF.Exp, bias=ngb[:, q:q + 1], scale=-1.0),
                             [pgt.r, ngb.r], [gr.r])
                        b.op("act", lambda q=q: A.activation(out=gr[:, q, :n], in_=gr[:, q, :n], func=AF.Ln, bias=onec[0:4, 0:1], scale=1.0),
                             [gr.r, onec.r], [gr.r])
                        b.op("dve", lambda q=q: V.tensor_scalar(out=gr[:, q, :n], in0=gr[:, q, :n], scalar1=-1.0, scalar2=None, op0=ALU.mult), [gr.r], [gr.r])
                dma("sp", ml_g, ml_g[:, :, c0:c0 + n], gr, gr[:, :, :n])
        with Scope(b) as st:
            def S_(name, shape, dt=F32):
                return T(st.enter_context(nc.sbuf_tensor(name, list(shape), dt)))
            cw = S_("mcw", [P, 8, 4]); cb = S_("mcb", [P, 8])
            dma("sp", cw, cw[:, :, :], ml_cw, ml_cw[:, :, :]); dma("sp", cb, cb[:, :], ml_cb, ml_cb[:, :])
            zp = Pool([S_("mzh%d" % i, [P, 516]) for i in range(2)])
            xr = S_("mxr", [P, 512]); qo = Pool([S_("mqo%d" % i, [P, 512], BF16) for i in range(2)])
            for ft in range(8):
                for (c0, n, isctx) in tl:
                    lo = 0 if isctx else NCTX
                    hi = NCTX if isctx else NT
                    zh = zp.get()
                    a0 = max(c0 - 2, lo); a1 = min(c0 + n + 1, hi)
                    if a0 > c0 - 2 or a1 < c0 + n + 1:
                        b.op("pool", lambda zh=zh: G.memset(zh[:, :], 0.0), [], [zh.r])
                    dma("sp", zh, zh[:, a0 - (c0 - 2):a1 - (c0 - 2)], ml_zqk, ml_zqk[ft * P:(ft + 1) * P, a0:a1])
                    b.op("dve", lambda zh=zh: V.tensor_scalar(out=xr[:, :n], in0=zh[:, 0:n], scalar1=cw[:, ft, 0:1], scalar2=cb[:, ft:ft + 1],
                                                              op0=ALU.mult, op1=ALU.add), [zh.r, cw.r, cb.r], [xr.r])
                    for j in range(1, 4):
                        b.op("dve", lambda zh=zh, j=j: V.scalar_tensor_tensor(out=xr[:, :n], in0=zh[:, j:j + n], scalar=cw[:, ft, j:j + 1], in1=xr[:, :n],
                                                                              op0=ALU.mult, op1=ALU.add), [zh.r, cw.r, xr.r], [xr.r])
                    qq = qo.get()
                    b.op("act", lambda: A.activation(out=xr[:, :n], in_=xr[:, :n], func=AF.Silu), [xr.r], [xr.r])
                    sc_ = (128.0 ** -0.5) if ft < 4 else 1.0
                    b.op("dve", lambda qq=qq: V.tensor_scalar(out=qq[:, :n], in0=xr[:, :n], scalar1=sc_, scalar2=None, op0=ALU.mult), [xr.r], [qq.r])
                    dst = ml_q if ft < 4 else ml_k
                    f4 = ft % 4
                    dma("sp", dst, dst[f4 * P:(f4 + 1) * P, c0:c0 + n], qq, qq[:, :n])
        with Scope(b) as st:
            def S_(name, shape, dt=F32):
                return T(st.enter_context(nc.sbuf_tensor(name, list(shape), dt)))
            msk = S_("mmsk", [P, 2, P]); sel4 = S_("msel4", [4, 512]); ng = S_("mng", [P, 8])
            dma("sp", msk, msk[:, :, :], ml_mask, ml_mask[:, :, :]); dma("sp", sel4, sel4[:, :], sel_d, sel_d[0:4, 0:512])
            dma("sp", ng, ng[:, :], ml_ng, ml_ng[:, :])
            Cst = S_("mC", [P, 4, 256]); Cb = S_("mCb", [P, 4, 256], BF16); nst = S_("mn", [P, 4, P]); nb = S_("mnb", [P, 4, P], BF16)
            qTp = Pool([S_("mqT%d" % i, [P, 4, P], BF16) for i in range(2)]); kTp = Pool([S_("mkT%d" % i, [P, 4, P], BF16) for i in range(2)])
            vcp = Pool([S_("mvc%d" % i, [P, D], BF16) for i in range(2)]); grp_ = Pool([S_("mgr%d" % i, [4, 2, P]) for i in range(2)])
            rb = S_("mrb", [4, P]); rc = S_("mrc", [4, P]); rk = S_("mrk", [4, P]); Rb = S_("mRb", [4, 257]); cols = S_("mcols", [P, 8])
            bc = S_("mbc", [P, 257]); Dm = S_("mDm", [P, P]); St = S_("mSt", [P, P], BF16); t1 = S_("mt1", [P, P]); num = S_("mnum", [P, 2, P])
            den = S_("mden", [P, P]); kw = S_("mkw", [P, P], BF16)
            hop = Pool([S_("mho%d" % i, [P, 8, P]) for i in range(2)]); hfp = Pool([S_("mhf%d" % i, [P, 8, P]) for i in range(2)])
            sop = Pool([S_("msoc%d" % i, [P, 8, P], BF16) for i in range(2)]); gop = Pool([S_("mgo%d" % i, [P, 8, P], BF16) for i in range(2)])
            sq2 = S_("msq2", [P, 2, P]); rstd = S_("mrstd", [P, P])
            nchk = NT // P
            for d in range(2):
                order = list(range(nchk)) if d == 0 else ([1, 0] + list(range(nchk - 1, 1, -1)))
                b.op("dve", lambda: V.memset(Cst[:, :, :], 0.0), [], [Cst.r]); b.op("dve", lambda: V.memset(Cb[:, :, :], 0.0), [], [Cb.r])
                b.op("dve", lambda: V.memset(nst[:, :, :], 0.0), [], [nst.r]); b.op("dve", lambda: V.memset(nb[:, :, :], 0.0), [], [nb.r])

                def dv(t):
                    ap = t[:, 0:P]
                    if d == 0:
                        return ap
                    return AP(ap.tensor, ap.offset + (P - 1), [[ap.ap[0][0], 4], [-1, P]])
                for j in order:
                    c0 = j * P
                    isctx = 1 if j < 2 else 0
                    qT = qTp.get(); kT = kTp.get(); vc = vcp.get(); gq = grp_.get()
                    dma("sp", qT, qT[:, :, :], ml_q, ml_q[:, c0:c0 + P].rearrange("(h p) t -> p h t", p=P))
                    dma("sp", kT, kT[:, :, :], ml_k, ml_k[:, c0:c0 + P].rearrange("(h p) t -> p h t", p=P))
                    dma("sp", vc, vc[:, :], ml_v, ml_v[c0:c0 + P, :])
                    dma("sp", gq, gq[:, :, :], ml_g, ml_g[:, 2 * d:2 * d + 2, c0:c0 + P])
                    gidx = P - 1 if d == 0 else 0
                    lfap = gq[:, 1, :]
                    if d == 1:
                        lfap = AP(lfap.tensor, lfap.offset + (P - 1), [[lfap.ap[0][0], 4], [-1, P]])
                    b.op("dve", lambda lfap=lfap: V.tensor_tensor_scan(out=dv(rb), data0=ones[0:4, 0:P], data1=lfap,
                                                                       initial=0.0, op0=ALU.mult, op1=ALU.add), [ones.r, gq.r], [rb.r])
                    b.op("dve", lambda gq=gq: V.tensor_tensor(out=rc[:, :], in0=gq[:, 0, :], in1=rb[:, :], op=ALU.subtract), [gq.r, rb.r], [rc.r])
                    b.op("act", lambda: A.activation(out=rk[:, :], in_=rc[:, :], func=AF.Exp, bias=rb[:, gidx:gidx + 1], scale=1.0), [rc.r, rb.r], [rk.r])
                    b.op("dve", lambda: V.tensor_copy(out=Rb[:, 0:P], in_=rb[:, :]), [rb.r], [Rb.r])
                    b.op("act", lambda: A.activation(out=Rb[:, P:2 * P], in_=rb[:, :], func=AF.Exp), [rb.r], [Rb.r])
                    b.op("act", lambda: A.activation(out=Rb[:, 256:257], in_=rb[:, gidx:gidx + 1], func=AF.Exp), [rb.r], [Rb.r])
                    pT = psf.get()
                    b.op("pe", lambda: PE.transpose(out=pT[:, 0:4], in_=rc[:, :], identity=ident[0:4, 0:4]), [rc.r, ident.r], [pT.r], inc=False)
                    b.op("pe", lambda: PE.transpose(out=pT[:, 4:8], in_=rk[:, :], identity=ident[0:4, 0:4]), [rk.r, ident.r], [pT.r])
                    b.op("dve", lambda: V.tensor_copy(out=cols[:, :], in_=pT[:, 0:8]), [pT.r], [cols.r])
                    ho = hop.get()
                    for h in range(4):
                        pbc = psf.get()
                        b.op("pe", lambda h=h: PE.matmul(pbc[:, 0:257], lhsT=sel4[:, h * P:(h + 1) * P], rhs=Rb[:, :], start=True, stop=True),
                             [sel4.r, Rb.r], [pbc.r])
                        b.op("act", lambda: A.copy(out=bc[:, :], in_=pbc[:, 0:257]), [pbc.r], [bc.r])
                        pst = psf.get()
                        b.op("pe", lambda h=h: PE.matmul(pst[:, 0:P], lhsT=kT[:, h, :], rhs=qT[:, h, :], start=True, stop=True), [kT.r, qT.r], [pst.r])
                        b.op("act", lambda h=h: A.activation(out=Dm[:, :], in_=bc[:, 0:P], func=AF.Exp, bias=cols[:, h:h + 1], scale=1.0),
                             [bc.r, cols.r], [Dm.r])
                        b.op("dve", lambda: V.tensor_tensor(out=Dm[:, :], in0=Dm[:, :], in1=msk[:, d, :], op=ALU.mult), [Dm.r, msk.r], [Dm.r])
                        b.op("dve", lambda: V.tensor_tensor(out=St[:, :], in0=Dm[:, :], in1=pst[:, 0:P], op=ALU.mult), [Dm.r, pst.r], [St.r])
                        for vt in range(2):
                            pn = psf.get()
                            b.op("pe", lambda h=h, vt=vt: PE.matmul(pn[:, 0:P], lhsT=vc[:, h * 256 + vt * P:h * 256 + (vt + 1) * P], rhs=St[:, :],
                                                                     start=True, stop=True), [vc.r, St.r], [pn.r], inc=False)
                            b.op("pe", lambda h=h, vt=vt: PE.matmul(pn[:, P:2 * P], lhsT=Cb[:, h, vt * P:(vt + 1) * P], rhs=qT[:, h, :],
                                                                     start=True, stop=True), [Cb.r, qT.r], [pn.r])
                            b.op("dve", lambda: V.tensor_tensor(out=t1[:, :], in0=pn[:, P:2 * P], in1=bc[:, P:2 * P], op=ALU.mult), [pn.r, bc.r], [t1.r])
                            b.op("dve", lambda vt=vt: V.tensor_tensor(out=num[:, vt, :], in0=t1[:, :], in1=pn[:, 0:P], op=ALU.add), [t1.r, pn.r], [num.r])
                        pd = psf.get()
                        b.op("pe", lambda: PE.matmul(pd[:, 0:P], lhsT=onesb[:, :], rhs=St[:, :], start=True, stop=True), [onesb.r, St.r], [pd.r], inc=False)
                        b.op("pe", lambda h=h: PE.matmul(pd[:, P:2 * P], lhsT=nb[:, h, :], rhs=qT[:, h, :], start=True, stop=True), [nb.r, qT.r], [pd.r])
                        b.op("dve", lambda: V.tensor_tensor(out=den[:, :], in0=pd[:, P:2 * P], in1=bc[:, P:2 * P], op=ALU.mult), [pd.r, bc.r], [den.r])
                        b.op("dve", lambda: V.tensor_tensor(out=den[:, :], in0=den[:, :], in1=pd[:, 0:P], op=ALU.add), [den.r, pd.r], [den.r])
                        b.op("dve", lambda: V.tensor_scalar(out=t1[:, :], in0=den[:, :], scalar1=-1.0, scalar2=None, op0=ALU.mult), [den.r], [t1.r])
                        b.op("dve", lambda: V.tensor_tensor(out=den[:, :], in0=den[:, :], in1=t1[:, :], op=ALU.max), [den.r, t1.r], [den.r])
                        b.op("dve", lambda: V.tensor_scalar(out=den[:, :], in0=den[:, :], scalar1=1.0, scalar2=None, op0=ALU.max), [den.r], [den.r])
                        b.op("dve", lambda: V.reciprocal(out=den[:, :], in_=den[:, :]), [den.r], [den.r])
                        for vt in range(2):
                            b.op("dve", lambda h=h, vt=vt: V.tensor_tensor(out=ho[:, 2 * h + vt, :], in0=num[:, vt, :], in1=den[:, :], op=ALU.mult),
                                 [num.r, den.r], [ho.r])
                        ptk = psb.get()
                        b.op("pe", lambda h=h: PE.transpose(out=ptk[:, 0:P], in_=kT[:, h, :], identity=identb[:, :]), [kT.r, identb.r], [ptk.r])
                        b.op("dve", lambda h=h: V.tensor_scalar(out=kw[:, :], in0=ptk[:, 0:P], scalar1=cols[:, 4 + h:5 + h], scalar2=None, op0=ALU.mult),
                             [ptk.r, cols.r], [kw.r])
                        pC = psf.get()
                        b.op("pe", lambda h=h: PE.matmul(pC[:, 0:256], lhsT=kw[:, :], rhs=vc[:, h * 256:(h + 1) * 256], start=True, stop=True),
                             [kw.r, vc.r], [pC.r], inc=False)
                        b.op("pe", lambda: PE.matmul(pC[:, 256:384], lhsT=kw[:, :], rhs=onesb[:, :], start=True, stop=True), [kw.r, onesb.r], [pC.r])
                        b.op("dve", lambda h=h: V.scalar_tensor_tensor(out=Cst[:, h, :], in0=Cst[:, h, :], scalar=bc[:, 256:257], in1=pC[:, 0:256],
                                                                        op0=ALU.mult, op1=ALU.add), [Cst.r, bc.r, pC.r], [Cst.r])
                        b.op("dve", lambda h=h: V.scalar_tensor_tensor(out=nst[:, h, :], in0=nst[:, h, :], scalar=bc[:, 256:257], in1=pC[:, 256:384],
                                                                        op0=ALU.mult, op1=ALU.add), [nst.r, bc.r, pC.r], [nst.r])
                        b.op("act", lambda h=h: A.copy(out=Cb[:, h, :], in_=Cst[:, h, :]), [Cst.r], [Cb.r])
                        b.op("act", lambda h=h: A.copy(out=nb[:, h, :], in_=nst[:, h, :]), [nst.r], [nb.r])
                    if d == 0:
                        dma("sp", ml_hf, ml_hf[:, c0:c0 + P].rearrange("(k p) t -> p k t", p=P), ho, ho[:, :, :])
                    else:
                        hf = hfp.get(); soc = sop.get(); go = gop.get()
                        dma("sp", hf, hf[:, :, :], ml_hf, ml_hf[:, c0:c0 + P].rearrange("(k p) t -> p k t", p=P))
                        dma("sp", soc, soc[:, :, :], ml_so, ml_so[:, c0:c0 + P].rearrange("(k p) t -> p k t", p=P))
                        b.op("dve", lambda: V.tensor_tensor(out=ho[:, :, :], in0=ho[:, :, :], in1=hf[:, :, :], op=ALU.add), [ho.r, hf.r], [ho.r])
                        for h in range(4):
                            b.op("act", lambda h=h: A.activation(out=sq2[:, :, :], in_=ho[:, 2 * h:2 * h + 2, :], func=AF.Square), [ho.r], [sq2.r])
                            pss = psf.get()
                            for vt in range(2):
                                b.op("pe", lambda vt=vt: PE.matmul(pss[:, 0:P], lhsT=ones[:, :], rhs=sq2[:, vt, :], start=(vt == 0), stop=(vt == 1)),
                                     [ones.r, sq2.r], [pss.r], inc=(vt == 1))
                            b.op("act", lambda: A.activation(out=rstd[:, :], in_=pss[:, 0:P], func=AF.Sqrt, bias=epsc[:, 0:1], scale=1.0 / 256.0),
                                 [pss.r, epsc.r], [rstd.r])
                            b.op("dve", lambda: V.reciprocal(out=rstd[:, :], in_=rstd[:, :]), [rstd.r], [rstd.r])
                            for vt in range(2):
                                kk = 2 * h + vt
                                b.op("dve", lambda kk=kk: V.scalar_tensor_tensor(out=ho[:, kk, :], in0=ho[:, kk, :], scalar=ng[:, kk:kk + 1], in1=rstd[:, :],
                                                                                 op0=ALU.mult, op1=ALU.mult), [ho.r, ng.r, rstd.r], [ho.r])
                        b.op("dve", lambda: V.tensor_tensor(out=go[:, :, :], in0=ho[:, :, :], in1=soc[:, :, :], op=ALU.mult), [ho.r, soc.r], [go.r])
                        dma("sp", ml_gated, ml_gated[:, c0:c0 + P].rearrange("(k p) t -> p k t", p=P), go, go[:, :, :])
        with Scope(b) as st:
            def S_(name, shape, dt=F32):
                return T(st.enter_context(nc.sbuf_tensor(name, list(shape), dt)))
            wout = S_("mwout", [P, 8, D], BF16)
            b.op("pool", lambda: G.dma_start(out=wout[:, :, :], in_=ml_w_out[:, :].rearrange("(k p) n -> p k n", p=P)), [ml_w_out.r], [wout.r], dma=True)
            xp = Pool([S_("mdx%d" % i, [P, 8, 512]) for i in range(2)])
            gp = Pool([S_("mdg%d" % i, [P, 8, 512], BF16) for i in range(2)])
            for (c0, n, isctx) in tl:
                xin = xp.get(); gg = gp.get()
                dma("sp", xin, xin[:, :, :n], resd, fm(resd, c0, n))
                dma("sp", gg, gg[:, :, :n], ml_gated, fm(ml_gated, c0, n))
                for o in range(8):
                    py = psf.get()
                    for k in range(8):
                        b.op("pe", lambda o=o, k=k: PE.matmul(py[:, :n], lhsT=wout[:, k, o * P:(o + 1) * P], rhs=gg[:, k, :n],
                                                               start=(k == 0), stop=(k == 7)), [wout.r, gg.r], [py.r], inc=(k == 7))
                    b.op("dve", lambda o=o: V.scalar_tensor_tensor(out=xin[:, o, :n], in0=py[:, :n], scalar=gt1[:, isctx, o:o + 1],
                                                                    in1=xin[:, o, :n], op0=ALU.mult, op1=ALU.add), [py.r, gt1.r, xin.r], [xin.r])
                dma("sp", resd, fm(resd, c0, n), xin, xin[:, :, :n])
        return tl

    def layer0(L):
        layer_scalars(L)
        tl = tiles(True)
        with Scope(b) as st:
            def S_(name, shape, dt=F32):
                return T(st.enter_context(nc.sbuf_tensor(name, list(shape), dt)))
            moe_begin(m, L, sum(t[1] for t in tl))
            win = S_("cmwin", [P, 8, 2 * D], BF16)
            wout = S_("cmwout", [P, 8, D], BF16)
            wsT = S_("cmwsT", [P, 4, P], BF16)
            vg = S_("cmvg", [P, D]); bs = S_("cmbs", [P, 4, 512])
            b.op("pool", lambda: G.dma_start(out=win[:, :, :], in_=cm_w_in[:, :].rearrange("(k p) n -> p k n", p=P)), [cm_w_in.r], [win.r], dma=True)
            b.op("pool", lambda: G.dma_start(out=wout[:, :, :], in_=cm_w_out[:, :].rearrange("(k p) n -> p k n", p=P)), [cm_w_out.r], [wout.r], dma=True)
            b.op("pool", lambda: G.dma_start(out=wsT[:, :, :], in_=cm_wsT[:, :, :]), [cm_wsT.r], [wsT.r], dma=True)
            dma("sp", vg, vg[:, :], cm_vg_rep, cm_vg_rep[:, :])
            dma("sp", bs, bs[:, :, :], cm_bs_rep, cm_bs_rep[:, :, :])
            xp = Pool([S_("xin%d" % i, [P, 8, 512]) for i in range(2)])
            sqb = S_("sqb", [P, 8, 512]); tb = S_("tb", [P, 8, 512])
            hT = S_("hT", [P, 8, 512], BF16)
            u = S_("u", [P, 8, 512], BF16)
            vt = S_("vt", [P, 1024]); vn = S_("vn", [P, 4, 1024], BF16)
            gated = S_("gated", [P, 8, 512], BF16)
            h2b = S_("h2b", [P, 8, 512], BF16)
            rowsp = Pool([S_("rows%d" % i, [P, D], BF16) for i in range(2)])
            t1 = S_("gsc1", [P, 512]); t2 = S_("gsc2", [P, 512])
            vss = S_("vss", [P, 2]); junk = S_("junk", [P, 1024])
            for (c0, n, isctx) in tl:
                xin = xp.get()
                dma("sp", xin, xin[:, :, :n], resd, fm(resd, c0, n))
                rmsnorm_mod(xin, n, isctx, g1, sh1, tb, sqb, [hT])
                for o in range(8):
                    pu = psf.get()
                    for k in range(8):
                        b.op("pe", lambda o=o, k=k: PE.matmul(pu[:, :n], lhsT=win[:, k, o * P:(o + 1) * P], rhs=hT[:, k, :n],
                                                               start=(k == 0), stop=(k == 7)), [win.r, hT.r], [pu.r], inc=(k == 7))
                    gelu_tanh(u[:, o, :n], u, pu[:, :n], pu, ((t1[:, :n], t1), (t2[:, :n], t2)), None)
                for ci in range(n // P):
                    for hf in range(2):
                        pv = psf.get()
                        for k in range(8):
                            b.op("pe", lambda k=k, hf=hf, ci=ci: PE.matmul(pv[:, :], lhsT=hT[:, k, ci * P:(ci + 1) * P],
                                                                            rhs=win[:, k, D + hf * 512:D + (hf + 1) * 512],
                                                                            start=(k == 0), stop=(k == 7)), [win.r, hT.r], [pv.r], inc=(k == 7))
                        gelu_tanh(vt[:, hf * 512:(hf + 1) * 512], vt, pv[:, :], pv, ((t1[:, :], t1), (t2[:, :], t2)), None)
                    b.op("act", lambda: A.activation(out=junk[:, :], in_=vt[:, :], func=AF.Square), [vt.r], [junk.r])
                    b.op("dve", lambda: V.reduce_sum(out=vss[:, 0:1], in_=junk[:, :], axis=AX.X), [junk.r], [vss.r])
                    b.op("act", lambda: A.activation(out=vss[:, 1:2], in_=vss[:, 0:1], func=AF.Sqrt, bias=epsc[:, 0:1], scale=1.0 / D),
                         [vss.r, epsc.r], [vss.r])
                    b.op("dve", lambda: V.reciprocal(out=vss[:, 1:2], in_=vss[:, 1:2]), [vss.r], [vss.r])
                    b.op("dve", lambda ci=ci: V.scalar_tensor_tensor(out=vn[:, ci, :], in0=vt[:, :], scalar=vss[:, 1:2], in1=vg[:, :],
                                                                      op0=ALU.mult, op1=ALU.mult), [vt.r, vss.r, vg.r], [vn.r], ss=True)
                for c in range(8):
                    psm = psf.get()
                    for ci in range(n // P):
                        b.op("pe", lambda c=c, ci=ci: PE.matmul(psm[:, ci * P:(ci + 1) * P], lhsT=vn[:, ci, c * P:(c + 1) * P], rhs=wsT[:, c // 2, :],
                                                                 start=True, stop=True), [vn.r, wsT.r], [psm.r], inc=(ci == n // P - 1))
                    b.op("dve", lambda c=c: V.tensor_tensor(out=t1[:, :n], in0=psm[:, :n], in1=bs[:, c // 2, :n], op=ALU.add), [psm.r, bs.r], [t1.r])
                    b.op("dve", lambda c=c: V.tensor_tensor(out=gated[:, c, :n], in0=t1[:, :n], in1=u[:, c, :n], op=ALU.mult), [t1.r, u.r], [gated.r])
                for o in range(8):
                    py = psf.get()
                    for k in range(8):
                        b.op("pe", lambda o=o, k=k: PE.matmul(py[:, :n], lhsT=wout[:, k, o * P:(o + 1) * P], rhs=gated[:, k, :n],
                                                               start=(k == 0), stop=(k == 7)), [wout.r, gated.r], [py.r], inc=(k == 7))
                    b.op("dve", lambda o=o: V.scalar_tensor_tensor(out=xin[:, o, :n], in0=py[:, :n], scalar=gt1[:, isctx, o:o + 1],
                                                                    in1=xin[:, o, :n], op0=ALU.mult, op1=ALU.add), [py.r, gt1.r, xin.r], [xin.r])
                dma("sp", resd, fm(resd, c0, n), xin, xin[:, :, :n])
                continue
                rmsnorm_mod(xin, n, isctx, g2, sh2, tb, sqb, [sqb, h2b])
                for ci in range(n // P):
                    ch = c0 // P + ci
                    moe_route_chunk(m, sqb, ci * P, ch)
                    moe_rows_chunk(m, h2b, ci * P, c0 + ci * P, rowsp.get())
            if dump_res:
                dbg.extend([("modall", modall, modall[:, :, :, :], [P, DEPTH, 48, 2], F32), ("g1", g1, g1[:, :, :], [P, 2, 8], F32),
                            ("hT", hT, hT[:, :, :], [P, 8, 512], BF16), ("u", u, u[:, :, :], [P, 8, 512], BF16),
                            ("vn", vn, vn[:, :, :], [P, 4, 1024], BF16), ("gated", gated, gated[:, :, :], [P, 8, 512], BF16),
                            ("tb", tb, tb[:, :, :], [P, 8, 512], F32), ("sqb", sqb, sqb[:, :, :], [P, 8, 512], F32),
                            ("sc", sc, sc[:, :, :], [P, 8, 2], F32),
                            ("vt", vt, vt[:, :], [P, 1024], F32), ("junk", junk, junk[:, :], [P, 1024], F32), ("vss", vss, vss[:, :], [P, 2], F32),
                            ("vg", vg, vg[:, :], [P, 1024], F32)])
                emit_dbg()
        return tl

    for L in layers:
        if L == 0:
            tl = layer0(L)
            if stage >= 5:
                moe_dense(L, tl)
        elif L == 1:
            tl = layer1(L)
            if stage >= 5:
                moe_dense(L, tl)
        elif L == 3:
            tl = layer3(L)
            if stage >= 5:
                moe_dense(L, tl)
        elif L == 2:
            tl = layer2(L)
            if stage >= 5:
                moe_dense(L, tl)
        else:
            raise NotImplementedError("mixer for layer %d not implemented yet" % L)

    with Scope(b) as st:
        fa = Pool([T(st.enter_context(nc.sbuf_tensor("fa%d" % i, [P, 8, 512], F32))) for i in range(2)])
        if out_res and not dump_res:
            for c0 in range(0, NT, 512):
                n = min(512, NT - c0)
                a = fa.get()
                dma("sp", a, a[:, :, :n], resd, fm(resd, c0, n))
                dma("sp", res_out, fm(res_out, c0, n), a, a[:, :, :n])
        if dump_res:
            dma("sp", info_out, info_out[:, :, :], m.info, m.info[:, :, :])
            dma("sp", dest_out, dest_out[:, :, :], m.dest, m.dest[:, :, :])
            dma("sp", carry_out, carry_out[:, :], m.carry, m.carry[:, :])
            for c0 in range(0, NT, 512):
                n = min(512, NT - c0)
                a = fa.get()
                dma("sp", a, a[:, :, :n], resd, fm(resd, c0, n))
                dma("sp", res_out, fm(res_out, c0, n), a, a[:, :, :n])
        if final:
            fsq = T(st.enter_context(nc.sbuf_tensor("fsq", [P, 8, 512], F32)))
            ftb = T(st.enter_context(nc.sbuf_tensor("ftb", [P, 8, 512], F32)))
            fsc = T(st.enter_context(nc.sbuf_tensor("fsc", [P, 2, 8], F32)))
            fzero = T(st.enter_context(nc.sbuf_tensor("fzero", [P, 2, 8], F32)))
            b.op("dve", lambda: V.memset(fzero[:, :, :], 0.0), [], [fzero.r])
            b.op("dve", lambda: V.tensor_copy(out=fsc[:, 0, :], in_=nfin[:, :]), [nfin.r], [fsc.r])
            for c0 in range(0, SEQ, 512):
                a = fa.get()
                dma("sp", a, a[:, :, :], resd, fm(resd, NCTX + c0, 512))
                rmsnorm_mod(a, 512, 0, fsc, fzero, ftb, fsq, [fsq])
                dma("sp", outT, fm(outT, c0, 512), fsq, fsq[:, :, :])
    b.finish("sp")
    return nc, b


def _pos_embedding_T():
    rows = SEQ // 64
    r = np.repeat(np.arange(rows, dtype=np.float32), 64)
    col = np.tile(np.arange(64, dtype=np.float32), rows)
    q = D // 4
    freq = np.exp(np.float32(-math.log(10000.0)) * np.arange(q, dtype=np.float32) / np.float32(q)).astype(np.float32)
    ar = r[:, None] * freq
    ac = col[:, None] * freq
    pe = np.concatenate([np.sin(ar), np.cos(ar), np.sin(ac), np.cos(ac)], axis=-1).astype(np.float32)
    return np.ascontiguousarray(pe.T)


def _fp(v, n):
    return np.ascontiguousarray(np.asarray(v, np.float32).reshape(n, P).T)


def host_prep(inp):
    f = lambda a: np.ascontiguousarray(np.asarray(a, np.float32))
    m = {}
    m["xT"] = f(inp["x"][0].T)
    m["posT"] = _pos_embedding_T()
    m["ctxT"] = f(inp["ctx"][0].T)
    m["cT"] = f(np.stack([_fp(inp["c"][0], 8), _fp(inp["c_ctx"], 8)], axis=-1))
    for l in range(DEPTH):
        m["ada_w%d" % l] = f(inp["ada_w"][l])
    m["ada_bT"] = f(np.stack([_fp(inp["ada_b"][l], 48) for l in range(DEPTH)]))
    m["nmixT"] = f(np.stack([_fp(inp["norm_mix_g"][l], 8) for l in range(DEPTH)]))
    m["nffnT"] = f(np.stack([_fp(inp["norm_ffn_g"][l], 8) for l in range(DEPTH)]))
    m["nfinT"] = _fp(inp["final_norm_g"], 8)
    m["rw"] = f(np.concatenate([inp["router_group_w"], inp["router_expert_w"]], axis=-1))
    rb = np.concatenate([inp["router_group_b"], inp["router_expert_b"]], axis=-1)
    m["rbrep"] = f(np.broadcast_to(rb[:, None, :], (DEPTH, P, 36)))
    for l in range(DEPTH):
        m["wg%d" % l] = f(inp["expert_w_gate"][l]).reshape(NEXP * D, HID)
        m["wu%d" % l] = f(inp["expert_w_up"][l]).reshape(NEXP * D, HID)
        m["wd%d" % l] = f(inp["expert_w_down"][l]).reshape(NEXP * HID, D)
    m["cm_w_in"] = f(inp["cm_w_in"][0])
    m["cm_vg_rep"] = f(np.broadcast_to(inp["cm_v_norm_g"][0][None, :], (P, D)))
    m["cm_wsT"] = f(np.transpose(inp["cm_w_s"][0], (2, 0, 1)))
    m["cm_bs_rep"] = f(np.broadcast_to(np.tile(inp["cm_b_s"][0], (1, 4))[None], (P, 4, 512)))
    m["cm_w_out"] = f(inp["cm_w_out"][0])
    m["lru_w_in"] = f(inp["lru_w_in"][0]); m["lru_w_out"] = f(inp["lru_w_out"][0])
    m["lru_wa"] = f(np.transpose(inp["lru_w_a"][0], (2, 0, 1, 3)))
    m["lru_wx"] = f(np.transpose(inp["lru_w_x"][0], (2, 0, 1, 3)))
    m["lru_cw"] = f(inp["lru_conv_w"][0].T.reshape(10, P, 4).transpose(1, 0, 2))
    vecs = [inp["lru_conv_b"][0], inp["lru_b_a"][0][0], inp["lru_b_a"][0][1], inp["lru_b_x"][0][0], inp["lru_b_x"][0][1],
            inp["lru_lambda"][0][0], inp["lru_lambda"][0][1]]
    m["lru_vec"] = f(np.stack([_fp(v, 10) for v in vecs], axis=1))
    m["fn_w_in"] = f(inp["fn_w_in"][0]); m["fn_w_out"] = f(inp["fn_w_out"][0])
    cc = np.arange(256, dtype=np.float64)
    ph = 2 * np.pi * np.outer(cc, cc) / 256.0
    csm = np.concatenate([np.cos(ph), np.sin(ph)], axis=1).astype(np.float32)
    m["fn_cs"] = f(csm.reshape(2, P, 512).transpose(1, 0, 2))
    kk = np.arange(P, dtype=np.float64)
    p128 = 2 * np.pi * np.outer(kk, kk) / 128.0
    m["fn_dft"] = f(np.stack([np.cos(p128), np.sin(p128), -np.sin(p128)], axis=1))
    ptw = 2 * np.pi * np.outer(kk, kk) / float(SEQ)
    m["fn_tw"] = f(np.stack([np.cos(ptw), np.sin(ptw)], axis=1))
    m["ml_w_in"] = f(inp["ml_w_in"][0]); m["ml_w_out"] = f(inp["ml_w_out"][0])
    m["ml_gb"] = f(inp["ml_gate_b"][0].reshape(4, 4).T)
    m["ml_cw"] = f(inp["ml_conv_w"][0].T.reshape(8, P, 4).transpose(1, 0, 2))
    m["ml_cb"] = _fp(inp["ml_conv_b"][0], 8)
    m["ml_mask"] = f(np.stack([np.triu(np.ones((P, P), np.float32)), np.tril(np.ones((P, P), np.float32))], axis=1))
    m["ml_ng"] = _fp(inp["ml_norm_g"][0], 8)
    m["ident"] = np.eye(P, dtype=np.float32)
    m["ustrict"] = np.triu(np.ones((P, P), np.float32), 1)
    m["iota32"] = f(np.broadcast_to(np.arange(32, dtype=np.float32)[None], (P, 32)))
    nbmax = (2 * NT + NEXP * 127 + 127) // 128
    m["blkpos"] = f(np.broadcast_to((np.arange(nbmax, dtype=np.float32) * 128)[None], (P, nbmax)))
    m["piota"] = np.arange(P, dtype=np.float32)[:, None].copy()
    selm = np.zeros((32, NEXP, P), np.float32)
    selm[np.arange(32), np.arange(32), :] = 1.0
    m["sel"] = selm.reshape(32, NEXP * P)
    return m


def kernel(**inputs):
    m = host_prep(inputs)
    nc, b = build(layers=(0, 1, 2, 3), in_res=False, final=True)
    im = {k: v for k, v in m.items() if k in b.in_names}
    r = run_bass_kernel_spmd(nc, [im], core_ids=[0]).results[0]
    return np.ascontiguousarray(r["outT"].T)[None].astype(np.float32)
```

```python
import math
from contextlib import ExitStack
import numpy as np
import concourse.bass as bass
import concourse.mybir as mybir
from concourse.bass_utils import run_bass_kernel_spmd
from concourse.ap import AP

F32 = mybir.dt.float32
BF16 = mybir.dt.bfloat16
I32 = mybir.dt.int32
AF = mybir.ActivationFunctionType
ALU = mybir.AluOpType
AX = mybir.AxisListType

D = 1024
SEQ = 16384
NCTX = 256
NT = SEQ + NCTX
DEPTH = 4
EPS = 1e-6
NEXP = 32
HID = 512
P = 128


class Res:
    __slots__ = ("w", "r")

    def __init__(self):
        self.w = None
        self.r = {}


class B:
    NDS = 28

    def __init__(self, nc):
        self.nc = nc
        self.engs = {"sp": nc.sync, "act": nc.scalar, "dve": nc.vector, "pool": nc.gpsimd, "pe": nc.tensor}
        self.csem = {e: nc.alloc_semaphore(name="c_" + e) for e in ("act", "dve", "pool", "pe")}
        self.ccnt = {e: 0 for e in self.csem}
        self.dsem = [nc.alloc_semaphore(name="d%d" % i) for i in range(self.NDS)]
        self.dcnt = [0] * self.NDS
        self.dnext = 0
        self.seen = {e: {} for e in self.engs}
        self.ninst = 0

    def _wait(self, e, tok):
        sem, val = tok
        k = id(sem)
        if self.seen[e].get(k, 0) >= val:
            return
        self.engs[e].wait_ge(sem, val)
        self.seen[e][k] = val
        self.ninst += 1

    def op(self, e, fn, reads=(), writes=(), dma=False, inc=True, ss=False, fence=None):
        toks = []
        for r in reads:
            if r.w is not None:
                toks.append(r.w)
        for w in writes:
            if w.w is not None:
                toks.append(w.w)
            toks.extend(w.r.values())
        own = self.csem.get(e)
        for t in toks:
            if (not dma) and t[0] is own and e == "pe":
                continue
            self._wait(e, t)
        self.ninst += 1
        if dma:
            j = self.dnext
            self.dnext = (j + 1) % self.NDS
            if self.dcnt[j] > 0:
                self._wait(e, (self.dsem[j], 16 * self.dcnt[j]))
            ins = fn()
            self.dcnt[j] += 1
            ins.then_inc(self.dsem[j], 16)
            tok = (self.dsem[j], 16 * self.dcnt[j])
        else:
            ins = fn()
            if not inc:
                return None
            if fence is not None:
                ins = fence()
                self.ninst += 1
            self.ccnt[e] += 1
            ins.then_inc(self.csem[e], 1)
            tok = (self.csem[e], self.ccnt[e])
        for r in reads:
            k = id(tok[0])
            o = r.r.get(k)
            if o is None or o[1] < tok[1]:
                r.r[k] = tok
        for w in writes:
            w.w = tok
            w.r = {}
        return tok

    ROT = 30000

    def barrier(self):
        for e in self.engs:
            self.finish(e)
        for e in list(self.csem):
            if self.ccnt[e] > self.ROT:
                self.epoch = getattr(self, "epoch", 0) + 1
                self.csem[e] = self.nc.alloc_semaphore(name="c_%s_%d" % (e, self.epoch))
                self.ccnt[e] = 0

    def finish(self, e="sp"):
        for j in range(self.NDS):
            if self.dcnt[j]:
                self._wait(e, (self.dsem[j], 16 * self.dcnt[j]))
        for k, s in self.csem.items():
            if self.ccnt[k]:
                self._wait(e, (s, self.ccnt[k]))


class Scope(ExitStack):
    def __init__(self, b):
        super().__init__()
        self._b = b

    def __exit__(self, *a):
        self._b.barrier()
        return super().__exit__(*a)


class T:
    def __init__(self, h, nres=1):
        self.h = h
        self.res = [Res() for _ in range(nres)]

    def __getitem__(self, k):
        return self.h[k]

    @property
    def r(self):
        return self.res[0]


class Pool:
    def __init__(self, tiles):
        self.tiles = tiles
        self.i = 0

    def get(self):
        t = self.tiles[self.i % len(self.tiles)]
        self.i += 1
        return t


def build(layers=(0, 1, 2, 3), in_res=False, dump_res=False, final=True, stage=9, ntl=None, out_res=False):
    nc = bass.Bass("TRN2", target_bir_lowering=False)
    b = B(nc)
    es = ExitStack()

    in_names = []
    b.in_names = in_names

    def dram_in(name, shape, dt=F32):
        in_names.append(name)
        return T(nc.dram_tensor(name, list(shape), dt, kind="ExternalInput").ap())

    def dram_tmp(name, shape, dt=F32):
        return T(nc.dram_tensor(name, list(shape), dt).ap())

    def sb(name, shape, dt=F32, stack=None):
        return T(nc.alloc_sbuf_tensor("s_" + name, list(shape), dt))

    def ps(name, shape, dt=F32):
        return T(nc.alloc_psum_tensor(name, list(shape), dt))

    if not in_res:
        xT = dram_in("xT", [D, SEQ])
        posT = dram_in("posT", [D, SEQ])
        ctxT = dram_in("ctxT", [D, NCTX])
    cT = dram_in("cT", [P, 8, 2])
    ada_w = {l: dram_in("ada_w%d" % l, [D, 6 * D]) for l in layers}
    ada_bT = dram_in("ada_bT", [DEPTH, P, 48])
    nmixT = dram_in("nmixT", [DEPTH, P, 8])
    nffnT = dram_in("nffnT", [DEPTH, P, 8])
    nfinT = dram_in("nfinT", [P, 8])
    rw = dram_in("rw", [DEPTH, D, 36])
    rbrep = dram_in("rbrep", [DEPTH, P, 36])
    if stage >= 5:
        wg = {l: dram_in("wg%d" % l, [NEXP * D, HID]) for l in layers}
        wu = {l: dram_in("wu%d" % l, [NEXP * D, HID]) for l in layers}
        wd = {l: dram_in("wd%d" % l, [NEXP * HID, D]) for l in layers}
    dbg = []
    if 0 in layers:
        cm_w_in = dram_in("cm_w_in", [D, 2 * D])
        cm_vg_rep = dram_in("cm_vg_rep", [P, D])
        cm_wsT = dram_in("cm_wsT", [P, 4, P])
        cm_bs_rep = dram_in("cm_bs_rep", [P, 4, 512])
        cm_w_out = dram_in("cm_w_out", [D, D])
    if 3 in layers:
        fn_w_in = dram_in("fn_w_in", [D, D]); fn_w_out = dram_in("fn_w_out", [D, D])
        fn_cs = dram_in("fn_cs", [P, 2, 512]); fn_dft = dram_in("fn_dft", [P, 3, P]); fn_tw = dram_in("fn_tw", [P, 2, P])
        fn_uv = dram_tmp("fn_uv", [SEQ, 2048]); fn_b = dram_tmp("fn_b", [P, P, 2048]); fn_r = dram_tmp("fn_r", [SEQ, D])
    if 1 in layers:
        ml_w_in = dram_in("ml_w_in", [D, 3088]); ml_w_out = dram_in("ml_w_out", [D, D])
        ml_gb = dram_in("ml_gb", [4, 4]); ml_cw = dram_in("ml_cw", [P, 8, 4]); ml_cb = dram_in("ml_cb", [P, 8])
        ml_mask = dram_in("ml_mask", [P, 2, P]); ml_ng = dram_in("ml_ng", [P, 8])
        ml_zqk = dram_tmp("ml_zqk", [D, NT]); ml_so = dram_tmp("ml_so", [D, NT], BF16); ml_v = dram_tmp("ml_v", [NT, D], BF16)
        ml_g = dram_tmp("ml_g", [4, 4, NT]); ml_q = dram_tmp("ml_q", [512, NT], BF16); ml_k = dram_tmp("ml_k", [512, NT], BF16)
        ml_hf = dram_tmp("ml_hf", [D, NT]); ml_gated = dram_tmp("ml_gated", [D, NT], BF16)
    LW = 1280
    if 2 in layers:
        lru_w_in = dram_in("lru_w_in", [D, 2 * LW])
        lru_w_out = dram_in("lru_w_out", [LW, D])
        lru_wa = dram_in("lru_wa", [P, 2, 10, P])
        lru_wx = dram_in("lru_wx", [P, 2, 10, P])
        lru_cw = dram_in("lru_cw", [P, 10, 4])
        lru_vec = dram_in("lru_vec", [P, 7, 10])
        lru_g = dram_tmp("lru_g", [LW, NT], BF16)
        lru_zx = dram_tmp("lru_zx", [LW, NT])
        lru_hf = dram_tmp("lru_hf", [LW, NT])
        lru_gated = dram_tmp("lru_gated", [LW, NT], BF16)
    ident_d = dram_in("ident", [P, P])
    ustrict_d = dram_in("ustrict", [P, P])
    iota32_d = dram_in("iota32", [P, 32])
    NBMAX = (2 * NT + NEXP * 127 + 127) // 128
    blkpos_d = dram_in("blkpos", [P, NBMAX])
    piota_d = dram_in("piota", [P, 1])
    sel_d = dram_in("sel", [32, NEXP * P])
    if final:
        outT = T(nc.dram_tensor("outT", [D, SEQ], F32, kind="ExternalOutput").ap())
    if out_res and not dump_res:
        res_out = T(nc.dram_tensor("res_out", [D, NT], F32, kind="ExternalOutput").ap())
    if in_res:
        res_in = dram_in("res_in", [D, NT])
    if dump_res:
        res_out = T(nc.dram_tensor("res_out", [D, NT], F32, kind="ExternalOutput").ap())
        info_out = T(nc.dram_tensor("info_out", [P, NT // P, 8], F32, kind="ExternalOutput").ap())
        dest_out = T(nc.dram_tensor("dest_out", [P, NT // P, 2], I32, kind="ExternalOutput").ap())
        carry_out = T(nc.dram_tensor("carry_out", [P, 32], F32, kind="ExternalOutput").ap())

    resd = dram_tmp("resd", [D, NT])
    h2rows = dram_tmp("h2rows", [NT, D], BF16)
    xbuf = dram_tmp("xbuf", [NBMAX * P, D], BF16)
    ybuf = dram_tmp("ybuf", [NBMAX * P, D], F32)

    def fm(t, c0, n):
        return t[:, c0:c0 + n].rearrange("(k p) t -> p k t", p=P)

    ident = sb("ident", [P, P])
    identb = sb("identb", [P, P], BF16)
    ones = sb("ones", [P, P])
    onesb = sb("onesb", [P, P], BF16)
    ustrict = sb("ustrictb", [P, P], BF16)
    ustrict_f = sb("ustrictf", [P, P])
    iota32 = sb("iota32", [P, 32])
    blkpos = sb("blkpos", [P, NBMAX])
    piota = sb("piota", [P, 1])
    sc = sb("sc", [P, 8, 2])
    modall = sb("modall", [P, DEPTH, 48, 2])
    nmix = sb("nmix", [P, DEPTH, 8])
    nffn = sb("nffn", [P, DEPTH, 8])
    nfin = sb("nfin", [P, 8])
    g1 = sb("g1", [P, 2, 8]); sh1 = sb("sh1", [P, 2, 8]); gt1 = sb("gt1", [P, 2, 8])
    g2 = sb("g2", [P, 2, 8]); sh2 = sb("sh2", [P, 2, 8]); gt2 = sb("gt2", [P, 2, 8])

    def dma(e, out_t, out_ap, in_t, in_ap):
        return b.op(e, lambda: b.engs[e].dma_start(out=out_ap, in_=in_ap), reads=[in_t.r], writes=[out_t.r], dma=True)

    dma("sp", ident, ident[:, :], ident_d, ident_d[:, :])
    dma("sp", ustrict_f, ustrict_f[:, :], ustrict_d, ustrict_d[:, :])
    dma("sp", iota32, iota32[:, :], iota32_d, iota32_d[:, :])
    dma("sp", blkpos, blkpos[:, :], blkpos_d, blkpos_d[:, :])
    dma("sp", piota, piota[:, :], piota_d, piota_d[:, :])
    dma("sp", sc, sc[:, :, :], cT, cT[:, :, :])
    dma("sp", nmix, nmix[:, :, :], nmixT, nmixT[:, :, :].rearrange("l p k -> p l k"))
    dma("sp", nffn, nffn[:, :, :], nffnT, nffnT[:, :, :].rearrange("l p k -> p l k"))
    dma("sp", nfin, nfin[:, :], nfinT, nfinT[:, :])
    V = nc.vector
    A = nc.scalar
    PE = nc.tensor
    G = nc.gpsimd
    b.op("dve", lambda: V.tensor_copy(out=identb[:, :], in_=ident[:, :]), [ident.r], [identb.r])
    b.op("dve", lambda: V.tensor_copy(out=ustrict[:, :], in_=ustrict_f[:, :]), [ustrict_f.r], [ustrict.r])
    b.op("dve", lambda: V.memset(ones[:, :], 1.0), [], [ones.r])
    b.op("dve", lambda: V.memset(onesb[:, :], 1.0), [], [onesb.r])
    b.op("act", lambda: A.activation(out=sc[:, :, :], in_=sc[:, :, :], func=AF.Silu), [sc.r], [sc.r])

    psf = Pool([ps("psf%d" % i, [P, 512]) for i in range(6)])
    psb = Pool([ps("psb%d" % i, [P, 1024], BF16) for i in range(2)])

    with Scope(b) as st:
        awt = T(st.enter_context(nc.sbuf_tensor("awt", [P, 8, 1024], F32)))
        abt = T(st.enter_context(nc.sbuf_tensor("abt", [P, 48], F32)))
        for L in layers:
            dma("sp", abt, abt[:, :], ada_bT, ada_bT[L, :, :])
            for m in range(6):
                dma("sp", awt, awt[:, :, :], ada_w[L],
                    ada_w[L][:, m * D:(m + 1) * D].rearrange("(k p) n -> p k n", p=P))
                pt = psf.get()
                for o in range(8):
                    for k in range(8):
                        last = (o == 7 and k == 7)
                        b.op("pe", lambda o=o, k=k: PE.matmul(pt[:, 2 * o:2 * o + 2], lhsT=awt[:, k, o * P:(o + 1) * P],
                                                               rhs=sc[:, k, :], start=(k == 0), stop=(k == 7)),
                             [awt.r, sc.r], [pt.r], inc=last)
                for j in range(2):
                    b.op("dve", lambda j=j, m=m: V.tensor_tensor(
                        out=modall[:, L, m * 8:(m + 1) * 8, j],
                        in0=pt[:, 0:16].rearrange("p (o j) -> p o j", j=2)[:, :, j],
                        in1=abt[:, m * 8:(m + 1) * 8], op=ALU.add), [pt.r, abt.r], [modall.r])

    def layer_scalars(L):
        for j in range(2):
            b.op("dve", lambda j=j: V.scalar_tensor_tensor(out=g1[:, j, :], in0=modall[:, L, 8:16, j], scalar=1.0,
                                                            in1=nmix[:, L, :], op0=ALU.add, op1=ALU.mult),
                 [modall.r, nmix.r], [g1.r])
            b.op("dve", lambda j=j: V.scalar_tensor_tensor(out=g2[:, j, :], in0=modall[:, L, 32:40, j], scalar=1.0,
                                                            in1=nffn[:, L, :], op0=ALU.add, op1=ALU.mult),
                 [modall.r, nffn.r], [g2.r])
            b.op("dve", lambda j=j: V.tensor_copy(out=sh1[:, j, :], in_=modall[:, L, 0:8, j]), [modall.r], [sh1.r])
            b.op("dve", lambda j=j: V.tensor_copy(out=gt1[:, j, :], in_=modall[:, L, 16:24, j]), [modall.r], [gt1.r])
            b.op("dve", lambda j=j: V.tensor_copy(out=sh2[:, j, :], in_=modall[:, L, 24:32, j]), [modall.r], [sh2.r])
            b.op("dve", lambda j=j: V.tensor_copy(out=gt2[:, j, :], in_=modall[:, L, 40:48, j]), [modall.r], [gt2.r])

    with Scope(b) as st:
        pa = Pool([T(st.enter_context(nc.sbuf_tensor("pa%d" % i, [P, 8, 512], F32))) for i in range(2)])
        pb = Pool([T(st.enter_context(nc.sbuf_tensor("pb%d" % i, [P, 8, 512], F32))) for i in range(2)])
        if in_res:
            for c0 in range(0, NT, 512):
                n = min(512, NT - c0)
                a = pa.get()
                dma("sp", a, a[:, :, :n], res_in, fm(res_in, c0, n))
                dma("sp", resd, fm(resd, c0, n), a, a[:, :, :n])
        else:
            a = pa.get()
            dma("sp", a, a[:, :, :NCTX], ctxT, fm(ctxT, 0, NCTX))
            dma("sp", resd, fm(resd, 0, NCTX), a, a[:, :, :NCTX])
            for c0 in range(0, SEQ, 512):
                a = pa.get(); p2 = pb.get()
                dma("sp", a, a[:, :, :], xT, fm(xT, c0, 512))
                dma("sp", p2, p2[:, :, :], posT, fm(posT, c0, 512))
                b.op("pool", lambda a=a, p2=p2: G.tensor_tensor(out=a[:, :, :], in0=a[:, :, :], in1=p2[:, :, :], op=ALU.add),
                     [a.r, p2.r], [a.r])
                dma("sp", resd, fm(resd, NCTX + c0, 512), a, a[:, :, :])

    def emit_dbg():
        for (nm, t_, ap_, shp, dt_) in dbg:
            o_ = T(nc.dram_tensor("dbg_" + nm, list(shp), dt_, kind="ExternalOutput").ap())
            b.op("pool", lambda o_=o_, ap_=ap_: G.dma_start(out=o_[tuple(slice(None) for _ in shp)], in_=ap_), [t_.r], [o_.r], dma=True)
        del dbg[:]

    def tiles(with_ctx):
        tl = [(0, NCTX, 1)] if with_ctx else []
        tl += [(NCTX + i * 512, 512, 0) for i in range(SEQ // 512)]
        if ntl is not None:
            tl = tl[:ntl]
        return tl

    def rmsnorm_mod(xin, n, j, gsc, shf, tbuf, sqb, out_tiles):
        pst = psf.get()
        for k in range(8):
            b.op("act", lambda k=k: A.activation(out=sqb[:, k, :n], in_=xin[:, k, :n], func=AF.Square), [xin.r], [sqb.r])
        for k in range(8):
            b.op("pe", lambda k=k: PE.matmul(pst[:, :n], lhsT=ones[:, :], rhs=sqb[:, k, :n], start=(k == 0), stop=(k == 7)),
                 [ones.r, sqb.r], [pst.r], inc=(k == 7))
        rstd = sqb
        b.op("act", lambda: A.activation(out=rstd[:, 0, :n], in_=pst[:, :n], func=AF.Sqrt, bias=epsc[:, 0:1], scale=1.0 / D),
             [pst.r, epsc.r], [sqb.r])
        b.op("dve", lambda: V.reciprocal(out=rstd[:, 0, :n], in_=rstd[:, 0, :n]), [sqb.r], [sqb.r])
        for k in range(8):
            b.op("dve", lambda k=k: V.tensor_tensor(out=tbuf[:, k, :n], in0=xin[:, k, :n], in1=rstd[:, 0, :n], op=ALU.mult),
                 [xin.r, sqb.r], [tbuf.r])
        for ot in out_tiles:
            for k in range(8):
                b.op("act", lambda k=k, ot=ot: A.activation(out=ot[:, k, :n], in_=tbuf[:, k, :n], func=AF.Identity,
                                                           bias=shf[:, j, k:k + 1], scale=gsc[:, j, k:k + 1]),
                     [tbuf.r, shf.r, gsc.r], [ot.r])

    epsc = sb("epsc", [P, 1])
    b.op("dve", lambda: V.memset(epsc[:, :], EPS), [], [epsc.r])
    onec = sb("onec", [P, 1])
    b.op("dve", lambda: V.memset(onec[:, :], 1.0), [], [onec.r])

    def gelu_tanh(out_ap, out_t, in_ap, in_t, tmp, n_shape):
        t1, t2 = tmp
        b.op("act", lambda: A.activation(out=t1[0], in_=in_ap, func=AF.Square), [in_t.r], [t1[1].r])
        b.op("dve", lambda: V.tensor_scalar(out=t1[0], in0=t1[0], scalar1=0.044715, scalar2=1.0, op0=ALU.mult, op1=ALU.add),
             [t1[1].r], [t1[1].r])
        b.op("dve", lambda: V.tensor_tensor(out=t1[0], in0=t1[0], in1=in_ap, op=ALU.mult), [t1[1].r, in_t.r], [t1[1].r])
        b.op("act", lambda: A.activation(out=t2[0], in_=t1[0], func=AF.Sigmoid, scale=1.5957691216057308),
             [t1[1].r], [t2[1].r])
        b.op("dve", lambda: V.tensor_tensor(out=out_ap, in0=t2[0], in1=in_ap, op=ALU.mult), [t2[1].r, in_t.r], [out_t.r])

    class Moe:
        pass

    m = Moe()
    m.rwt = sb("rwt", [P, 8, 36]); m.rbt = sb("rbt", [P, 36]); m.carry = sb("carry", [P, 32])
    m.info = sb("rinfo", [P, NT // P, 8])
    m.dest = T(nc.alloc_sbuf_tensor("dest", [P, NT // P, 2], I32))
    m.sm = Pool([sb("rsm%d" % i, [P, 256]) for i in range(2)])
    m.mb = Pool([T(nc.alloc_sbuf_tensor("mb%d" % i, [P, 32], BF16)) for i in range(2)])

    def moe_begin(m, L, ntok):
        m.nch = ntok // P
        m.nb = (2 * ntok + NEXP * 127 + 127) // 128
        dma("sp", m.rwt, m.rwt[:, :, :], rw, rw[L, :, :].rearrange("(k p) n -> p k n", p=P))
        dma("sp", m.rbt, m.rbt[:, :], rbrep, rbrep[L, :, :])
        b.op("dve", lambda: V.memset(m.carry[:, :], 0.0), [], [m.carry.r])

    def moe_route_chunk(m, h2f, off, ch, wt=None):
        s = m.sm.get()
        S = s.h
        pl = psf.get()
        for k in range(8):
            b.op("pe", lambda k=k: PE.matmul(pl[:, 0:36], lhsT=h2f[:, k, off:off + P], rhs=m.rwt[:, k, :],
                                              start=(k == 0), stop=(k == 7)), [h2f.r, m.rwt.r], [pl.r], inc=(k == 7))
        ops = []
        rs = [s.r]
        b.op("dve", lambda: V.tensor_tensor(out=S[:, 0:36], in0=pl[:, 0:36], in1=m.rbt[:, :], op=ALU.add), [pl.r, m.rbt.r], rs)
        b.op("dve", lambda: V.reduce_max(out=S[:, 40:41], in_=S[:, 0:4], axis=AX.X), rs, rs, ss=True)
        b.op("dve", lambda: V.tensor_scalar(out=S[:, 36:40], in0=S[:, 0:4], scalar1=S[:, 40:41], scalar2=None, op0=ALU.is_ge), rs, rs, ss=True)
        b.op("dve", lambda: V.tensor_scalar(out=S[:, 208:212], in0=S[:, 0:4], scalar1=S[:, 40:41], scalar2=None, op0=ALU.subtract), rs, rs, ss=True)
        b.op("act", lambda: A.activation(out=S[:, 208:212], in_=S[:, 208:212], func=AF.Exp), rs, rs, ss=True)
        b.op("dve", lambda: V.reduce_sum(out=S[:, 41:42], in_=S[:, 208:212], axis=AX.X), rs, rs, ss=True)
        b.op("dve", lambda: V.reciprocal(out=S[:, 41:42], in_=S[:, 41:42]), rs, rs)
        b.op("dve", lambda: V.tensor_scalar(out=S[:, 44:52], in0=S[:, 4:12], scalar1=S[:, 36:37], scalar2=None, op0=ALU.mult), rs, rs, ss=True)
        for g in range(1, 4):
            b.op("dve", lambda g=g: V.scalar_tensor_tensor(out=S[:, 44:52], in0=S[:, 4 + 8 * g:12 + 8 * g], scalar=S[:, 36 + g:37 + g],
                                                            in1=S[:, 44:52], op0=ALU.mult, op1=ALU.add), rs, rs, ss=True)
        b.op("dve", lambda: V.reduce_max(out=S[:, 76:77], in_=S[:, 44:52], axis=AX.X), rs, rs, ss=True)
        b.op("dve", lambda: V.tensor_scalar(out=S[:, 52:60], in0=S[:, 44:52], scalar1=S[:, 76:77], scalar2=None, op0=ALU.is_ge), rs, rs, ss=True)
        b.op("dve", lambda: V.scalar_tensor_tensor(out=S[:, 60:68], in0=S[:, 52:60], scalar=-1e30, in1=S[:, 44:52],
                                                    op0=ALU.mult, op1=ALU.add), rs, rs, ss=True)
        b.op("dve", lambda: V.reduce_max(out=S[:, 77:78], in_=S[:, 60:68], axis=AX.X), rs, rs, ss=True)
        b.op("dve", lambda: V.tensor_scalar(out=S[:, 68:76], in0=S[:, 60:68], scalar1=S[:, 77:78], scalar2=None, op0=ALU.is_ge), rs, rs, ss=True)
        b.op("dve", lambda: V.tensor_tensor(out=S[:, 78:79], in0=S[:, 77:78], in1=S[:, 76:77], op=ALU.subtract), rs, rs, ss=True)
        b.op("act", lambda: A.activation(out=S[:, 78:79], in_=S[:, 78:79], func=AF.Exp), rs, rs)
        b.op("dve", lambda: V.tensor_scalar(out=S[:, 79:80], in0=S[:, 78:79], scalar1=1.0, scalar2=None, op0=ALU.add), rs, rs, ss=True)
        b.op("dve", lambda: V.reciprocal(out=S[:, 79:80], in_=S[:, 79:80]), rs, rs, ss=True)
        ir = [m.info.r]
        b.op("dve", lambda: V.tensor_tensor(out=m.info[:, ch, 2:3], in0=S[:, 79:80], in1=S[:, 41:42], op=ALU.mult), rs, ir)
        b.op("dve", lambda: V.tensor_tensor(out=m.info[:, ch, 3:4], in0=m.info[:, ch, 2:3], in1=S[:, 78:79], op=ALU.mult), rs + ir, ir, ss=True)
        for g in range(4):
            b.op("dve", lambda g=g: V.tensor_scalar(out=S[:, 80 + 8 * g:88 + 8 * g], in0=S[:, 52:60], scalar1=S[:, 36 + g:37 + g],
                                                     scalar2=None, op0=ALU.mult), rs, rs, ss=True)
            b.op("dve", lambda g=g: V.tensor_scalar(out=S[:, 112 + 8 * g:120 + 8 * g], in0=S[:, 68:76], scalar1=S[:, 36 + g:37 + g],
                                                     scalar2=None, op0=ALU.mult), rs, rs, ss=True)
        if wt is not None:
            wt_t, wt_ap = wt
            b.op("dve", lambda: V.tensor_scalar(out=S[:, 144:176], in0=S[:, 80:112], scalar1=m.info[:, ch, 2:3], scalar2=None, op0=ALU.mult),
                 rs + ir, rs)
            b.op("dve", lambda: V.scalar_tensor_tensor(out=wt_ap, in0=S[:, 112:144], scalar=m.info[:, ch, 3:4], in1=S[:, 144:176],
                                                        op0=ALU.mult, op1=ALU.add), rs + ir, [wt_t.r])
            return
        mb = m.mb.get()
        b.op("dve", lambda: V.tensor_tensor(out=mb[:, :], in0=S[:, 80:112], in1=S[:, 112:144], op=ALU.add), rs, [mb.r])
        b.op("dve", lambda: V.tensor_tensor(out=S[:, 144:176], in0=S[:, 80:112], in1=iota32[:, :], op=ALU.mult), rs + [iota32.r], rs)
        b.op("dve", lambda: V.reduce_sum(out=m.info[:, ch, 0:1], in_=S[:, 144:176], axis=AX.X), rs, ir, ss=True)
        b.op("dve", lambda: V.tensor_tensor(out=S[:, 144:176], in0=S[:, 112:144], in1=iota32[:, :], op=ALU.mult), rs + [iota32.r], rs)
        b.op("dve", lambda: V.reduce_sum(out=m.info[:, ch, 1:2], in_=S[:, 144:176], axis=AX.X), rs, ir, ss=True)
        pc = psf.get()
        b.op("pe", lambda: PE.matmul(pc[:, 0:32], lhsT=ustrict[:, :], rhs=mb[:, :], start=True, stop=True), [ustrict.r, mb.r], [pc.r], inc=False)
        b.op("pe", lambda: PE.matmul(pc[:, 32:64], lhsT=onesb[:, :], rhs=mb[:, :], start=True, stop=True), [onesb.r, mb.r], [pc.r])
        b.op("dve", lambda: V.tensor_tensor(out=S[:, 176:208], in0=pc[:, 0:32], in1=m.carry[:, :], op=ALU.add), [pc.r, m.carry.r], rs)
        b.op("dve", lambda: V.tensor_tensor(out=m.carry[:, :], in0=pc[:, 32:64], in1=m.carry[:, :], op=ALU.add), [pc.r, m.carry.r], [m.carry.r])
        b.op("dve", lambda: V.tensor_tensor(out=S[:, 144:176], in0=S[:, 80:112], in1=S[:, 176:208], op=ALU.mult), rs, rs, ss=True)
        b.op("dve", lambda: V.reduce_sum(out=m.info[:, ch, 4:5], in_=S[:, 144:176], axis=AX.X), rs, ir, ss=True)
        b.op("dve", lambda: V.tensor_tensor(out=S[:, 144:176], in0=S[:, 112:144], in1=S[:, 176:208], op=ALU.mult), rs, rs, ss=True)
        b.op("dve", lambda: V.reduce_sum(out=m.info[:, ch, 5:6], in_=S[:, 144:176], axis=AX.X), rs, ir, ss=True)

    def moe_rows_chunk(m, h2b, off, tok0, rows):
        pt = psb.get()
        for k in range(8):
            b.op("pe", lambda k=k: PE.transpose(out=pt[:, k * P:(k + 1) * P], in_=h2b[:, k, off:off + P], identity=identb[:, :]),
                 [h2b.r, identb.r], [pt.r], inc=(k == 7))
        b.op("act", lambda: A.copy(out=rows[:, :], in_=pt[:, :]), [pt.r], [rows.r])
        dma("sp", h2rows, h2rows[tok0:tok0 + P, :], rows, rows[:, :])

    def moe_finish(m, L, tl, gt, tok_base):
        nb = m.nb
        with Scope(b) as st:
            def S_(name, shape, dt=F32):
                return T(st.enter_context(nc.sbuf_tensor(name, list(shape), dt)))
            padded = S_("padded", [P, 32]); pend = S_("pend", [P, 32]); pstart = S_("pstart", [P, 32])
            cmp3 = S_("cmp3", [P, nb, 32]); eb = S_("eb", [P, nb]); idxg = S_("idxg", [P, nb], I32); idxd = S_("idxd", [P, nb], I32)
            tmp32 = S_("tmp32", [P, 32]); destf = S_("destf", [P, m.nch, 2])
            b.op("dve", lambda: V.tensor_scalar(out=tmp32[:, :], in0=m.carry[:, :], scalar1=127.0, scalar2=1.0 / 128.0, op0=ALU.add, op1=ALU.mult),
                 [m.carry.r], [tmp32.r])
            b.op("dve", lambda: V.tensor_scalar(out=tmp32[:, :], in0=tmp32[:, :], scalar1=-0.498046875, scalar2=None, op0=ALU.add), [tmp32.r], [tmp32.r])
            b.op("dve", lambda: V.tensor_scalar(out=tmp32[:, :], in0=tmp32[:, :], scalar1=8388608.0, scalar2=None, op0=ALU.add), [tmp32.r], [tmp32.r])
            b.op("dve", lambda: V.tensor_scalar(out=padded[:, :], in0=tmp32[:, :], scalar1=-8388608.0, scalar2=128.0, op0=ALU.add, op1=ALU.mult),
                 [tmp32.r], [padded.r])
            pp = [pend, tmp32]
            b.op("dve", lambda: V.tensor_copy(out=pend[:, :], in_=padded[:, :]), [padded.r], [pend.r])
            cur = 0
            for sh in (1, 2, 4, 8, 16):
                A_, B_ = pp[cur], pp[1 - cur]
                b.op("dve", lambda A_=A_, B_=B_, sh=sh: V.tensor_copy(out=B_[:, 0:sh], in_=A_[:, 0:sh]), [A_.r], [B_.r])
                b.op("dve", lambda A_=A_, B_=B_, sh=sh: V.tensor_tensor(out=B_[:, sh:32], in0=A_[:, sh:32], in1=A_[:, 0:32 - sh], op=ALU.add), [A_.r], [B_.r])
                cur = 1 - cur
            if cur == 1:
                b.op("dve", lambda: V.tensor_copy(out=pend[:, :], in_=tmp32[:, :]), [tmp32.r], [pend.r])
            b.op("dve", lambda: V.tensor_tensor(out=pstart[:, :], in0=pend[:, :], in1=padded[:, :], op=ALU.subtract),
                 [pend.r, padded.r], [pstart.r])
            b.op("dve", lambda: V.tensor_tensor(out=cmp3[:, :, :], in0=pend[:, :].unsqueeze(1).to_broadcast([P, nb, 32]),
                                                 in1=blkpos[:, 0:nb].unsqueeze(2).to_broadcast([P, nb, 32]), op=ALU.is_le),
                 [pend.r, blkpos.r], [cmp3.r])
            b.op("dve", lambda: V.reduce_sum(out=eb[:, :], in_=cmp3[:, :, :], axis=AX.X), [cmp3.r], [eb.r])
            b.op("dve", lambda: V.tensor_scalar(out=eb[:, :], in0=eb[:, :], scalar1=31.0, scalar2=None, op0=ALU.min), [eb.r], [eb.r])
            b.op("dve", lambda: V.tensor_scalar(out=idxg[:, :], in0=eb[:, :], scalar1=float(D), scalar2=piota[:, 0:1], op0=ALU.mult, op1=ALU.add),
                 [eb.r, piota.r], [idxg.r])
            b.op("dve", lambda: V.tensor_scalar(out=idxd[:, :], in0=eb[:, :], scalar1=float(HID), scalar2=piota[:, 0:1], op0=ALU.mult, op1=ALU.add),
                 [eb.r, piota.r], [idxd.r])
            oh = S_("ohd", [P, 32])
            for ch in range(m.nch):
                for kk in range(2):
                    b.op("dve", lambda ch=ch, kk=kk: V.tensor_scalar(out=oh[:, :], in0=iota32[:, :], scalar1=m.info[:, ch, kk:kk + 1],
                                                                      scalar2=None, op0=ALU.is_equal), [iota32.r, m.info.r], [oh.r])
                    b.op("dve", lambda: V.tensor_tensor(out=oh[:, :], in0=oh[:, :], in1=pstart[:, :], op=ALU.mult), [oh.r, pstart.r], [oh.r])
                    b.op("dve", lambda ch=ch, kk=kk: V.reduce_sum(out=destf[:, ch, kk:kk + 1], in_=oh[:, :], axis=AX.X), [oh.r], [destf.r])
            b.op("dve", lambda: V.tensor_tensor(out=destf[:, :, :], in0=destf[:, :, :], in1=m.info[:, 0:m.nch, 4:6], op=ALU.add),
                 [destf.r, m.info.r], [destf.r])
            b.op("dve", lambda: V.tensor_copy(out=m.dest[:, 0:m.nch, :], in_=destf[:, :, :]), [destf.r], [m.dest.r])
            if stage < 4:
                if dump_res:
                    dbg.extend([("padded", padded, padded[:, :], [P, 32], F32), ("pend", pend, pend[:, :], [P, 32], F32),
                                ("pstart", pstart, pstart[:, :], [P, 32], F32), ("destf", destf, destf[:, :, :], [P, m.nch, 2], F32),
                                ("eb", eb, eb[:, :], [P, nb], F32), ("idxg", idxg, idxg[:, :], [P, nb], I32)])
                    emit_dbg()
                    b.barrier()
                return
            rp = Pool([S_("scr%d" % i, [P, D], BF16) for i in range(3)])
            for ch in range(m.nch):
                r_ = rp.get()
                dma("sp", r_, r_[:, :], h2rows, h2rows[ch * P:(ch + 1) * P, :])
                for kk in range(2):
                    b.op("pool", lambda ch=ch, kk=kk, r_=r_: G.indirect_dma_start(
                        out=xbuf[:, :], out_offset=bass.IndirectOffsetOnAxis(ap=m.dest[:, ch, kk:kk + 1], axis=0),
                        in_=r_[:, :], in_offset=None), [r_.r, m.dest.r], [xbuf.r], dma=True)
            if stage < 5:
                return
            wgp = Pool([S_("wgt%d" % i, [P, 8, HID], BF16) for i in range(2)])
            wup = Pool([S_("wut%d" % i, [P, 8, HID], BF16) for i in range(2)])
            wdp = Pool([S_("wdt%d" % i, [P, 4, D], BF16) for i in range(2)])
            xrp = Pool([S_("xr%d" % i, [P, D], BF16) for i in range(2)])
            xtp = Pool([S_("xt%d" % i, [P, 8, P], BF16) for i in range(2)])
            hp = Pool([S_("hh%d" % i, [P, 4, P], BF16) for i in range(2)])
            sgp = Pool([S_("sg%d" % i, [P, P], F32) for i in range(2)])
            yp = Pool([S_("yy%d" % i, [P, D], F32) for i in range(2)])
            wgL = wg[L][:, :]; wuL = wu[L][:, :]; wdL = wd[L][:, :]
            for blk in range(nb):
                wgt = wgp.get(); wut = wup.get(); wdt = wdp.get()
                for k in range(8):
                    b.op("pool", lambda k=k, wgt=wgt, blk=blk: G.indirect_dma_start(
                        out=wgt[:, k, :], out_offset=None, in_=wgL,
                        in_offset=bass.IndirectOffsetOnAxis(ap=idxg[:, blk:blk + 1], axis=0), element_offset=k * P * HID),
                        [wg[L].r, idxg.r], [wgt.r], dma=True)
                    b.op("pool", lambda k=k, wut=wut, blk=blk: G.indirect_dma_start(
                        out=wut[:, k, :], out_offset=None, in_=wuL,
                        in_offset=bass.IndirectOffsetOnAxis(ap=idxg[:, blk:blk + 1], axis=0), element_offset=k * P * HID),
                        [wu[L].r, idxg.r], [wut.r], dma=True)
                for k in range(4):
                    b.op("pool", lambda k=k, wdt=wdt, blk=blk: G.indirect_dma_start(
                        out=wdt[:, k, :], out_offset=None, in_=wdL,
                        in_offset=bass.IndirectOffsetOnAxis(ap=idxd[:, blk:blk + 1], axis=0), element_offset=k * P * D),
                        [wd[L].r, idxd.r], [wdt.r], dma=True)
                xr = xrp.get(); xt = xtp.get()
                dma("sp", xr, xr[:, :], xbuf, xbuf[blk * P:(blk + 1) * P, :])
                pt = psb.get()
                for k in range(8):
                    b.op("pe", lambda k=k: PE.transpose(out=pt[:, k * P:(k + 1) * P], in_=xr[:, k * P:(k + 1) * P], identity=identb[:, :]),
                         [xr.r, identb.r], [pt.r], inc=(k == 7))
                b.op("act", lambda: A.copy(out=xt[:, :, :], in_=pt[:, :].rearrange("p (k s) -> p k s", k=8)), [pt.r], [xt.r])
                hh = hp.get()
                for j in range(4):
                    pg = psf.get()
                    for k in range(8):
                        b.op("pe", lambda k=k, j=j: PE.matmul(pg[:, 0:P], lhsT=wgt[:, k, j * P:(j + 1) * P], rhs=xt[:, k, :],
                                                               start=(k == 0), stop=(k == 7)), [wgt.r, xt.r], [pg.r], inc=False)
                    for k in range(8):
                        b.op("pe", lambda k=k, j=j: PE.matmul(pg[:, P:2 * P], lhsT=wut[:, k, j * P:(j + 1) * P], rhs=xt[:, k, :],
                                                               start=(k == 0), stop=(k == 7)), [wut.r, xt.r], [pg.r], inc=(k == 7))
                    sg = sgp.get()
                    b.op("act", lambda: A.activation(out=sg[:, :], in_=pg[:, 0:P], func=AF.Silu), [pg.r], [sg.r])
                    b.op("dve", lambda j=j: V.tensor_tensor(out=hh[:, j, :], in0=sg[:, :], in1=pg[:, P:2 * P], op=ALU.mult),
                         [sg.r, pg.r], [hh.r])
                yy = yp.get()
                for half in range(2):
                    py = psf.get()
                    for j in range(4):
                        b.op("pe", lambda j=j, half=half: PE.matmul(py[:, :], lhsT=hh[:, j, :], rhs=wdt[:, j, half * 512:(half + 1) * 512],
                                                                     start=(j == 0), stop=(j == 3)), [hh.r, wdt.r], [py.r], inc=(j == 3))
                    if half == 0:
                        b.op("act", lambda: A.copy(out=yy[:, 0:512], in_=py[:, :]), [py.r], [yy.r])
                    else:
                        b.op("dve", lambda: V.tensor_copy(out=yy[:, 512:1024], in_=py[:, :]), [py.r], [yy.r])
                dma("sp", ybuf, ybuf[blk * P:(blk + 1) * P, :], yy, yy[:, :])
        if stage < 6:
            return
        with Scope(b) as st:
            def S_(name, shape, dt=F32):
                return T(st.enter_context(nc.sbuf_tensor(name, list(shape), dt)))
            y0p = Pool([S_("y0_%d" % i, [P, D]) for i in range(2)])
            y1p = Pool([S_("y1_%d" % i, [P, D]) for i in range(2)])
            xp = Pool([S_("xd%d" % i, [P, 8, 512]) for i in range(2)])
            for (c0, n, isctx) in tl:
                xin = xp.get()
                dma("sp", xin, xin[:, :, :n], resd, fm(resd, c0, n))
                for ci in range(n // P):
                    ch = (c0 - tok_base) // P + ci
                    y0 = y0p.get(); y1 = y1p.get()
                    for kk, yt in ((0, y0), (1, y1)):
                        b.op("pool", lambda ch=ch, kk=kk, yt=yt: G.indirect_dma_start(
                            out=yt[:, :], out_offset=None, in_=ybuf[:, :],
                            in_offset=bass.IndirectOffsetOnAxis(ap=m.dest[:, ch, kk:kk + 1], axis=0)),
                            [ybuf.r, m.dest.r], [yt.r], dma=True)
                    b.op("dve", lambda ch=ch, y0=y0: V.tensor_scalar(out=y0[:, :], in0=y0[:, :], scalar1=m.info[:, ch, 2:3], scalar2=None,
                                                                      op0=ALU.mult), [y0.r, m.info.r], [y0.r])
                    b.op("dve", lambda ch=ch, y0=y0, y1=y1: V.scalar_tensor_tensor(out=y0[:, :], in0=y1[:, :], scalar=m.info[:, ch, 3:4],
                                                                                    in1=y0[:, :], op0=ALU.mult, op1=ALU.add),
                         [y0.r, y1.r, m.info.r], [y0.r])
                    for hf in range(2):
                        pt = psf.get()
                        for kq in range(4):
                            k = hf * 4 + kq
                            b.op("pe", lambda k=k, kq=kq, y0=y0, pt=pt: PE.transpose(out=pt[:, kq * P:(kq + 1) * P], in_=y0[:, k * P:(k + 1) * P],
                                                                                     identity=ident[:, :]), [y0.r, ident.r], [pt.r], inc=(kq == 3))
                        for kq in range(4):
                            k = hf * 4 + kq
                            b.op("dve", lambda k=k, kq=kq, pt=pt, ci=ci: V.scalar_tensor_tensor(
                                out=xin[:, k, ci * P:(ci + 1) * P], in0=pt[:, kq * P:(kq + 1) * P], scalar=gt[:, isctx, k:k + 1],
                                in1=xin[:, k, ci * P:(ci + 1) * P], op0=ALU.mult, op1=ALU.add), [pt.r, gt.r, xin.r], [xin.r])
                dma("sp", resd, fm(resd, c0, n), xin, xin[:, :, :n])

    GT = 2

    def moe_dense(L, tl):
        moe_begin(m, L, sum(t[1] for t in tl))
        wgL = wg[L]; wuL = wu[L]; wdL = wd[L]
        with Scope(b) as st:
            def S_(name, shape, dt=F32):
                return T(st.enter_context(nc.sbuf_tensor("%s_L%d" % (name, L), list(shape), dt)))
            xin = S_("mx", [P, 8, 512]); sqb = S_("msq", [P, 8, 512]); tb = xin
            h2 = [S_("mh2_%d" % i, [P, 8, 512], BF16) for i in range(GT)]
            yac = [S_("myac%d" % i, [P, 8, 512]) for i in range(GT)]
            wtT = [S_("mwtT%d" % i, [32, 512]) for i in range(GT)]
            wtc = S_("mwtc", [P, 4, 32])
            sel = S_("msel", [32, NEXP * P])
            dma("sp", sel, sel[:, :], sel_d, sel_d[:, :])
            wgp = Pool([S_("mwg%d" % i, [P, 8, HID], BF16) for i in range(2)])
            wup = Pool([S_("mwu%d" % i, [P, 8, HID], BF16) for i in range(2)])
            wdp = Pool([S_("mwd%d" % i, [P, 4, D], BF16) for i in range(2)])
            wrow = S_("mwrow", [P, 512]); sgP = Pool([S_("msg%d" % i, [P, 512]) for i in range(2)]); ttP = Pool([S_("mtt%d" % i, [P, 512]) for i in range(2)])
            hsp = Pool([S_("mhs%d" % i, [P, 4, 512], BF16) for i in range(2)])
            for g0 in range(0, len(tl), GT):
                grp = tl[g0:g0 + GT]
                for i, (c0, n, isctx) in enumerate(grp):
                    dma("sp", xin, xin[:, :, :n], resd, fm(resd, c0, n))
                    rmsnorm_mod(xin, n, isctx, g2, sh2, tb, sqb, [sqb, h2[i]])
                    for ci in range(n // P):
                        moe_route_chunk(m, sqb, ci * P, c0 // P + ci, wt=(wtc, wtc[:, ci, :]))
                    pT = psf.get()
                    for ci in range(n // P):
                        b.op("pe", lambda ci=ci: PE.transpose(out=pT[0:32, ci * P:(ci + 1) * P], in_=wtc[:, ci, :], identity=ident[:, :]),
                             [wtc.r, ident.r], [pT.r], inc=(ci == n // P - 1))
                    b.op("act", lambda i=i, n=n: A.copy(out=wtT[i][:, :n], in_=pT[0:32, :n]), [pT.r], [wtT[i].r])
                    b.op("pool", lambda i=i: G.memset(yac[i][:, :, :], 0.0), [], [yac[i].r])
                for e in range(NEXP):
                    wgt = wgp.get(); wut = wup.get(); wdt = wdp.get()
                    b.op("pool", lambda: G.dma_start(out=wgt[:, :, :], in_=wgL[e * D:(e + 1) * D, :].rearrange("(k p) n -> p k n", p=P)),
                         [wgL.r], [wgt.r], dma=True)
                    b.op("pool", lambda: G.dma_start(out=wut[:, :, :], in_=wuL[e * D:(e + 1) * D, :].rearrange("(k p) n -> p k n", p=P)),
                         [wuL.r], [wut.r], dma=True)
                    b.op("pool", lambda: G.dma_start(out=wdt[:, :, :], in_=wdL[e * HID:(e + 1) * HID, :].rearrange("(k p) n -> p k n", p=P)),
                         [wdL.r], [wdt.r], dma=True)
                    for i, (c0, n, isctx) in enumerate(grp):
                        pw = psf.get()
                        b.op("pe", lambda i=i, n=n: PE.matmul(pw[:, :n], lhsT=sel[:, e * P:(e + 1) * P], rhs=wtT[i][:, :n], start=True, stop=True),
                             [sel.r, wtT[i].r], [pw.r])
                        b.op("act", lambda n=n: A.copy(out=wrow[:, :n], in_=pw[:, :n]), [pw.r], [wrow.r])
                        hs = hsp.get()
                        for j in range(4):
                            sg = sgP.get(); tt = ttP.get()
                            pg = psf.get(); pu = psf.get()
                            for k in range(8):
                                b.op("pe", lambda k=k, j=j, i=i, n=n: PE.matmul(pg[:, :n], lhsT=wgt[:, k, j * P:(j + 1) * P], rhs=h2[i][:, k, :n],
                                                                               start=(k == 0), stop=(k == 7)), [wgt.r, h2[i].r], [pg.r], inc=(k == 7))
                            for k in range(8):
                                b.op("pe", lambda k=k, j=j, i=i, n=n: PE.matmul(pu[:, :n], lhsT=wut[:, k, j * P:(j + 1) * P], rhs=h2[i][:, k, :n],
                                                                               start=(k == 0), stop=(k == 7)), [wut.r, h2[i].r], [pu.r], inc=(k == 7))
                            b.op("act", lambda n=n: A.activation(out=sg[:, :n], in_=pg[:, :n], func=AF.Silu), [pg.r], [sg.r])
                            b.op("dve", lambda n=n: V.tensor_tensor(out=tt[:, :n], in0=sg[:, :n], in1=pu[:, :n], op=ALU.mult), [sg.r, pu.r], [tt.r])
                            b.op("dve", lambda n=n, j=j: V.tensor_tensor(out=hs[:, j, :n], in0=tt[:, :n], in1=wrow[:, :n], op=ALU.mult),
                                 [tt.r, wrow.r], [hs.r])
                        for o in range(8):
                            py = psf.get()
                            for j in range(4):
                                b.op("pe", lambda o=o, j=j, n=n: PE.matmul(py[:, :n], lhsT=wdt[:, j, o * P:(o + 1) * P], rhs=hs[:, j, :n],
                                                                          start=(j == 0), stop=(j == 3)), [wdt.r, hs.r], [py.r], inc=(j == 3))
                            b.op("dve", lambda o=o, i=i, n=n: V.tensor_tensor(out=yac[i][:, o, :n], in0=yac[i][:, o, :n], in1=py[:, :n], op=ALU.add),
                                 [py.r, yac[i].r], [yac[i].r])
                for i, (c0, n, isctx) in enumerate(grp):
                    dma("sp", xin, xin[:, :, :n], resd, fm(resd, c0, n))
                    for o in range(8):
                        b.op("dve", lambda o=o, i=i, n=n, isctx=isctx: V.scalar_tensor_tensor(
                            out=xin[:, o, :n], in0=yac[i][:, o, :n], scalar=gt2[:, isctx, o:o + 1], in1=xin[:, o, :n],
                            op0=ALU.mult, op1=ALU.add), [yac[i].r, gt2.r, xin.r], [xin.r])
                    dma("sp", resd, fm(resd, c0, n), xin, xin[:, :, :n])

    def layer2(L):
        layer_scalars(L)
        tl = tiles(True)
        with Scope(b) as st:
            def S_(name, shape, dt=F32):
                return T(st.enter_context(nc.sbuf_tensor(name, list(shape), dt)))
            win = S_("lwin", [P, 8, 2 * LW], BF16)
            b.op("pool", lambda: G.dma_start(out=win[:, :, :], in_=lru_w_in[:, :].rearrange("(k p) n -> p k n", p=P)), [lru_w_in.r], [win.r], dma=True)
            xin = S_("lxin", [P, 8, 512]); sqb = S_("lsqb", [P, 8, 512])
            hT = S_("lhT", [P, 8, 512], BF16)
            gsb = S_("lgsb", [P, 10, 512], BF16); zsb = S_("lzsb", [P, 10, 512])
            t1 = S_("lt1", [P, 512]); t2 = S_("lt2", [P, 512])
            for (c0, n, isctx) in tl:
                dma("sp", xin, xin[:, :, :n], resd, fm(resd, c0, n))
                rmsnorm_mod(xin, n, isctx, g1, sh1, xin, sqb, [hT])
                for o in range(20):
                    if o < 10 and isctx:
                        continue
                    pz = psf.get()
                    for k in range(8):
                        b.op("pe", lambda o=o, k=k: PE.matmul(pz[:, :n], lhsT=win[:, k, o * P:(o + 1) * P], rhs=hT[:, k, :n],
                                                               start=(k == 0), stop=(k == 7)), [win.r, hT.r], [pz.r], inc=(k == 7))
                    if o < 10:
                        gelu_tanh(gsb[:, o, :n], gsb, pz[:, :n], pz, ((t1[:, :n], t1), (t2[:, :n], t2)), None)
                    else:
                        b.op("act", lambda o=o: A.copy(out=zsb[:, o - 10, :n], in_=pz[:, :n]), [pz.r], [zsb.r])
                if not isctx:
                    dma("sp", lru_g, lru_g[:, c0:c0 + n].rearrange("(k p) t -> p k t", p=P), gsb, gsb[:, :, :n])
                dma("sp", lru_zx, lru_zx[:, c0:c0 + n].rearrange("(k p) t -> p k t", p=P), zsb, zsb[:, :, :n])
        with Scope(b) as st:
            def S_(name, shape, dt=F32):
                return T(st.enter_context(nc.sbuf_tensor(name, list(shape), dt)))
            wa = S_("lwa", [P, 2, 10, P]); wx = S_("lwx", [P, 2, 10, P]); cw = S_("lcw", [P, 10, 4]); vec = S_("lvec", [P, 7, 10])
            nsp = S_("lnsp", [P, 2, 10])
            dma("sp", wa, wa[:, :, :, :], lru_wa, lru_wa[:, :, :, :]); dma("sp", wx, wx[:, :, :, :], lru_wx, lru_wx[:, :, :, :])
            dma("sp", cw, cw[:, :, :], lru_cw, lru_cw[:, :, :]); dma("sp", vec, vec[:, :, :], lru_vec, lru_vec[:, :, :])
            b.op("act", lambda: A.activation(out=nsp[:, :, :], in_=vec[:, 5:7, :], func=AF.Exp, scale=-1.0), [vec.r], [nsp.r])
            b.op("act", lambda: A.activation(out=nsp[:, :, :], in_=nsp[:, :, :], func=AF.Ln, bias=onec[:, 0:1], scale=1.0), [nsp.r, onec.r], [nsp.r])
            b.op("dve", lambda: V.tensor_scalar(out=nsp[:, :, :], in0=nsp[:, :, :], scalar1=-8.0, scalar2=None, op0=ALU.mult), [nsp.r], [nsp.r])
            zp = Pool([S_("lzh%d" % i, [P, 516]) for i in range(2)])
            xr = S_("lxr", [P, 512]); rr = S_("lrr", [P, 512]); ii = S_("lii", [P, 512]); aa = S_("laa", [P, 512]); uu = S_("luu", [P, 512])
            hh = S_("lhh", [P, 512]); hfp = Pool([S_("lhf%d" % i, [P, 512]) for i in range(2)])
            gtp = Pool([S_("lgt%d" % i, [P, 512], BF16) for i in range(2)]); gout = S_("lgo", [P, 512], BF16)
            carry = S_("lcar", [P, 1])
            lat_tl = [t for t in tl if not t[2]]
            for ct in range(10):
                for d in range(2):
                    order = tl if d == 0 else ([t for t in tl if t[2]] + lat_tl[::-1])
                    b.op("dve", lambda: V.memset(carry[:, :], 0.0), [], [carry.r])
                    for (c0, n, isctx) in order:
                        lo = 0 if isctx else NCTX
                        hi = NCTX if isctx else NT
                        zh = zp.get()
                        a0 = max(c0 - 2, lo); a1 = min(c0 + n + 1, hi)
                        if a0 > c0 - 2 or a1 < c0 + n + 1:
                            b.op("pool", lambda zh=zh: G.memset(zh[:, :], 0.0), [], [zh.r])
                        dma("sp", zh, zh[:, a0 - (c0 - 2):a1 - (c0 - 2)], lru_zx, lru_zx[ct * P:(ct + 1) * P, a0:a1])
                        b.op("dve", lambda zh=zh: V.tensor_scalar(out=xr[:, :n], in0=zh[:, 0:n], scalar1=cw[:, ct, 0:1], scalar2=vec[:, 0, ct:ct + 1],
                                                                  op0=ALU.mult, op1=ALU.add), [zh.r, cw.r, vec.r], [xr.r])
                        for j in range(1, 4):
                            b.op("dve", lambda zh=zh, j=j: V.scalar_tensor_tensor(out=xr[:, :n], in0=zh[:, j:j + n], scalar=cw[:, ct, j:j + 1], in1=xr[:, :n],
                                                                                  op0=ALU.mult, op1=ALU.add), [zh.r, cw.r, xr.r], [xr.r])
                        pr = psf.get(); pi = psf.get()
                        b.op("pe", lambda: PE.matmul(pr[:, :n], lhsT=wa[:, d, ct, :], rhs=xr[:, :n], start=True, stop=True), [wa.r, xr.r], [pr.r])
                        b.op("pe", lambda: PE.matmul(pi[:, :n], lhsT=wx[:, d, ct, :], rhs=xr[:, :n], start=True, stop=True), [wx.r, xr.r], [pi.r])
                        b.op("act", lambda: A.activation(out=rr[:, :n], in_=pr[:, :n], func=AF.Sigmoid, bias=vec[:, 1 + d, ct:ct + 1], scale=1.0),
                             [pr.r, vec.r], [rr.r])
                        b.op("act", lambda: A.activation(out=ii[:, :n], in_=pi[:, :n], func=AF.Sigmoid, bias=vec[:, 3 + d, ct:ct + 1], scale=1.0),
                             [pi.r, vec.r], [ii.r])
                        b.op("act", lambda: A.activation(out=aa[:, :n], in_=rr[:, :n], func=AF.Exp, scale=nsp[:, d, ct:ct + 1]), [rr.r, nsp.r], [aa.r])
                        b.op("dve", lambda: V.tensor_tensor(out=uu[:, :n], in0=aa[:, :n], in1=aa[:, :n], op=ALU.mult), [aa.r], [uu.r])
                        b.op("act", lambda: A.activation(out=uu[:, :n], in_=uu[:, :n], func=AF.Sqrt, bias=onec[:, 0:1], scale=-1.0), [uu.r, onec.r], [uu.r])
                        b.op("dve", lambda: V.tensor_tensor(out=ii[:, :n], in0=ii[:, :n], in1=xr[:, :n], op=ALU.mult), [ii.r, xr.r], [ii.r])
                        b.op("dve", lambda: V.tensor_tensor(out=uu[:, :n], in0=uu[:, :n], in1=ii[:, :n], op=ALU.mult), [uu.r, ii.r], [uu.r])

                        def r2(t):
                            ap = t[:, 0:n]
                            return AP(ap.tensor, ap.offset + (n - 1), [[ap.ap[0][0], P], [-1, n]])
                        if d == 0:
                            hf = hfp.get()
                            b.op("dve", lambda hf=hf: V.tensor_tensor_scan(out=hf[:, :n], data0=aa[:, :n], data1=uu[:, :n], initial=carry[:, 0:1],
                                                                           op0=ALU.mult, op1=ALU.add), [aa.r, uu.r, carry.r], [hf.r])
                            b.op("dve", lambda hf=hf: V.tensor_copy(out=carry[:, :], in_=hf[:, n - 1:n]), [hf.r], [carry.r])
                            if not isctx:
                                dma("sp", lru_hf, lru_hf[ct * P:(ct + 1) * P, c0:c0 + n], hf, hf[:, :n])
                        else:
                            b.op("dve", lambda: V.tensor_tensor_scan(out=r2(hh), data0=r2(aa), data1=r2(uu), initial=carry[:, 0:1],
                                                                      op0=ALU.mult, op1=ALU.add), [aa.r, uu.r, carry.r], [hh.r])
                            b.op("dve", lambda: V.tensor_copy(out=carry[:, :], in_=hh[:, 0:1]), [hh.r], [carry.r])
                            if not isctx:
                                hf = hfp.get(); gt_ = gtp.get()
                                dma("sp", hf, hf[:, :n], lru_hf, lru_hf[ct * P:(ct + 1) * P, c0:c0 + n])
                                dma("sp", gt_, gt_[:, :n], lru_g, lru_g[ct * P:(ct + 1) * P, c0:c0 + n])
                                b.op("dve", lambda hf=hf: V.tensor_tensor(out=hh[:, :n], in0=hh[:, :n], in1=hf[:, :n], op=ALU.add), [hh.r, hf.r], [hh.r])
                                b.op("dve", lambda gt_=gt_: V.tensor_tensor(out=gout[:, :n], in0=hh[:, :n], in1=gt_[:, :n], op=ALU.mult), [hh.r, gt_.r], [gout.r])
                                dma("sp", lru_gated, lru_gated[ct * P:(ct + 1) * P, c0:c0 + n], gout, gout[:, :n])
        with Scope(b) as st:
            def S_(name, shape, dt=F32):
                return T(st.enter_context(nc.sbuf_tensor(name, list(shape), dt)))
            wout = S_("lwout", [P, 10, D], BF16)
            b.op("pool", lambda: G.dma_start(out=wout[:, :, :], in_=lru_w_out[:, :].rearrange("(k p) n -> p k n", p=P)), [lru_w_out.r], [wout.r], dma=True)
            xp = Pool([S_("lcx%d" % i, [P, 8, 512]) for i in range(2)])
            gp = Pool([S_("lcg%d" % i, [P, 10, 512], BF16) for i in range(2)])
            for (c0, n, isctx) in tiles(False):
                xin = xp.get(); gg = gp.get()
                dma("sp", xin, xin[:, :, :n], resd, fm(resd, c0, n))
                dma("sp", gg, gg[:, :, :n], lru_gated, lru_gated[:, c0:c0 + n].rearrange("(k p) t -> p k t", p=P))
                for o in range(8):
                    py = psf.get()
                    for k in range(10):
                        b.op("pe", lambda o=o, k=k: PE.matmul(py[:, :n], lhsT=wout[:, k, o * P:(o + 1) * P], rhs=gg[:, k, :n],
                                                               start=(k == 0), stop=(k == 9)), [wout.r, gg.r], [py.r], inc=(k == 9))
                    b.op("dve", lambda o=o: V.scalar_tensor_tensor(out=xin[:, o, :n], in0=py[:, :n], scalar=gt1[:, 0, o:o + 1],
                                                                    in1=xin[:, o, :n], op0=ALU.mult, op1=ALU.add), [py.r, gt1.r, xin.r], [xin.r])
                dma("sp", resd, fm(resd, c0, n), xin, xin[:, :, :n])
        return tiles(False)

    def layer3(L):
        layer_scalars(L)
        tl = tiles(False)
        with Scope(b) as st:
            def S_(name, shape, dt=F32):
                return T(st.enter_context(nc.sbuf_tensor(name, list(shape), dt)))
            win = S_("fwin", [P, 8, D], BF16)
            b.op("pool", lambda: G.dma_start(out=win[:, :, :], in_=fn_w_in[:, :].rearrange("(k p) n -> p k n", p=P)), [fn_w_in.r], [win.r], dma=True)
            cs = S_("fcs", [P, 2, 512])
            dma("sp", cs, cs[:, :, :], fn_cs, fn_cs[:, :, :])
            xin = S_("fxin", [P, 8, 512]); sqb = S_("fsqb", [P, 8, 512]); hT = S_("fhT", [P, 8, 512], BF16)
            zs = S_("fzs", [P, 8, 512]); uvp = Pool([S_("fuv%d" % i, [P, 2048]) for i in range(2)])
            for (c0, n, isctx) in tl:
                dma("sp", xin, xin[:, :, :n], resd, fm(resd, c0, n))
                rmsnorm_mod(xin, n, 0, g1, sh1, xin, sqb, [hT])
                for o in range(8):
                    pz = psf.get()
                    for k in range(8):
                        b.op("pe", lambda o=o, k=k: PE.matmul(pz[:, :n], lhsT=win[:, k, o * P:(o + 1) * P], rhs=hT[:, k, :n],
                                                               start=(k == 0), stop=(k == 7)), [win.r, hT.r], [pz.r], inc=(k == 7))
                    b.op("act", lambda o=o: A.copy(out=zs[:, o, :n], in_=pz[:, :n]), [pz.r], [zs.r])
                for ci in range(n // P):
                    uv = uvp.get()
                    for g in range(4):
                        pu = psf.get()
                        for kk in range(2):
                            b.op("pe", lambda g=g, kk=kk, ci=ci: PE.matmul(pu[:, :], lhsT=zs[:, 2 * g + kk, ci * P:(ci + 1) * P], rhs=cs[:, kk, :],
                                                                            start=(kk == 0), stop=(kk == 1)), [zs.r, cs.r], [pu.r], inc=(kk == 1))
                        if g % 2 == 0:
                            b.op("act", lambda g=g, uv=uv: A.copy(out=uv[:, g * 512:(g + 1) * 512], in_=pu[:, :]), [pu.r], [uv.r])
                        else:
                            b.op("dve", lambda g=g, uv=uv: V.tensor_copy(out=uv[:, g * 512:(g + 1) * 512], in_=pu[:, :]), [pu.r], [uv.r])
                    t0 = c0 - NCTX + ci * P
                    dma("sp", fn_uv, fn_uv[t0:t0 + P, :], uv, uv[:, :])
        with Scope(b) as st:
            def S_(name, shape, dt=F32):
                return T(st.enter_context(nc.sbuf_tensor(name, list(shape), dt)))
            dft = S_("fdft", [P, 3, P]); tw = S_("ftw", [P, 2, P])
            dma("sp", dft, dft[:, :, :], fn_dft, fn_dft[:, :, :]); dma("sp", tw, tw[:, :, :], fn_tw, fn_tw[:, :, :])
            inp_ = Pool([S_("fin%d" % i, [P, 512]) for i in range(3)])
            outp = Pool([S_("fout%d" % i, [P, 512]) for i in range(3)])
            tmp = S_("ftmp", [P, 512])
            uv3 = fn_uv[:, :].rearrange("(t1 t2) c -> t1 t2 c", t2=P)
            for t2 in range(P):
                for g in range(4):
                    xi = inp_.get(); bo = outp.get()
                    dma("sp", xi, xi[:, :], fn_uv, uv3[:, t2, g * 512:(g + 1) * 512])
                    pa = psf.get()
                    b.op("pe", lambda: PE.matmul(pa[:, 0:256], lhsT=dft[:, 0, :], rhs=xi[:, 0:256], start=True, stop=False), [dft.r, xi.r], [pa.r], inc=False)
                    b.op("pe", lambda: PE.matmul(pa[:, 0:256], lhsT=dft[:, 2, :], rhs=xi[:, 256:512], start=False, stop=True), [dft.r, xi.r], [pa.r], inc=False)
                    b.op("pe", lambda: PE.matmul(pa[:, 256:512], lhsT=dft[:, 1, :], rhs=xi[:, 0:256], start=True, stop=False), [dft.r, xi.r], [pa.r], inc=False)
                    b.op("pe", lambda: PE.matmul(pa[:, 256:512], lhsT=dft[:, 0, :], rhs=xi[:, 256:512], start=False, stop=True), [dft.r, xi.r], [pa.r])
                    b.op("dve", lambda: V.tensor_scalar(out=tmp[:, 0:256], in0=pa[:, 256:512], scalar1=tw[:, 1, t2:t2 + 1], scalar2=None, op0=ALU.mult),
                         [pa.r, tw.r], [tmp.r])
                    b.op("dve", lambda: V.scalar_tensor_tensor(out=bo[:, 0:256], in0=pa[:, 0:256], scalar=tw[:, 0, t2:t2 + 1], in1=tmp[:, 0:256],
                                                                op0=ALU.mult, op1=ALU.subtract), [pa.r, tw.r, tmp.r], [bo.r])
                    b.op("dve", lambda: V.tensor_scalar(out=tmp[:, 256:512], in0=pa[:, 256:512], scalar1=tw[:, 0, t2:t2 + 1], scalar2=None, op0=ALU.mult),
                         [pa.r, tw.r], [tmp.r])
                    b.op("dve", lambda: V.scalar_tensor_tensor(out=bo[:, 256:512], in0=pa[:, 0:256], scalar=tw[:, 1, t2:t2 + 1], in1=tmp[:, 256:512],
                                                                op0=ALU.mult, op1=ALU.add), [pa.r, tw.r, tmp.r], [bo.r])
                    dma("sp", fn_b, fn_b[t2, :, g * 512:(g + 1) * 512], bo, bo[:, :])
            rp = Pool([S_("frr%d" % i, [P, D]) for i in range(2)])
            r3 = fn_r[:, :].rearrange("(k2 k1) c -> k2 k1 c", k1=P)
            for k1 in range(P):
                ro = rp.get()
                for g in range(4):
                    xi = inp_.get()
                    dma("sp", xi, xi[:, :], fn_b, fn_b[:, k1, g * 512:(g + 1) * 512])
                    pr = psf.get()
                    b.op("pe", lambda: PE.matmul(pr[:, 0:256], lhsT=dft[:, 0, :], rhs=xi[:, 0:256], start=True, stop=False), [dft.r, xi.r], [pr.r], inc=False)
                    b.op("pe", lambda: PE.matmul(pr[:, 0:256], lhsT=dft[:, 2, :], rhs=xi[:, 256:512], start=False, stop=True), [dft.r, xi.r], [pr.r])
                    b.op("act", lambda g=g, ro=ro: A.activation(out=ro[:, g * 256:(g + 1) * 256], in_=pr[:, 0:256], func=AF.Copy, scale=1.0 / 2048.0),
                         [pr.r], [ro.r])
                dma("sp", fn_r, r3[:, k1, :], ro, ro[:, :])
        with Scope(b) as st:
            def S_(name, shape, dt=F32):
                return T(st.enter_context(nc.sbuf_tensor(name, list(shape), dt)))
            wout = S_("fwout", [P, 8, D], BF16)
            b.op("pool", lambda: G.dma_start(out=wout[:, :, :], in_=fn_w_out[:, :].rearrange("(k p) n -> p k n", p=P)), [fn_w_out.r], [wout.r], dma=True)
            xp = Pool([S_("fdx%d" % i, [P, 8, 512]) for i in range(2)])
            rtp = Pool([S_("frt%d" % i, [P, D]) for i in range(2)])
            rT = S_("frT", [P, 8, 512], BF16)
            for (c0, n, isctx) in tl:
                xin = xp.get()
                dma("sp", xin, xin[:, :, :n], resd, fm(resd, c0, n))
                for ci in range(n // P):
                    rt = rtp.get()
                    t0 = c0 - NCTX + ci * P
                    dma("sp", rt, rt[:, :], fn_r, fn_r[t0:t0 + P, :])
                    for hf in range(2):
                        pt = psf.get()
                        for kq in range(4):
                            k = hf * 4 + kq
                            b.op("pe", lambda k=k, kq=kq, rt=rt: PE.transpose(out=pt[:, kq * P:(kq + 1) * P], in_=rt[:, k * P:(k + 1) * P], identity=ident[:, :]),
                                 [rt.r, ident.r], [pt.r], inc=(kq == 3))
                        b.op("act", lambda hf=hf, ci=ci: A.copy(out=rT[:, hf * 4:(hf + 1) * 4, ci * P:(ci + 1) * P],
                                                                 in_=pt[:, :].rearrange("p (k t) -> p k t", k=4)), [pt.r], [rT.r])
                for o in range(8):
                    py = psf.get()
                    for k in range(8):
                        b.op("pe", lambda o=o, k=k: PE.matmul(py[:, :n], lhsT=wout[:, k, o * P:(o + 1) * P], rhs=rT[:, k, :n],
                                                               start=(k == 0), stop=(k == 7)), [wout.r, rT.r], [py.r], inc=(k == 7))
                    b.op("dve", lambda o=o: V.scalar_tensor_tensor(out=xin[:, o, :n], in0=py[:, :n], scalar=gt1[:, 0, o:o + 1],
                                                                    in1=xin[:, o, :n], op0=ALU.mult, op1=ALU.add), [py.r, gt1.r, xin.r], [xin.r])
                dma("sp", resd, fm(resd, c0, n), xin, xin[:, :, :n])
        return tl

    def layer1(L):
        layer_scalars(L)
        tl = tiles(True)
        MLI = 3088
        with Scope(b) as st:
            def S_(name, shape, dt=F32):
                return T(st.enter_context(nc.sbuf_tensor(name, list(shape), dt)))
            win = S_("mwin", [P, 8, MLI], BF16)
            b.op("pool", lambda: G.dma_start(out=win[:, :, :], in_=ml_w_in[:, :].rearrange("(k p) n -> p k n", p=P)), [ml_w_in.r], [win.r], dma=True)
            gb = S_("mgb", [4, 4]); ngb = S_("mngb", [4, 4])
            dma("sp", gb, gb[:, :], ml_gb, ml_gb[:, :])
            b.op("dve", lambda: V.tensor_scalar(out=ngb[:, :], in0=gb[:, :], scalar1=-1.0, scalar2=None, op0=ALU.mult), [gb.r], [ngb.r])
            xin = S_("mxin", [P, 8, 512]); sqb = S_("msqb", [P, 8, 512]); hT = S_("mhT", [P, 8, 512], BF16)
            zq = S_("mzq", [P, 8, 512]); so = S_("mso", [P, 8, 512], BF16)
            vp = Pool([S_("mvv%d" % i, [P, D], BF16) for i in range(2)])
            gr = S_("mgr", [4, 4, 512])
            for (c0, n, isctx) in tl:
                dma("sp", xin, xin[:, :, :n], resd, fm(resd, c0, n))
                rmsnorm_mod(xin, n, isctx, g1, sh1, xin, sqb, [hT])
                for o in range(8):
                    pz = psf.get()
                    for k in range(8):
                        b.op("pe", lambda o=o, k=k: PE.matmul(pz[:, :n], lhsT=win[:, k, o * P:(o + 1) * P], rhs=hT[:, k, :n],
                                                               start=(k == 0), stop=(k == 7)), [win.r, hT.r], [pz.r], inc=(k == 7))
                    b.op("act", lambda o=o: A.copy(out=zq[:, o, :n], in_=pz[:, :n]), [pz.r], [zq.r])
                dma("sp", ml_zqk, fm(ml_zqk, c0, n), zq, zq[:, :, :n])
                for o in range(8):
                    pz = psf.get()
                    for k in range(8):
                        b.op("pe", lambda o=o, k=k: PE.matmul(pz[:, :n], lhsT=win[:, k, 2048 + o * P:2048 + (o + 1) * P], rhs=hT[:, k, :n],
                                                               start=(k == 0), stop=(k == 7)), [win.r, hT.r], [pz.r], inc=(k == 7))
                    b.op("act", lambda o=o: A.activation(out=so[:, o, :n], in_=pz[:, :n], func=AF.Sigmoid), [pz.r], [so.r])
                dma("sp", ml_so, fm(ml_so, c0, n), so, so[:, :, :n])
                for ci in range(n // P):
                    vv = vp.get()
                    for hf in range(2):
                        pv = psf.get()
                        for k in range(8):
                            b.op("pe", lambda k=k, hf=hf, ci=ci: PE.matmul(pv[:, :], lhsT=hT[:, k, ci * P:(ci + 1) * P],
                                                                            rhs=win[:, k, 1024 + hf * 512:1024 + (hf + 1) * 512],
                                                                            start=(k == 0), stop=(k == 7)), [win.r, hT.r], [pv.r], inc=(k == 7))
                        b.op("act", lambda hf=hf, vv=vv: A.copy(out=vv[:, hf * 512:(hf + 1) * 512], in_=pv[:, :]), [pv.r], [vv.r])
                    dma("sp", ml_v, ml_v[c0 + ci * P:c0 + (ci + 1) * P, :], vv, vv[:, :])
                for q in range(4):
                    pgt = psf.get()
                    for k in range(8):
                        b.op("pe", lambda q=q, k=k: PE.matmul(pgt[0:4, :n], lhsT=win[:, k, 3072 + 4 * q:3072 + 4 * q + 4], rhs=hT[:, k, :n],
                                                               start=(k == 0), stop=(k == 7)), [win.r, hT.r], [pgt.r], inc=(k == 7))
                    if q % 2 == 0:
                        b.op("act", lambda q=q: A.activation(out=gr[:, q, :n], in_=pgt[0:4, :n], func=AF.Identity, bias=gb[:, q:q + 1], scale=1.0),
                             [pgt.r, gb.r], [gr.r])
                    else:
                        b.op("act", lambda q=q: A.activation(out=gr[:, q, :n], in_=pgt[0:4, :n], func=AF.Exp, bias=ngb[:, q:q + 1], scale=-1.0),
                             [pgt.r, ngb.r], [gr.r])
                        b.op("act", lambda q=q: A.activation(out=gr[:, q, :n], in_=gr[:, q, :n], func=AF.Ln, bias=onec[0:4, 0:1], scale=1.0),
                             [gr.r, onec.r], [gr.r])
                        b.op("dve", lambda q=q: V.tensor_scalar(out=gr[:, q, :n], in0=gr[:, q, :n], scalar1=-1.0, scalar2=None, op0=ALU.mult), [gr.r], [gr.r])
                dma("sp", ml_g, ml_g[:, :, c0:c0 + n], gr, gr[:, :, :n])
        with Scope(b) as st:
            def S_(name, shape, dt=F32):
                return T(st.enter_context(nc.sbuf_tensor(name, list(shape), dt)))
            cw = S_("mcw", [P, 8, 4]); cb = S_("mcb", [P, 8])
            dma("sp", cw, cw[:, :, :], ml_cw, ml_cw[:, :, :]); dma("sp", cb, cb[:, :], ml_cb, ml_cb[:, :])
            zp = Pool([S_("mzh%d" % i, [P, 516]) for i in range(2)])
            xr = S_("mxr", [P, 512]); qo = Pool([S_("mqo%d" % i, [P, 512], BF16) for i in range(2)])
            for ft in range(8):
                for (c0, n, isctx) in tl:
                    lo = 0 if isctx else NCTX
                    hi = NCTX if isctx else NT
                    zh = zp.get()
                    a0 = max(c0 - 2, lo); a1 = min(c0 + n + 1, hi)
                    if a0 > c0 - 2 or a1 < c0 + n + 1:
                        b.op("pool", lambda zh=zh: G.memset(zh[:, :], 0.0), [], [zh.r])
                    dma("sp", zh, zh[:, a0 - (c0 - 2):a1 - (c0 - 2)], ml_zqk, ml_zqk[ft * P:(ft + 1) * P, a0:a1])
                    b.op("dve", lambda zh=zh: V.tensor_scalar(out=xr[:, :n], in0=zh[:, 0:n], scalar1=cw[:, ft, 0:1], scalar2=cb[:, ft:ft + 1],
                                                              op0=ALU.mult, op1=ALU.add), [zh.r, cw.r, cb.r], [xr.r])
                    for j in range(1, 4):
                        b.op("dve", lambda zh=zh, j=j: V.scalar_tensor_tensor(out=xr[:, :n], in0=zh[:, j:j + n], scalar=cw[:, ft, j:j + 1], in1=xr[:, :n],
                                                                              op0=ALU.mult, op1=ALU.add), [zh.r, cw.r, xr.r], [xr.r])
                    qq = qo.get()
                    b.op("act", lambda: A.activation(out=xr[:, :n], in_=xr[:, :n], func=AF.Silu), [xr.r], [xr.r])
                    sc_ = (128.0 ** -0.5) if ft < 4 else 1.0
                    b.op("dve", lambda qq=qq: V.tensor_scalar(out=qq[:, :n], in0=xr[:, :n], scalar1=sc_, scalar2=None, op0=ALU.mult), [xr.r], [qq.r])
                    dst = ml_q if ft < 4 else ml_k
                    f4 = ft % 4
                    dma("sp", dst, dst[f4 * P:(f4 + 1) * P, c0:c0 + n], qq, qq[:, :n])
        with Scope(b) as st:
            def S_(name, shape, dt=F32):
                return T(st.enter_context(nc.sbuf_tensor(name, list(shape), dt)))
            msk = S_("mmsk", [P, 2, P]); sel4 = S_("msel4", [4, 512]); ng = S_("mng", [P, 8])
            dma("sp", msk, msk[:, :, :], ml_mask, ml_mask[:, :, :]); dma("sp", sel4, sel4[:, :], sel_d, sel_d[0:4, 0:512])
            dma("sp", ng, ng[:, :], ml_ng, ml_ng[:, :])
            Cst = S_("mC", [P, 4, 256]); Cb = S_("mCb", [P, 4, 256], BF16); nst = S_("mn", [P, 4, P]); nb = S_("mnb", [P, 4, P], BF16)
            qTp = Pool([S_("mqT%d" % i, [P, 4, P], BF16) for i in range(2)]); kTp = Pool([S_("mkT%d" % i, [P, 4, P], BF16) for i in range(2)])
            vcp = Pool([S_("mvc%d" % i, [P, D], BF16) for i in range(2)]); grp_ = Pool([S_("mgr%d" % i, [4, 2, P]) for i in range(2)])
            rb = S_("mrb", [4, P]); rc = S_("mrc", [4, P]); rk = S_("mrk", [4, P]); Rb = S_("mRb", [4, 257]); cols = S_("mcols", [P, 8])
            bcP = Pool([S_("mbc%d" % i, [P, 257]) for i in range(2)]); DmP = Pool([S_("mDm%d" % i, [P, P]) for i in range(2)])
            StP = Pool([S_("mSt%d" % i, [P, P], BF16) for i in range(2)]); t1P = Pool([S_("mt1%d" % i, [P, P]) for i in range(2)])
            numP = Pool([S_("mnum%d" % i, [P, 2, P]) for i in range(2)]); denP = Pool([S_("mden%d" % i, [P, P]) for i in range(2)])
            kwP = Pool([S_("mkw%d" % i, [P, P], BF16) for i in range(2)])
            hop = Pool([S_("mho%d" % i, [P, 8, P]) for i in range(2)]); hfp = Pool([S_("mhf%d" % i, [P, 8, P]) for i in range(2)])
            sop = Pool([S_("msoc%d" % i, [P, 8, P], BF16) for i in range(2)]); gop = Pool([S_("mgo%d" % i, [P, 8, P], BF16) for i in range(2)])
            sq2 = S_("msq2", [P, 2, P]); rstd = S_("mrstd", [P, P])
            nchk = NT // P
            for d in range(2):
                order = list(range(nchk)) if d == 0 else ([1, 0] + list(range(nchk - 1, 1, -1)))
                b.op("dve", lambda: V.memset(Cst[:, :, :], 0.0), [], [Cst.r]); b.op("dve", lambda: V.memset(Cb[:, :, :], 0.0), [], [Cb.r])
                b.op("dve", lambda: V.memset(nst[:, :, :], 0.0), [], [nst.r]); b.op("dve", lambda: V.memset(nb[:, :, :], 0.0), [], [nb.r])

                def dv(t):
                    ap = t[:, 0:P]
                    if d == 0:
                        return ap
                    return AP(ap.tensor, ap.offset + (P - 1), [[ap.ap[0][0], 4], [-1, P]])
                for j in order:
                    c0 = j * P
                    isctx = 1 if j < 2 else 0
                    qT = qTp.get(); kT = kTp.get(); vc = vcp.get(); gq = grp_.get()
                    dma("sp", qT, qT[:, :, :], ml_q, ml_q[:, c0:c0 + P].rearrange("(h p) t -> p h t", p=P))
                    dma("sp", kT, kT[:, :, :], ml_k, ml_k[:, c0:c0 + P].rearrange("(h p) t -> p h t", p=P))
                    dma("sp", vc, vc[:, :], ml_v, ml_v[c0:c0 + P, :])
                    dma("sp", gq, gq[:, :, :], ml_g, ml_g[:, 2 * d:2 * d + 2, c0:c0 + P])
                    gidx = P - 1 if d == 0 else 0
                    lfap = gq[:, 1, :]
                    if d == 1:
                        lfap = AP(lfap.tensor, lfap.offset + (P - 1), [[lfap.ap[0][0], 4], [-1, P]])
                    b.op("dve", lambda lfap=lfap: V.tensor_tensor_scan(out=dv(rb), data0=ones[0:4, 0:P], data1=lfap,
                                                                       initial=0.0, op0=ALU.mult, op1=ALU.add), [ones.r, gq.r], [rb.r])
                    b.op("dve", lambda gq=gq: V.tensor_tensor(out=rc[:, :], in0=gq[:, 0, :], in1=rb[:, :], op=ALU.subtract), [gq.r, rb.r], [rc.r])
                    b.op("act", lambda: A.activation(out=rk[:, :], in_=rc[:, :], func=AF.Exp, bias=rb[:, gidx:gidx + 1], scale=1.0), [rc.r, rb.r], [rk.r])
                    b.op("dve", lambda: V.tensor_copy(out=Rb[:, 0:P], in_=rb[:, :]), [rb.r], [Rb.r])
                    b.op("act", lambda: A.activation(out=Rb[:, P:2 * P], in_=rb[:, :], func=AF.Exp), [rb.r], [Rb.r])
                    b.op("act", lambda: A.activation(out=Rb[:, 256:257], in_=rb[:, gidx:gidx + 1], func=AF.Exp), [rb.r], [Rb.r])
                    pT = psf.get()
                    b.op("pe", lambda: PE.transpose(out=pT[:, 0:4], in_=rc[:, :], identity=ident[0:4, 0:4]), [rc.r, ident.r], [pT.r], inc=False)
                    b.op("pe", lambda: PE.transpose(out=pT[:, 4:8], in_=rk[:, :], identity=ident[0:4, 0:4]), [rk.r, ident.r], [pT.r])
                    b.op("dve", lambda: V.tensor_copy(out=cols[:, :], in_=pT[:, 0:8]), [pT.r], [cols.r])
                    ho = hop.get()
                    for h in range(4):
                        bc = bcP.get(); Dm = DmP.get(); St = StP.get(); t1 = t1P.get(); num = numP.get(); den = denP.get(); kw = kwP.get()
                        pbc = psf.get()
                        b.op("pe", lambda h=h: PE.matmul(pbc[:, 0:257], lhsT=sel4[:, h * P:(h + 1) * P], rhs=Rb[:, :], start=True, stop=True),
                             [sel4.r, Rb.r], [pbc.r])
                        b.op("act", lambda: A.copy(out=bc[:, :], in_=pbc[:, 0:257]), [pbc.r], [bc.r])
                        pst = psf.get()
                        b.op("pe", lambda h=h: PE.matmul(pst[:, 0:P], lhsT=kT[:, h, :], rhs=qT[:, h, :], start=True, stop=True), [kT.r, qT.r], [pst.r])
                        b.op("act", lambda h=h: A.activation(out=Dm[:, :], in_=bc[:, 0:P], func=AF.Exp, bias=cols[:, h:h + 1], scale=1.0),
                             [bc.r, cols.r], [Dm.r])
                        b.op("dve", lambda: V.tensor_tensor(out=Dm[:, :], in0=Dm[:, :], in1=msk[:, d, :], op=ALU.mult), [Dm.r, msk.r], [Dm.r])
                        b.op("dve", lambda: V.tensor_tensor(out=St[:, :], in0=Dm[:, :], in1=pst[:, 0:P], op=ALU.mult), [Dm.r, pst.r], [St.r])
                        for vt in range(2):
                            pn = psf.get()
                            b.op("pe", lambda h=h, vt=vt: PE.matmul(pn[:, 0:P], lhsT=vc[:, h * 256 + vt * P:h * 256 + (vt + 1) * P], rhs=St[:, :],
                                                                     start=True, stop=True), [vc.r, St.r], [pn.r], inc=False)
                            b.op("pe", lambda h=h, vt=vt: PE.matmul(pn[:, P:2 * P], lhsT=Cb[:, h, vt * P:(vt + 1) * P], rhs=qT[:, h, :],
                                                                     start=True, stop=True), [Cb.r, qT.r], [pn.r])
                            b.op("dve", lambda: V.tensor_tensor(out=t1[:, :], in0=pn[:, P:2 * P], in1=bc[:, P:2 * P], op=ALU.mult), [pn.r, bc.r], [t1.r])
                            b.op("dve", lambda vt=vt: V.tensor_tensor(out=num[:, vt, :], in0=t1[:, :], in1=pn[:, 0:P], op=ALU.add), [t1.r, pn.r], [num.r])
                        pd = psf.get()
                        b.op("pe", lambda: PE.matmul(pd[:, 0:P], lhsT=onesb[:, :], rhs=St[:, :], start=True, stop=True), [onesb.r, St.r], [pd.r], inc=False)
                        b.op("pe", lambda h=h: PE.matmul(pd[:, P:2 * P], lhsT=nb[:, h, :], rhs=qT[:, h, :], start=True, stop=True), [nb.r, qT.r], [pd.r])
                        b.op("dve", lambda: V.tensor_tensor(out=den[:, :], in0=pd[:, P:2 * P], in1=bc[:, P:2 * P], op=ALU.mult), [pd.r, bc.r], [den.r])
                        b.op("dve", lambda: V.tensor_tensor(out=den[:, :], in0=den[:, :], in1=pd[:, 0:P], op=ALU.add), [den.r, pd.r], [den.r])
                        b.op("dve", lambda: V.tensor_scalar(out=t1[:, :], in0=den[:, :], scalar1=-1.0, scalar2=None, op0=ALU.mult), [den.r], [t1.r])
                        b.op("dve", lambda: V.tensor_tensor(out=den[:, :], in0=den[:, :], in1=t1[:, :], op=ALU.max), [den.r, t1.r], [den.r])
                        b.op("dve", lambda: V.tensor_scalar(out=den[:, :], in0=den[:, :], scalar1=1.0, scalar2=None, op0=ALU.max), [den.r], [den.r])
                        b.op("dve", lambda: V.reciprocal(out=den[:, :], in_=den[:, :]), [den.r], [den.r])
                        for vt in range(2):
                            b.op("dve", lambda h=h, vt=vt: V.tensor_tensor(out=ho[:, 2 * h + vt, :], in0=num[:, vt, :], in1=den[:, :], op=ALU.mult),
                                 [num.r, den.r], [ho.r])
                        ptk = psb.get()
                        b.op("pe", lambda h=h: PE.transpose(out=ptk[:, 0:P], in_=kT[:, h, :], identity=identb[:, :]), [kT.r, identb.r], [ptk.r])
                        b.op("dve", lambda h=h: V.tensor_scalar(out=kw[:, :], in0=ptk[:, 0:P], scalar1=cols[:, 4 + h:5 + h], scalar2=None, op0=ALU.mult),
                             [ptk.r, cols.r], [kw.r])
                        pC = psf.get()
                        b.op("pe", lambda h=h: PE.matmul(pC[:, 0:256], lhsT=kw[:, :], rhs=vc[:, h * 256:(h + 1) * 256], start=True, stop=True),
                             [kw.r, vc.r], [pC.r], inc=False)
                        b.op("pe", lambda: PE.matmul(pC[:, 256:384], lhsT=kw[:, :], rhs=onesb[:, :], start=True, stop=True), [kw.r, onesb.r], [pC.r])
                        b.op("dve", lambda h=h: V.scalar_tensor_tensor(out=Cst[:, h, :], in0=Cst[:, h, :], scalar=bc[:, 256:257], in1=pC[:, 0:256],
                                                                        op0=ALU.mult, op1=ALU.add), [Cst.r, bc.r, pC.r], [Cst.r])
                        b.op("dve", lambda h=h: V.scalar_tensor_tensor(out=nst[:, h, :], in0=nst[:, h, :], scalar=bc[:, 256:257], in1=pC[:, 256:384],
                                                                        op0=ALU.mult, op1=ALU.add), [nst.r, bc.r, pC.r], [nst.r])
                        b.op("act", lambda h=h: A.copy(out=Cb[:, h, :], in_=Cst[:, h, :]), [Cst.r], [Cb.r])
                        b.op("act", lambda h=h: A.copy(out=nb[:, h, :], in_=nst[:, h, :]), [nst.r], [nb.r])
                    if d == 0:
                        dma("sp", ml_hf, ml_hf[:, c0:c0 + P].rearrange("(k p) t -> p k t", p=P), ho, ho[:, :, :])
                    else:
                        hf = hfp.get(); soc = sop.get(); go = gop.get()
                        dma("sp", hf, hf[:, :, :], ml_hf, ml_hf[:, c0:c0 + P].rearrange("(k p) t -> p k t", p=P))
                        dma("sp", soc, soc[:, :, :], ml_so, ml_so[:, c0:c0 + P].rearrange("(k p) t -> p k t", p=P))
                        b.op("dve", lambda: V.tensor_tensor(out=ho[:, :, :], in0=ho[:, :, :], in1=hf[:, :, :], op=ALU.add), [ho.r, hf.r], [ho.r])
                        for h in range(4):
                            b.op("act", lambda h=h: A.activation(out=sq2[:, :, :], in_=ho[:, 2 * h:2 * h + 2, :], func=AF.Square), [ho.r], [sq2.r])
                            pss = psf.get()
                            for vt in range(2):
                                b.op("pe", lambda vt=vt: PE.matmul(pss[:, 0:P], lhsT=ones[:, :], rhs=sq2[:, vt, :], start=(vt == 0), stop=(vt == 1)),
                                     [ones.r, sq2.r], [pss.r], inc=(vt == 1))
                            b.op("act", lambda: A.activation(out=rstd[:, :], in_=pss[:, 0:P], func=AF.Sqrt, bias=epsc[:, 0:1], scale=1.0 / 256.0),
                                 [pss.r, epsc.r], [rstd.r])
                            b.op("dve", lambda: V.reciprocal(out=rstd[:, :], in_=rstd[:, :]), [rstd.r], [rstd.r])
                            for vt in range(2):
                                kk = 2 * h + vt
                                b.op("dve", lambda kk=kk: V.scalar_tensor_tensor(out=ho[:, kk, :], in0=ho[:, kk, :], scalar=ng[:, kk:kk + 1], in1=rstd[:, :],
                                                                                 op0=ALU.mult, op1=ALU.mult), [ho.r, ng.r, rstd.r], [ho.r])
                        b.op("dve", lambda: V.tensor_tensor(out=go[:, :, :], in0=ho[:, :, :], in1=soc[:, :, :], op=ALU.mult), [ho.r, soc.r], [go.r])
                        dma("sp", ml_gated, ml_gated[:, c0:c0 + P].rearrange("(k p) t -> p k t", p=P), go, go[:, :, :])
        with Scope(b) as st:
            def S_(name, shape, dt=F32):
                return T(st.enter_context(nc.sbuf_tensor(name, list(shape), dt)))
            wout = S_("mwout", [P, 8, D], BF16)
            b.op("pool", lambda: G.dma_start(out=wout[:, :, :], in_=ml_w_out[:, :].rearrange("(k p) n -> p k n", p=P)), [ml_w_out.r], [wout.r], dma=True)
            xp = Pool([S_("mdx%d" % i, [P, 8, 512]) for i in range(2)])
            gp = Pool([S_("mdg%d" % i, [P, 8, 512], BF16) for i in range(2)])
            for (c0, n, isctx) in tl:
                xin = xp.get(); gg = gp.get()
                dma("sp", xin, xin[:, :, :n], resd, fm(resd, c0, n))
                dma("sp", gg, gg[:, :, :n], ml_gated, fm(ml_gated, c0, n))
                for o in range(8):
                    py = psf.get()
                    for k in range(8):
                        b.op("pe", lambda o=o, k=k: PE.matmul(py[:, :n], lhsT=wout[:, k, o * P:(o + 1) * P], rhs=gg[:, k, :n],
                                                               start=(k == 0), stop=(k == 7)), [wout.r, gg.r], [py.r], inc=(k == 7))
                    b.op("dve", lambda o=o: V.scalar_tensor_tensor(out=xin[:, o, :n], in0=py[:, :n], scalar=gt1[:, isctx, o:o + 1],
                                                                    in1=xin[:, o, :n], op0=ALU.mult, op1=ALU.add), [py.r, gt1.r, xin.r], [xin.r])
                dma("sp", resd, fm(resd, c0, n), xin, xin[:, :, :n])
        return tl

    def layer0(L):
        layer_scalars(L)
        tl = tiles(True)
        with Scope(b) as st:
            def S_(name, shape, dt=F32):
                return T(st.enter_context(nc.sbuf_tensor(name, list(shape), dt)))
            moe_begin(m, L, sum(t[1] for t in tl))
            win = S_("cmwin", [P, 8, 2 * D], BF16)
            wout = S_("cmwout", [P, 8, D], BF16)
            wsT = S_("cmwsT", [P, 4, P], BF16)
            vg = S_("cmvg", [P, D]); bs = S_("cmbs", [P, 4, 512])
            b.op("pool", lambda: G.dma_start(out=win[:, :, :], in_=cm_w_in[:, :].rearrange("(k p) n -> p k n", p=P)), [cm_w_in.r], [win.r], dma=True)
            b.op("pool", lambda: G.dma_start(out=wout[:, :, :], in_=cm_w_out[:, :].rearrange("(k p) n -> p k n", p=P)), [cm_w_out.r], [wout.r], dma=True)
            b.op("pool", lambda: G.dma_start(out=wsT[:, :, :], in_=cm_wsT[:, :, :]), [cm_wsT.r], [wsT.r], dma=True)
            dma("sp", vg, vg[:, :], cm_vg_rep, cm_vg_rep[:, :])
            dma("sp", bs, bs[:, :, :], cm_bs_rep, cm_bs_rep[:, :, :])
            xp = Pool([S_("xin%d" % i, [P, 8, 512]) for i in range(2)])
            sqb = S_("sqb", [P, 8, 512]); tb = S_("tb", [P, 8, 512])
            hT = S_("hT", [P, 8, 512], BF16)
            u = S_("u", [P, 8, 512], BF16)
            vt = S_("vt", [P, 1024]); vn = S_("vn", [P, 4, 1024], BF16)
            gated = S_("gated", [P, 8, 512], BF16)
            h2b = S_("h2b", [P, 8, 512], BF16)
            rowsp = Pool([S_("rows%d" % i, [P, D], BF16) for i in range(2)])
            t1 = S_("gsc1", [P, 512]); t2 = S_("gsc2", [P, 512])
            vss = S_("vss", [P, 2]); junk = S_("junk", [P, 1024])
            for (c0, n, isctx) in tl:
                xin = xp.get()
                dma("sp", xin, xin[:, :, :n], resd, fm(resd, c0, n))
                rmsnorm_mod(xin, n, isctx, g1, sh1, tb, sqb, [hT])
                for o in range(8):
                    pu = psf.get()
                    for k in range(8):
                        b.op("pe", lambda o=o, k=k: PE.matmul(pu[:, :n], lhsT=win[:, k, o * P:(o + 1) * P], rhs=hT[:, k, :n],
                                                               start=(k == 0), stop=(k == 7)), [win.r, hT.r], [pu.r], inc=(k == 7))
                    gelu_tanh(u[:, o, :n], u, pu[:, :n], pu, ((t1[:, :n], t1), (t2[:, :n], t2)), None)
                for ci in range(n // P):
                    for hf in range(2):
                        pv = psf.get()
                        for k in range(8):
                            b.op("pe", lambda k=k, hf=hf, ci=ci: PE.matmul(pv[:, :], lhsT=hT[:, k, ci * P:(ci + 1) * P],
                                                                            rhs=win[:, k, D + hf * 512:D + (hf + 1) * 512],
                                                                            start=(k == 0), stop=(k == 7)), [win.r, hT.r], [pv.r], inc=(k == 7))
                        gelu_tanh(vt[:, hf * 512:(hf + 1) * 512], vt, pv[:, :], pv, ((t1[:, :], t1), (t2[:, :], t2)), None)
                    b.op("act", lambda: A.activation(out=junk[:, :], in_=vt[:, :], func=AF.Square), [vt.r], [junk.r])
                    b.op("dve", lambda: V.reduce_sum(out=vss[:, 0:1], in_=junk[:, :], axis=AX.X), [junk.r], [vss.r])
                    b.op("act", lambda: A.activation(out=vss[:, 1:2], in_=vss[:, 0:1], func=AF.Sqrt, bias=epsc[:, 0:1], scale=1.0 / D),
                         [vss.r, epsc.r], [vss.r])
                    b.op("dve", lambda: V.reciprocal(out=vss[:, 1:2], in_=vss[:, 1:2]), [vss.r], [vss.r])
                    b.op("dve", lambda ci=ci: V.scalar_tensor_tensor(out=vn[:, ci, :], in0=vt[:, :], scalar=vss[:, 1:2], in1=vg[:, :],
                                                                      op0=ALU.mult, op1=ALU.mult), [vt.r, vss.r, vg.r], [vn.r], ss=True)
                for c in range(8):
                    psm = psf.get()
                    for ci in range(n // P):
                        b.op("pe", lambda c=c, ci=ci: PE.matmul(psm[:, ci * P:(ci + 1) * P], lhsT=vn[:, ci, c * P:(c + 1) * P], rhs=wsT[:, c // 2, :],
                                                                 start=True, stop=True), [vn.r, wsT.r], [psm.r], inc=(ci == n // P - 1))
                    b.op("dve", lambda c=c: V.tensor_tensor(out=t1[:, :n], in0=psm[:, :n], in1=bs[:, c // 2, :n], op=ALU.add), [psm.r, bs.r], [t1.r])
                    b.op("dve", lambda c=c: V.tensor_tensor(out=gated[:, c, :n], in0=t1[:, :n], in1=u[:, c, :n], op=ALU.mult), [t1.r, u.r], [gated.r])
                for o in range(8):
                    py = psf.get()
                    for k in range(8):
                        b.op("pe", lambda o=o, k=k: PE.matmul(py[:, :n], lhsT=wout[:, k, o * P:(o + 1) * P], rhs=gated[:, k, :n],
                                                               start=(k == 0), stop=(k == 7)), [wout.r, gated.r], [py.r], inc=(k == 7))
                    b.op("dve", lambda o=o: V.scalar_tensor_tensor(out=xin[:, o, :n], in0=py[:, :n], scalar=gt1[:, isctx, o:o + 1],
                                                                    in1=xin[:, o, :n], op0=ALU.mult, op1=ALU.add), [py.r, gt1.r, xin.r], [xin.r])
                dma("sp", resd, fm(resd, c0, n), xin, xin[:, :, :n])
                continue
                rmsnorm_mod(xin, n, isctx, g2, sh2, tb, sqb, [sqb, h2b])
                for ci in range(n // P):
                    ch = c0 // P + ci
                    moe_route_chunk(m, sqb, ci * P, ch)
                    moe_rows_chunk(m, h2b, ci * P, c0 + ci * P, rowsp.get())
            if dump_res:
                dbg.extend([("modall", modall, modall[:, :, :, :], [P, DEPTH, 48, 2], F32), ("g1", g1, g1[:, :, :], [P, 2, 8], F32),
                            ("hT", hT, hT[:, :, :], [P, 8, 512], BF16), ("u", u, u[:, :, :], [P, 8, 512], BF16),
                            ("vn", vn, vn[:, :, :], [P, 4, 1024], BF16), ("gated", gated, gated[:, :, :], [P, 8, 512], BF16),
                            ("tb", tb, tb[:, :, :], [P, 8, 512], F32), ("sqb", sqb, sqb[:, :, :], [P, 8, 512], F32),
                            ("sc", sc, sc[:, :, :], [P, 8, 2], F32),
                            ("vt", vt, vt[:, :], [P, 1024], F32), ("junk", junk, junk[:, :], [P, 1024], F32), ("vss", vss, vss[:, :], [P, 2], F32),
                            ("vg", vg, vg[:, :], [P, 1024], F32)])
                emit_dbg()
        return tl

    for L in layers:
        if L == 0:
            tl = layer0(L)
            if stage >= 5:
                moe_dense(L, tl)
        elif L == 1:
            tl = layer1(L)
            if stage >= 5:
                moe_dense(L, tl)
        elif L == 3:
            tl = layer3(L)
            if stage >= 5:
                moe_dense(L, tl)
        elif L == 2:
            tl = layer2(L)
            if stage >= 5:
                moe_dense(L, tl)
        else:
            raise NotImplementedError("mixer for layer %d not implemented yet" % L)

    with Scope(b) as st:
        fa = Pool([T(st.enter_context(nc.sbuf_tensor("fa%d" % i, [P, 8, 512], F32))) for i in range(2)])
        if out_res and not dump_res:
            for c0 in range(0, NT, 512):
                n = min(512, NT - c0)
                a = fa.get()
                dma("sp", a, a[:, :, :n], resd, fm(resd, c0, n))
                dma("sp", res_out, fm(res_out, c0, n), a, a[:, :, :n])
        if dump_res:
            dma("sp", info_out, info_out[:, :, :], m.info, m.info[:, :, :])
            dma("sp", dest_out, dest_out[:, :, :], m.dest, m.dest[:, :, :])
            dma("sp", carry_out, carry_out[:, :], m.carry, m.carry[:, :])
            for c0 in range(0, NT, 512):
                n = min(512, NT - c0)
                a = fa.get()
                dma("sp", a, a[:, :, :n], resd, fm(resd, c0, n))
                dma("sp", res_out, fm(res_out, c0, n), a, a[:, :, :n])
        if final:
            fsq = T(st.enter_context(nc.sbuf_tensor("fsq", [P, 8, 512], F32)))
            ftb = T(st.enter_context(nc.sbuf_tensor("ftb", [P, 8, 512], F32)))
            fsc = T(st.enter_context(nc.sbuf_tensor("fsc", [P, 2, 8], F32)))
            fzero = T(st.enter_context(nc.sbuf_tensor("fzero", [P, 2, 8], F32)))
            b.op("dve", lambda: V.memset(fzero[:, :, :], 0.0), [], [fzero.r])
            b.op("dve", lambda: V.tensor_copy(out=fsc[:, 0, :], in_=nfin[:, :]), [nfin.r], [fsc.r])
            for c0 in range(0, SEQ, 512):
                a = fa.get()
                dma("sp", a, a[:, :, :], resd, fm(resd, NCTX + c0, 512))
                rmsnorm_mod(a, 512, 0, fsc, fzero, ftb, fsq, [fsq])
                dma("sp", outT, fm(outT, c0, 512), fsq, fsq[:, :, :])
    b.finish("sp")
    return nc, b


def _pos_embedding_T():
    rows = SEQ // 64
    r = np.repeat(np.arange(rows, dtype=np.float32), 64)
    col = np.tile(np.arange(64, dtype=np.float32), rows)
    q = D // 4
    freq = np.exp(np.float32(-math.log(10000.0)) * np.arange(q, dtype=np.float32) / np.float32(q)).astype(np.float32)
    ar = r[:, None] * freq
    ac = col[:, None] * freq
    pe = np.concatenate([np.sin(ar), np.cos(ar), np.sin(ac), np.cos(ac)], axis=-1).astype(np.float32)
    return np.ascontiguousarray(pe.T)


def _fp(v, n):
    return np.ascontiguousarray(np.asarray(v, np.float32).reshape(n, P).T)


def host_prep(inp):
    f = lambda a: np.ascontiguousarray(np.asarray(a, np.float32))
    m = {}
    m["xT"] = f(inp["x"][0].T)
    m["posT"] = _pos_embedding_T()
    m["ctxT"] = f(inp["ctx"][0].T)
    m["cT"] = f(np.stack([_fp(inp["c"][0], 8), _fp(inp["c_ctx"], 8)], axis=-1))
    for l in range(DEPTH):
        m["ada_w%d" % l] = f(inp["ada_w"][l])
    m["ada_bT"] = f(np.stack([_fp(inp["ada_b"][l], 48) for l in range(DEPTH)]))
    m["nmixT"] = f(np.stack([_fp(inp["norm_mix_g"][l], 8) for l in range(DEPTH)]))
    m["nffnT"] = f(np.stack([_fp(inp["norm_ffn_g"][l], 8) for l in range(DEPTH)]))
    m["nfinT"] = _fp(inp["final_norm_g"], 8)
    m["rw"] = f(np.concatenate([inp["router_group_w"], inp["router_expert_w"]], axis=-1))
    rb = np.concatenate([inp["router_group_b"], inp["router_expert_b"]], axis=-1)
    m["rbrep"] = f(np.broadcast_to(rb[:, None, :], (DEPTH, P, 36)))
    for l in range(DEPTH):
        m["wg%d" % l] = f(inp["expert_w_gate"][l]).reshape(NEXP * D, HID)
        m["wu%d" % l] = f(inp["expert_w_up"][l]).reshape(NEXP * D, HID)
        m["wd%d" % l] = f(inp["expert_w_down"][l]).reshape(NEXP * HID, D)
    m["cm_w_in"] = f(inp["cm_w_in"][0])
    m["cm_vg_rep"] = f(np.broadcast_to(inp["cm_v_norm_g"][0][None, :], (P, D)))
    m["cm_wsT"] = f(np.transpose(inp["cm_w_s"][0], (2, 0, 1)))
    m["cm_bs_rep"] = f(np.broadcast_to(np.tile(inp["cm_b_s"][0], (1, 4))[None], (P, 4, 512)))
    m["cm_w_out"] = f(inp["cm_w_out"][0])
    m["lru_w_in"] = f(inp["lru_w_in"][0]); m["lru_w_out"] = f(inp["lru_w_out"][0])
    m["lru_wa"] = f(np.transpose(inp["lru_w_a"][0], (2, 0, 1, 3)))
    m["lru_wx"] = f(np.transpose(inp["lru_w_x"][0], (2, 0, 1, 3)))
    m["lru_cw"] = f(inp["lru_conv_w"][0].T.reshape(10, P, 4).transpose(1, 0, 2))
    vecs = [inp["lru_conv_b"][0], inp["lru_b_a"][0][0], inp["lru_b_a"][0][1], inp["lru_b_x"][0][0], inp["lru_b_x"][0][1],
            inp["lru_lambda"][0][0], inp["lru_lambda"][0][1]]
    m["lru_vec"] = f(np.stack([_fp(v, 10) for v in vecs], axis=1))
    m["fn_w_in"] = f(inp["fn_w_in"][0]); m["fn_w_out"] = f(inp["fn_w_out"][0])
    cc = np.arange(256, dtype=np.float64)
    ph = 2 * np.pi * np.outer(cc, cc) / 256.0
    csm = np.concatenate([np.cos(ph), np.sin(ph)], axis=1).astype(np.float32)
    m["fn_cs"] = f(csm.reshape(2, P, 512).transpose(1, 0, 2))
    kk = np.arange(P, dtype=np.float64)
    p128 = 2 * np.pi * np.outer(kk, kk) / 128.0
    m["fn_dft"] = f(np.stack([np.cos(p128), np.sin(p128), -np.sin(p128)], axis=1))
    ptw = 2 * np.pi * np.outer(kk, kk) / float(SEQ)
    m["fn_tw"] = f(np.stack([np.cos(ptw), np.sin(ptw)], axis=1))
    m["ml_w_in"] = f(inp["ml_w_in"][0]); m["ml_w_out"] = f(inp["ml_w_out"][0])
    m["ml_gb"] = f(inp["ml_gate_b"][0].reshape(4, 4).T)
    m["ml_cw"] = f(inp["ml_conv_w"][0].T.reshape(8, P, 4).transpose(1, 0, 2))
    m["ml_cb"] = _fp(inp["ml_conv_b"][0], 8)
    m["ml_mask"] = f(np.stack([np.triu(np.ones((P, P), np.float32)), np.tril(np.ones((P, P), np.float32))], axis=1))
    m["ml_ng"] = _fp(inp["ml_norm_g"][0], 8)
    m["ident"] = np.eye(P, dtype=np.float32)
    m["ustrict"] = np.triu(np.ones((P, P), np.float32), 1)
    m["iota32"] = f(np.broadcast_to(np.arange(32, dtype=np.float32)[None], (P, 32)))
    nbmax = (2 * NT + NEXP * 127 + 127) // 128
    m["blkpos"] = f(np.broadcast_to((np.arange(nbmax, dtype=np.float32) * 128)[None], (P, nbmax)))
    m["piota"] = np.arange(P, dtype=np.float32)[:, None].copy()
    selm = np.zeros((32, NEXP, P), np.float32)
    selm[np.arange(32), np.arange(32), :] = 1.0
    m["sel"] = selm.reshape(32, NEXP * P)
    return m


def kernel(**inputs):
    m = host_prep(inputs)
    nc, b = build(layers=(0, 1, 2, 3), in_res=False, final=True)
    im = {k: v for k, v in m.items() if k in b.in_names}
    r = run_bass_kernel_spmd(nc, [im], core_ids=[0]).results[0]
    return np.ascontiguousarray(r["outT"].T)[None].astype(np.float32)
```

```python
import math
from contextlib import ExitStack
import numpy as np
import concourse.bass as bass
import concourse.mybir as mybir
from concourse.bass_utils import run_bass_kernel_spmd
from concourse.ap import AP

F32 = mybir.dt.float32
BF16 = mybir.dt.bfloat16
I32 = mybir.dt.int32
AF = mybir.ActivationFunctionType
ALU = mybir.AluOpType
AX = mybir.AxisListType

D = 1024
SEQ = 16384
NCTX = 256
NT = SEQ + NCTX
DEPTH = 4
EPS = 1e-6
NEXP = 32
HID = 512
P = 128


class Res:
    __slots__ = ("w", "r")

    def __init__(self):
        self.w = None
        self.r = {}


class B:
    NDS = 28

    def __init__(self, nc):
        self.nc = nc
        self.engs = {"sp": nc.sync, "act": nc.scalar, "dve": nc.vector, "pool": nc.gpsimd, "pe": nc.tensor}
        self.csem = {e: nc.alloc_semaphore(name="c_" + e) for e in ("act", "dve", "pool", "pe")}
        self.ccnt = {e: 0 for e in self.csem}
        self.dsem = [nc.alloc_semaphore(name="d%d" % i) for i in range(self.NDS)]
        self.dcnt = [0] * self.NDS
        self.dnext = 0
        self.seen = {e: {} for e in self.engs}
        self.ninst = 0

    def _wait(self, e, tok):
        sem, val = tok
        k = id(sem)
        if self.seen[e].get(k, 0) >= val:
            return
        self.engs[e].wait_ge(sem, val)
        self.seen[e][k] = val
        self.ninst += 1

    def op(self, e, fn, reads=(), writes=(), dma=False, inc=True, ss=False, fence=None):
        toks = []
        for r in reads:
            if r.w is not None:
                toks.append(r.w)
        for w in writes:
            if w.w is not None:
                toks.append(w.w)
            toks.extend(w.r.values())
        own = self.csem.get(e)
        for t in toks:
            if (not dma) and t[0] is own and e == "pe":
                continue
            self._wait(e, t)
        self.ninst += 1
        if dma:
            j = self.dnext
            self.dnext = (j + 1) % self.NDS
            if self.dcnt[j] > 0:
                self._wait(e, (self.dsem[j], 16 * self.dcnt[j]))
            ins = fn()
            self.dcnt[j] += 1
            ins.then_inc(self.dsem[j], 16)
            tok = (self.dsem[j], 16 * self.dcnt[j])
        else:
            ins = fn()
            if not inc:
                return None
            if fence is not None:
                ins = fence()
                self.ninst += 1
            self.ccnt[e] += 1
            ins.then_inc(self.csem[e], 1)
            tok = (self.csem[e], self.ccnt[e])
        for r in reads:
            k = id(tok[0])
            o = r.r.get(k)
            if o is None or o[1] < tok[1]:
                r.r[k] = tok
        for w in writes:
            w.w = tok
            w.r = {}
        return tok

    ROT = 30000

    def barrier(self):
        for e in self.engs:
            self.finish(e)
        for e in list(self.csem):
            if self.ccnt[e] > self.ROT:
                self.epoch = getattr(self, "epoch", 0) + 1
                self.csem[e] = self.nc.alloc_semaphore(name="c_%s_%d" % (e, self.epoch))
                self.ccnt[e] = 0

    def finish(self, e="sp"):
        for j in range(self.NDS):
            if self.dcnt[j]:
                self._wait(e, (self.dsem[j], 16 * self.dcnt[j]))
        for k, s in self.csem.items():
            if self.ccnt[k]:
                self._wait(e, (s, self.ccnt[k]))


class Scope(ExitStack):
    def __init__(self, b):
        super().__init__()
        self._b = b

    def __exit__(self, *a):
        self._b.barrier()
        return super().__exit__(*a)


class T:
    def __init__(self, h, nres=1):
        self.h = h
        self.res = [Res() for _ in range(nres)]

    def __getitem__(self, k):
        return self.h[k]

    @property
    def r(self):
        return self.res[0]


class Pool:
    def __init__(self, tiles):
        self.tiles = tiles
        self.i = 0

    def get(self):
        t = self.tiles[self.i % len(self.tiles)]
        self.i += 1
        return t


def build(layers=(0, 1, 2, 3), in_res=False, dump_res=False, final=True, stage=9, ntl=None, out_res=False):
    nc = bass.Bass("TRN2", target_bir_lowering=False)
    b = B(nc)
    es = ExitStack()

    in_names = []
    b.in_names = in_names

    def dram_in(name, shape, dt=F32):
        in_names.append(name)
        return T(nc.dram_tensor(name, list(shape), dt, kind="ExternalInput").ap())

    def dram_tmp(name, shape, dt=F32):
        return T(nc.dram_tensor(name, list(shape), dt).ap())

    def sb(name, shape, dt=F32, stack=None):
        return T(nc.alloc_sbuf_tensor("s_" + name, list(shape), dt))

    def ps(name, shape, dt=F32):
        return T(nc.alloc_psum_tensor(name, list(shape), dt))

    if not in_res:
        xT = dram_in("xT", [D, SEQ])
        posT = dram_in("posT", [D, SEQ])
        ctxT = dram_in("ctxT", [D, NCTX])
    cT = dram_in("cT", [P, 8, 2])
    ada_w = {l: dram_in("ada_w%d" % l, [D, 6 * D]) for l in layers}
    ada_bT = dram_in("ada_bT", [DEPTH, P, 48])
    nmixT = dram_in("nmixT", [DEPTH, P, 8])
    nffnT = dram_in("nffnT", [DEPTH, P, 8])
    nfinT = dram_in("nfinT", [P, 8])
    rw = dram_in("rw", [DEPTH, D, 36])
    rbrep = dram_in("rbrep", [DEPTH, P, 36])
    if stage >= 5:
        wg = {l: dram_in("wg%d" % l, [NEXP * D, HID]) for l in layers}
        wu = {l: dram_in("wu%d" % l, [NEXP * D, HID]) for l in layers}
        wd = {l: dram_in("wd%d" % l, [NEXP * HID, D]) for l in layers}
    dbg = []
    if 0 in layers:
        cm_w_in = dram_in("cm_w_in", [D, 2 * D])
        cm_vg_rep = dram_in("cm_vg_rep", [P, D])
        cm_wsT = dram_in("cm_wsT", [P, 4, P])
        cm_bs_rep = dram_in("cm_bs_rep", [P, 4, 512])
        cm_w_out = dram_in("cm_w_out", [D, D])
    if 3 in layers:
        fn_w_in = dram_in("fn_w_in", [D, D]); fn_w_out = dram_in("fn_w_out", [D, D])
        fn_cs = dram_in("fn_cs", [P, 2, 512]); fn_dft = dram_in("fn_dft", [P, 3, P]); fn_tw = dram_in("fn_tw", [P, 2, P])
        fn_uv = dram_tmp("fn_uv", [SEQ, 2048]); fn_b = dram_tmp("fn_b", [P, P, 2048]); fn_r = dram_tmp("fn_r", [SEQ, D])
    if 1 in layers:
        ml_w_in = dram_in("ml_w_in", [D, 3088]); ml_w_out = dram_in("ml_w_out", [D, D])
        ml_gb = dram_in("ml_gb", [4, 4]); ml_cw = dram_in("ml_cw", [P, 8, 4]); ml_cb = dram_in("ml_cb", [P, 8])
        ml_mask = dram_in("ml_mask", [P, 2, P]); ml_ng = dram_in("ml_ng", [P, 8])
        ml_zqk = dram_tmp("ml_zqk", [D, NT]); ml_so = dram_tmp("ml_so", [D, NT], BF16); ml_v = dram_tmp("ml_v", [NT, D], BF16)
        ml_g = dram_tmp("ml_g", [4, 4, NT]); ml_q = dram_tmp("ml_q", [512, NT], BF16); ml_k = dram_tmp("ml_k", [512, NT], BF16)
        ml_hf = dram_tmp("ml_hf", [D, NT]); ml_gated = dram_tmp("ml_gated", [D, NT], BF16)
    LW = 1280
    if 2 in layers:
        lru_w_in = dram_in("lru_w_in", [D, 2 * LW])
        lru_w_out = dram_in("lru_w_out", [LW, D])
        lru_wa = dram_in("lru_wa", [P, 2, 10, P])
        lru_wx = dram_in("lru_wx", [P, 2, 10, P])
        lru_cw = dram_in("lru_cw", [P, 10, 4])
        lru_vec = dram_in("lru_vec", [P, 7, 10])
        lru_g = dram_tmp("lru_g", [LW, NT], BF16)
        lru_zx = dram_tmp("lru_zx", [LW, NT])
        lru_hf = dram_tmp("lru_hf", [LW, NT])
        lru_gated = dram_tmp("lru_gated", [LW, NT], BF16)
    ident_d = dram_in("ident", [P, P])
    ustrict_d = dram_in("ustrict", [P, P])
    iota32_d = dram_in("iota32", [P, 32])
    NBMAX = (2 * NT + NEXP * 127 + 127) // 128
    blkpos_d = dram_in("blkpos", [P, NBMAX])
    piota_d = dram_in("piota", [P, 1])
    sel_d = dram_in("sel", [32, NEXP * P])
    if final:
        outT = T(nc.dram_tensor("outT", [D, SEQ], F32, kind="ExternalOutput").ap())
    if out_res and not dump_res:
        res_out = T(nc.dram_tensor("res_out", [D, NT], F32, kind="ExternalOutput").ap())
    if in_res:
        res_in = dram_in("res_in", [D, NT])
    if dump_res:
        res_out = T(nc.dram_tensor("res_out", [D, NT], F32, kind="ExternalOutput").ap())
        info_out = T(nc.dram_tensor("info_out", [P, NT // P, 8], F32, kind="ExternalOutput").ap())
        dest_out = T(nc.dram_tensor("dest_out", [P, NT // P, 2], I32, kind="ExternalOutput").ap())
        carry_out = T(nc.dram_tensor("carry_out", [P, 32], F32, kind="ExternalOutput").ap())

    resd = dram_tmp("resd", [D, NT])
    h2rows = dram_tmp("h2rows", [NT, D], BF16)
    xbuf = dram_tmp("xbuf", [NBMAX * P, D], BF16)
    ybuf = dram_tmp("ybuf", [NBMAX * P, D], F32)

    def fm(t, c0, n):
        return t[:, c0:c0 + n].rearrange("(k p) t -> p k t", p=P)

    ident = sb("ident", [P, P])
    identb = sb("identb", [P, P], BF16)
    ones = sb("ones", [P, P])
    onesb = sb("onesb", [P, P], BF16)
    ustrict = sb("ustrictb", [P, P], BF16)
    ustrict_f = sb("ustrictf", [P, P])
    iota32 = sb("iota32", [P, 32])
    blkpos = sb("blkpos", [P, NBMAX])
    piota = sb("piota", [P, 1])
    sc = sb("sc", [P, 8, 2])
    modall = sb("modall", [P, DEPTH, 48, 2])
    nmix = sb("nmix", [P, DEPTH, 8])
    nffn = sb("nffn", [P, DEPTH, 8])
    nfin = sb("nfin", [P, 8])
    g1 = sb("g1", [P, 2, 8]); sh1 = sb("sh1", [P, 2, 8]); gt1 = sb("gt1", [P, 2, 8])
    g2 = sb("g2", [P, 2, 8]); sh2 = sb("sh2", [P, 2, 8]); gt2 = sb("gt2", [P, 2, 8])

    def dma(e, out_t, out_ap, in_t, in_ap):
        return b.op(e, lambda: b.engs[e].dma_start(out=out_ap, in_=in_ap), reads=[in_t.r], writes=[out_t.r], dma=True)

    dma("sp", ident, ident[:, :], ident_d, ident_d[:, :])
    dma("sp", ustrict_f, ustrict_f[:, :], ustrict_d, ustrict_d[:, :])
    dma("sp", iota32, iota32[:, :], iota32_d, iota32_d[:, :])
    dma("sp", blkpos, blkpos[:, :], blkpos_d, blkpos_d[:, :])
    dma("sp", piota, piota[:, :], piota_d, piota_d[:, :])
    dma("sp", sc, sc[:, :, :], cT, cT[:, :, :])
    dma("sp", nmix, nmix[:, :, :], nmixT, nmixT[:, :, :].rearrange("l p k -> p l k"))
    dma("sp", nffn, nffn[:, :, :], nffnT, nffnT[:, :, :].rearrange("l p k -> p l k"))
    dma("sp", nfin, nfin[:, :], nfinT, nfinT[:, :])
    V = nc.vector
    A = nc.scalar
    PE = nc.tensor
    G = nc.gpsimd
    b.op("dve", lambda: V.tensor_copy(out=identb[:, :], in_=ident[:, :]), [ident.r], [identb.r])
    b.op("dve", lambda: V.tensor_copy(out=ustrict[:, :], in_=ustrict_f[:, :]), [ustrict_f.r], [ustrict.r])
    b.op("dve", lambda: V.memset(ones[:, :], 1.0), [], [ones.r])
    b.op("dve", lambda: V.memset(onesb[:, :], 1.0), [], [onesb.r])
    b.op("act", lambda: A.activation(out=sc[:, :, :], in_=sc[:, :, :], func=AF.Silu), [sc.r], [sc.r])

    psf = Pool([ps("psf%d" % i, [P, 512]) for i in range(6)])
    psb = Pool([ps("psb%d" % i, [P, 1024], BF16) for i in range(2)])

    with Scope(b) as st:
        awt = T(st.enter_context(nc.sbuf_tensor("awt", [P, 8, 1024], F32)))
        abt = T(st.enter_context(nc.sbuf_tensor("abt", [P, 48], F32)))
        for L in layers:
            dma("sp", abt, abt[:, :], ada_bT, ada_bT[L, :, :])
            for m in range(6):
                dma("sp", awt, awt[:, :, :], ada_w[L],
                    ada_w[L][:, m * D:(m + 1) * D].rearrange("(k p) n -> p k n", p=P))
                pt = psf.get()
                for o in range(8):
                    for k in range(8):
                        last = (o == 7 and k == 7)
                        b.op("pe", lambda o=o, k=k: PE.matmul(pt[:, 2 * o:2 * o + 2], lhsT=awt[:, k, o * P:(o + 1) * P],
                                                               rhs=sc[:, k, :], start=(k == 0), stop=(k == 7)),
                             [awt.r, sc.r], [pt.r], inc=last)
                for j in range(2):
                    b.op("dve", lambda j=j, m=m: V.tensor_tensor(
                        out=modall[:, L, m * 8:(m + 1) * 8, j],
                        in0=pt[:, 0:16].rearrange("p (o j) -> p o j", j=2)[:, :, j],
                        in1=abt[:, m * 8:(m + 1) * 8], op=ALU.add), [pt.r, abt.r], [modall.r])

    def layer_scalars(L):
        for j in range(2):
            b.op("dve", lambda j=j: V.scalar_tensor_tensor(out=g1[:, j, :], in0=modall[:, L, 8:16, j], scalar=1.0,
                                                            in1=nmix[:, L, :], op0=ALU.add, op1=ALU.mult),
                 [modall.r, nmix.r], [g1.r])
            b.op("dve", lambda j=j: V.scalar_tensor_tensor(out=g2[:, j, :], in0=modall[:, L, 32:40, j], scalar=1.0,
                                                            in1=nffn[:, L, :], op0=ALU.add, op1=ALU.mult),
                 [modall.r, nffn.r], [g2.r])
            b.op("dve", lambda j=j: V.tensor_copy(out=sh1[:, j, :], in_=modall[:, L, 0:8, j]), [modall.r], [sh1.r])
            b.op("dve", lambda j=j: V.tensor_copy(out=gt1[:, j, :], in_=modall[:, L, 16:24, j]), [modall.r], [gt1.r])
            b.op("dve", lambda j=j: V.tensor_copy(out=sh2[:, j, :], in_=modall[:, L, 24:32, j]), [modall.r], [sh2.r])
            b.op("dve", lambda j=j: V.tensor_copy(out=gt2[:, j, :], in_=modall[:, L, 40:48, j]), [modall.r], [gt2.r])

    with Scope(b) as st:
        pa = Pool([T(st.enter_context(nc.sbuf_tensor("pa%d" % i, [P, 8, 512], F32))) for i in range(2)])
        pb = Pool([T(st.enter_context(nc.sbuf_tensor("pb%d" % i, [P, 8, 512], F32))) for i in range(2)])
        if in_res:
            for c0 in range(0, NT, 512):
                n = min(512, NT - c0)
                a = pa.get()
                dma("sp", a, a[:, :, :n], res_in, fm(res_in, c0, n))
                dma("sp", resd, fm(resd, c0, n), a, a[:, :, :n])
        else:
            a = pa.get()
            dma("sp", a, a[:, :, :NCTX], ctxT, fm(ctxT, 0, NCTX))
            dma("sp", resd, fm(resd, 0, NCTX), a, a[:, :, :NCTX])
            for c0 in range(0, SEQ, 512):
                a = pa.get(); p2 = pb.get()
                dma("sp", a, a[:, :, :], xT, fm(xT, c0, 512))
                dma("sp", p2, p2[:, :, :], posT, fm(posT, c0, 512))
                b.op("pool", lambda a=a, p2=p2: G.tensor_tensor(out=a[:, :, :], in0=a[:, :, :], in1=p2[:, :, :], op=ALU.add),
                     [a.r, p2.r], [a.r])
                dma("sp", resd, fm(resd, NCTX + c0, 512), a, a[:, :, :])

    def emit_dbg():
        for (nm, t_, ap_, shp, dt_) in dbg:
            o_ = T(nc.dram_tensor("dbg_" + nm, list(shp), dt_, kind="ExternalOutput").ap())
            b.op("pool", lambda o_=o_, ap_=ap_: G.dma_start(out=o_[tuple(slice(None) for _ in shp)], in_=ap_), [t_.r], [o_.r], dma=True)
        del dbg[:]

    def tiles(with_ctx):
        tl = [(0, NCTX, 1)] if with_ctx else []
        tl += [(NCTX + i * 512, 512, 0) for i in range(SEQ // 512)]
        if ntl is not None:
            tl = tl[:ntl]
        return tl

    def rmsnorm_mod(xin, n, j, gsc, shf, tbuf, sqb, out_tiles):
        pst = psf.get()
        for k in range(8):
            b.op("act", lambda k=k: A.activation(out=sqb[:, k, :n], in_=xin[:, k, :n], func=AF.Square), [xin.r], [sqb.r])
        for k in range(8):
            b.op("pe", lambda k=k: PE.matmul(pst[:, :n], lhsT=ones[:, :], rhs=sqb[:, k, :n], start=(k == 0), stop=(k == 7)),
                 [ones.r, sqb.r], [pst.r], inc=(k == 7))
        rstd = sqb
        b.op("act", lambda: A.activation(out=rstd[:, 0, :n], in_=pst[:, :n], func=AF.Sqrt, bias=epsc[:, 0:1], scale=1.0 / D),
             [pst.r, epsc.r], [sqb.r])
        b.op("dve", lambda: V.reciprocal(out=rstd[:, 0, :n], in_=rstd[:, 0, :n]), [sqb.r], [sqb.r])
        for k in range(8):
            b.op("dve", lambda k=k: V.tensor_tensor(out=tbuf[:, k, :n], in0=xin[:, k, :n], in1=rstd[:, 0, :n], op=ALU.mult),
                 [xin.r, sqb.r], [tbuf.r])
        for ot in out_tiles:
            for k in range(8):
                b.op("act", lambda k=k, ot=ot: A.activation(out=ot[:, k, :n], in_=tbuf[:, k, :n], func=AF.Identity,
                                                           bias=shf[:, j, k:k + 1], scale=gsc[:, j, k:k + 1]),
                     [tbuf.r, shf.r, gsc.r], [ot.r])

    epsc = sb("epsc", [P, 1])
    b.op("dve", lambda: V.memset(epsc[:, :], EPS), [], [epsc.r])
    onec = sb("onec", [P, 1])
    b.op("dve", lambda: V.memset(onec[:, :], 1.0), [], [onec.r])

    def gelu_tanh(out_ap, out_t, in_ap, in_t, tmp, n_shape):
        t1, t2 = tmp
        b.op("act", lambda: A.activation(out=t1[0], in_=in_ap, func=AF.Square), [in_t.r], [t1[1].r])
        b.op("dve", lambda: V.tensor_scalar(out=t1[0], in0=t1[0], scalar1=0.044715, scalar2=1.0, op0=ALU.mult, op1=ALU.add),
             [t1[1].r], [t1[1].r])
        b.op("dve", lambda: V.tensor_tensor(out=t1[0], in0=t1[0], in1=in_ap, op=ALU.mult), [t1[1].r, in_t.r], [t1[1].r])
        b.op("act", lambda: A.activation(out=t2[0], in_=t1[0], func=AF.Sigmoid, scale=1.5957691216057308),
             [t1[1].r], [t2[1].r])
        b.op("dve", lambda: V.tensor_tensor(out=out_ap, in0=t2[0], in1=in_ap, op=ALU.mult), [t2[1].r, in_t.r], [out_t.r])

    class Moe:
        pass

    m = Moe()
    m.rwt = sb("rwt", [P, 8, 36]); m.rbt = sb("rbt", [P, 36]); m.carry = sb("carry", [P, 32])
    m.info = sb("rinfo", [P, NT // P, 8])
    m.dest = T(nc.alloc_sbuf_tensor("dest", [P, NT // P, 2], I32))
    m.sm = Pool([sb("rsm%d" % i, [P, 256]) for i in range(2)])
    m.mb = Pool([T(nc.alloc_sbuf_tensor("mb%d" % i, [P, 32], BF16)) for i in range(2)])

    def moe_begin(m, L, ntok):
        m.nch = ntok // P
        m.nb = (2 * ntok + NEXP * 127 + 127) // 128
        dma("sp", m.rwt, m.rwt[:, :, :], rw, rw[L, :, :].rearrange("(k p) n -> p k n", p=P))
        dma("sp", m.rbt, m.rbt[:, :], rbrep, rbrep[L, :, :])
        b.op("dve", lambda: V.memset(m.carry[:, :], 0.0), [], [m.carry.r])

    def moe_route_chunk(m, h2f, off, ch, wt=None):
        s = m.sm.get()
        S = s.h
        pl = psf.get()
        for k in range(8):
            b.op("pe", lambda k=k: PE.matmul(pl[:, 0:36], lhsT=h2f[:, k, off:off + P], rhs=m.rwt[:, k, :],
                                              start=(k == 0), stop=(k == 7)), [h2f.r, m.rwt.r], [pl.r], inc=(k == 7))
        ops = []
        rs = [s.r]
        b.op("dve", lambda: V.tensor_tensor(out=S[:, 0:36], in0=pl[:, 0:36], in1=m.rbt[:, :], op=ALU.add), [pl.r, m.rbt.r], rs)
        b.op("dve", lambda: V.reduce_max(out=S[:, 40:41], in_=S[:, 0:4], axis=AX.X), rs, rs, ss=True)
        b.op("dve", lambda: V.tensor_scalar(out=S[:, 36:40], in0=S[:, 0:4], scalar1=S[:, 40:41], scalar2=None, op0=ALU.is_ge), rs, rs, ss=True)
        b.op("dve", lambda: V.tensor_scalar(out=S[:, 208:212], in0=S[:, 0:4], scalar1=S[:, 40:41], scalar2=None, op0=ALU.subtract), rs, rs, ss=True)
        b.op("act", lambda: A.activation(out=S[:, 208:212], in_=S[:, 208:212], func=AF.Exp), rs, rs, ss=True)
        b.op("dve", lambda: V.reduce_sum(out=S[:, 41:42], in_=S[:, 208:212], axis=AX.X), rs, rs, ss=True)
        b.op("dve", lambda: V.reciprocal(out=S[:, 41:42], in_=S[:, 41:42]), rs, rs)
        b.op("dve", lambda: V.tensor_scalar(out=S[:, 44:52], in0=S[:, 4:12], scalar1=S[:, 36:37], scalar2=None, op0=ALU.mult), rs, rs, ss=True)
        for g in range(1, 4):
            b.op("dve", lambda g=g: V.scalar_tensor_tensor(out=S[:, 44:52], in0=S[:, 4 + 8 * g:12 + 8 * g], scalar=S[:, 36 + g:37 + g],
                                                            in1=S[:, 44:52], op0=ALU.mult, op1=ALU.add), rs, rs, ss=True)
        b.op("dve", lambda: V.reduce_max(out=S[:, 76:77], in_=S[:, 44:52], axis=AX.X), rs, rs, ss=True)
        b.op("dve", lambda: V.tensor_scalar(out=S[:, 52:60], in0=S[:, 44:52], scalar1=S[:, 76:77], scalar2=None, op0=ALU.is_ge), rs, rs, ss=True)
        b.op("dve", lambda: V.scalar_tensor_tensor(out=S[:, 60:68], in0=S[:, 52:60], scalar=-1e30, in1=S[:, 44:52],
                                                    op0=ALU.mult, op1=ALU.add), rs, rs, ss=True)
        b.op("dve", lambda: V.reduce_max(out=S[:, 77:78], in_=S[:, 60:68], axis=AX.X), rs, rs, ss=True)
        b.op("dve", lambda: V.tensor_scalar(out=S[:, 68:76], in0=S[:, 60:68], scalar1=S[:, 77:78], scalar2=None, op0=ALU.is_ge), rs, rs, ss=True)
        b.op("dve", lambda: V.tensor_tensor(out=S[:, 78:79], in0=S[:, 77:78], in1=S[:, 76:77], op=ALU.subtract), rs, rs, ss=True)
        b.op("act", lambda: A.activation(out=S[:, 78:79], in_=S[:, 78:79], func=AF.Exp), rs, rs)
        b.op("dve", lambda: V.tensor_scalar(out=S[:, 79:80], in0=S[:, 78:79], scalar1=1.0, scalar2=None, op0=ALU.add), rs, rs, ss=True)
        b.op("dve", lambda: V.reciprocal(out=S[:, 79:80], in_=S[:, 79:80]), rs, rs, ss=True)
        ir = [m.info.r]
        b.op("dve", lambda: V.tensor_tensor(out=m.info[:, ch, 2:3], in0=S[:, 79:80], in1=S[:, 41:42], op=ALU.mult), rs, ir)
        b.op("dve", lambda: V.tensor_tensor(out=m.info[:, ch, 3:4], in0=m.info[:, ch, 2:3], in1=S[:, 78:79], op=ALU.mult), rs + ir, ir, ss=True)
        for g in range(4):
            b.op("dve", lambda g=g: V.tensor_scalar(out=S[:, 80 + 8 * g:88 + 8 * g], in0=S[:, 52:60], scalar1=S[:, 36 + g:37 + g],
                                                     scalar2=None, op0=ALU.mult), rs, rs, ss=True)
            b.op("dve", lambda g=g: V.tensor_scalar(out=S[:, 112 + 8 * g:120 + 8 * g], in0=S[:, 68:76], scalar1=S[:, 36 + g:37 + g],
                                                     scalar2=None, op0=ALU.mult), rs, rs, ss=True)
        if wt is not None:
            wt_t, wt_ap = wt
            b.op("dve", lambda: V.tensor_scalar(out=S[:, 144:176], in0=S[:, 80:112], scalar1=m.info[:, ch, 2:3], scalar2=None, op0=ALU.mult),
                 rs + ir, rs)
            b.op("dve", lambda: V.scalar_tensor_tensor(out=wt_ap, in0=S[:, 112:144], scalar=m.info[:, ch, 3:4], in1=S[:, 144:176],
                                                        op0=ALU.mult, op1=ALU.add), rs + ir, [wt_t.r])
            return
        mb = m.mb.get()
        b.op("dve", lambda: V.tensor_tensor(out=mb[:, :], in0=S[:, 80:112], in1=S[:, 112:144], op=ALU.add), rs, [mb.r])
        b.op("dve", lambda: V.tensor_tensor(out=S[:, 144:176], in0=S[:, 80:112], in1=iota32[:, :], op=ALU.mult), rs + [iota32.r], rs)
        b.op("dve", lambda: V.reduce_sum(out=m.info[:, ch, 0:1], in_=S[:, 144:176], axis=AX.X), rs, ir, ss=True)
        b.op("dve", lambda: V.tensor_tensor(out=S[:, 144:176], in0=S[:, 112:144], in1=iota32[:, :], op=ALU.mult), rs + [iota32.r], rs)
        b.op("dve", lambda: V.reduce_sum(out=m.info[:, ch, 1:2], in_=S[:, 144:176], axis=AX.X), rs, ir, ss=True)
        pc = psf.get()
        b.op("pe", lambda: PE.matmul(pc[:, 0:32], lhsT=ustrict[:, :], rhs=mb[:, :], start=True, stop=True), [ustrict.r, mb.r], [pc.r], inc=False)
        b.op("pe", lambda: PE.matmul(pc[:, 32:64], lhsT=onesb[:, :], rhs=mb[:, :], start=True, stop=True), [onesb.r, mb.r], [pc.r])
        b.op("dve", lambda: V.tensor_tensor(out=S[:, 176:208], in0=pc[:, 0:32], in1=m.carry[:, :], op=ALU.add), [pc.r, m.carry.r], rs)
        b.op("dve", lambda: V.tensor_tensor(out=m.carry[:, :], in0=pc[:, 32:64], in1=m.carry[:, :], op=ALU.add), [pc.r, m.carry.r], [m.carry.r])
        b.op("dve", lambda: V.tensor_tensor(out=S[:, 144:176], in0=S[:, 80:112], in1=S[:, 176:208], op=ALU.mult), rs, rs, ss=True)
        b.op("dve", lambda: V.reduce_sum(out=m.info[:, ch, 4:5], in_=S[:, 144:176], axis=AX.X), rs, ir, ss=True)
        b.op("dve", lambda: V.tensor_tensor(out=S[:, 144:176], in0=S[:, 112:144], in1=S[:, 176:208], op=ALU.mult), rs, rs, ss=True)
        b.op("dve", lambda: V.reduce_sum(out=m.info[:, ch, 5:6], in_=S[:, 144:176], axis=AX.X), rs, ir, ss=True)

    def moe_rows_chunk(m, h2b, off, tok0, rows):
        pt = psb.get()
        for k in range(8):
            b.op("pe", lambda k=k: PE.transpose(out=pt[:, k * P:(k + 1) * P], in_=h2b[:, k, off:off + P], identity=identb[:, :]),
                 [h2b.r, identb.r], [pt.r], inc=(k == 7))
        b.op("act", lambda: A.copy(out=rows[:, :], in_=pt[:, :]), [pt.r], [rows.r])
        dma("sp", h2rows, h2rows[tok0:tok0 + P, :], rows, rows[:, :])

    def moe_finish(m, L, tl, gt, tok_base):
        nb = m.nb
        with Scope(b) as st:
            def S_(name, shape, dt=F32):
                return T(st.enter_context(nc.sbuf_tensor(name, list(shape), dt)))
            padded = S_("padded", [P, 32]); pend = S_("pend", [P, 32]); pstart = S_("pstart", [P, 32])
            cmp3 = S_("cmp3", [P, nb, 32]); eb = S_("eb", [P, nb]); idxg = S_("idxg", [P, nb], I32); idxd = S_("idxd", [P, nb], I32)
            tmp32 = S_("tmp32", [P, 32]); destf = S_("destf", [P, m.nch, 2])
            b.op("dve", lambda: V.tensor_scalar(out=tmp32[:, :], in0=m.carry[:, :], scalar1=127.0, scalar2=1.0 / 128.0, op0=ALU.add, op1=ALU.mult),
                 [m.carry.r], [tmp32.r])
            b.op("dve", lambda: V.tensor_scalar(out=tmp32[:, :], in0=tmp32[:, :], scalar1=-0.498046875, scalar2=None, op0=ALU.add), [tmp32.r], [tmp32.r])
            b.op("dve", lambda: V.tensor_scalar(out=tmp32[:, :], in0=tmp32[:, :], scalar1=8388608.0, scalar2=None, op0=ALU.add), [tmp32.r], [tmp32.r])
            b.op("dve", lambda: V.tensor_scalar(out=padded[:, :], in0=tmp32[:, :], scalar1=-8388608.0, scalar2=128.0, op0=ALU.add, op1=ALU.mult),
                 [tmp32.r], [padded.r])
            pp = [pend, tmp32]
            b.op("dve", lambda: V.tensor_copy(out=pend[:, :], in_=padded[:, :]), [padded.r], [pend.r])
            cur = 0
            for sh in (1, 2, 4, 8, 16):
                A_, B_ = pp[cur], pp[1 - cur]
                b.op("dve", lambda A_=A_, B_=B_, sh=sh: V.tensor_copy(out=B_[:, 0:sh], in_=A_[:, 0:sh]), [A_.r], [B_.r])
                b.op("dve", lambda A_=A_, B_=B_, sh=sh: V.tensor_tensor(out=B_[:, sh:32], in0=A_[:, sh:32], in1=A_[:, 0:32 - sh], op=ALU.add), [A_.r], [B_.r])
                cur = 1 - cur
            if cur == 1:
                b.op("dve", lambda: V.tensor_copy(out=pend[:, :], in_=tmp32[:, :]), [tmp32.r], [pend.r])
            b.op("dve", lambda: V.tensor_tensor(out=pstart[:, :], in0=pend[:, :], in1=padded[:, :], op=ALU.subtract),
                 [pend.r, padded.r], [pstart.r])
            b.op("dve", lambda: V.tensor_tensor(out=cmp3[:, :, :], in0=pend[:, :].unsqueeze(1).to_broadcast([P, nb, 32]),
                                                 in1=blkpos[:, 0:nb].unsqueeze(2).to_broadcast([P, nb, 32]), op=ALU.is_le),
                 [pend.r, blkpos.r], [cmp3.r])
            b.op("dve", lambda: V.reduce_sum(out=eb[:, :], in_=cmp3[:, :, :], axis=AX.X), [cmp3.r], [eb.r])
            b.op("dve", lambda: V.tensor_scalar(out=eb[:, :], in0=eb[:, :], scalar1=31.0, scalar2=None, op0=ALU.min), [eb.r], [eb.r])
            b.op("dve", lambda: V.tensor_scalar(out=idxg[:, :], in0=eb[:, :], scalar1=float(D), scalar2=piota[:, 0:1], op0=ALU.mult, op1=ALU.add),
                 [eb.r, piota.r], [idxg.r])
            b.op("dve", lambda: V.tensor_scalar(out=idxd[:, :], in0=eb[:, :], scalar1=float(HID), scalar2=piota[:, 0:1], op0=ALU.mult, op1=ALU.add),
                 [eb.r, piota.r], [idxd.r])
            oh = S_("ohd", [P, 32])
            for ch in range(m.nch):
                for kk in range(2):
                    b.op("dve", lambda ch=ch, kk=kk: V.tensor_scalar(out=oh[:, :], in0=iota32[:, :], scalar1=m.info[:, ch, kk:kk + 1],
                                                                      scalar2=None, op0=ALU.is_equal), [iota32.r, m.info.r], [oh.r])
                    b.op("dve", lambda: V.tensor_tensor(out=oh[:, :], in0=oh[:, :], in1=pstart[:, :], op=ALU.mult), [oh.r, pstart.r], [oh.r])
                    b.op("dve", lambda ch=ch, kk=kk: V.reduce_sum(out=destf[:, ch, kk:kk + 1], in_=oh[:, :], axis=AX.X), [oh.r], [destf.r])
            b.op("dve", lambda: V.tensor_tensor(out=destf[:, :, :], in0=destf[:, :, :], in1=m.info[:, 0:m.nch, 4:6], op=ALU.add),
                 [destf.r, m.info.r], [destf.r])
            b.op("dve", lambda: V.tensor_copy(out=m.dest[:, 0:m.nch, :], in_=destf[:, :, :]), [destf.r], [m.dest.r])
            if stage < 4:
                if dump_res:
                    dbg.extend([("padded", padded, padded[:, :], [P, 32], F32), ("pend", pend, pend[:, :], [P, 32], F32),
                                ("pstart", pstart, pstart[:, :], [P, 32], F32), ("destf", destf, destf[:, :, :], [P, m.nch, 2], F32),
                                ("eb", eb, eb[:, :], [P, nb], F32), ("idxg", idxg, idxg[:, :], [P, nb], I32)])
                    emit_dbg()
                    b.barrier()
                return
            rp = Pool([S_("scr%d" % i, [P, D], BF16) for i in range(3)])
            for ch in range(m.nch):
                r_ = rp.get()
                dma("sp", r_, r_[:, :], h2rows, h2rows[ch * P:(ch + 1) * P, :])
                for kk in range(2):
                    b.op("pool", lambda ch=ch, kk=kk, r_=r_: G.indirect_dma_start(
                        out=xbuf[:, :], out_offset=bass.IndirectOffsetOnAxis(ap=m.dest[:, ch, kk:kk + 1], axis=0),
                        in_=r_[:, :], in_offset=None), [r_.r, m.dest.r], [xbuf.r], dma=True)
            if stage < 5:
                return
            wgp = Pool([S_("wgt%d" % i, [P, 8, HID], BF16) for i in range(2)])
            wup = Pool([S_("wut%d" % i, [P, 8, HID], BF16) for i in range(2)])
            wdp = Pool([S_("wdt%d" % i, [P, 4, D], BF16) for i in range(2)])
            xrp = Pool([S_("xr%d" % i, [P, D], BF16) for i in range(2)])
            xtp = Pool([S_("xt%d" % i, [P, 8, P], BF16) for i in range(2)])
            hp = Pool([S_("hh%d" % i, [P, 4, P], BF16) for i in range(2)])
            sgp = Pool([S_("sg%d" % i, [P, P], F32) for i in range(2)])
            yp = Pool([S_("yy%d" % i, [P, D], F32) for i in range(2)])
            wgL = wg[L][:, :]; wuL = wu[L][:, :]; wdL = wd[L][:, :]
            for blk in range(nb):
                wgt = wgp.get(); wut = wup.get(); wdt = wdp.get()
                for k in range(8):
                    b.op("pool", lambda k=k, wgt=wgt, blk=blk: G.indirect_dma_start(
                        out=wgt[:, k, :], out_offset=None, in_=wgL,
                        in_offset=bass.IndirectOffsetOnAxis(ap=idxg[:, blk:blk + 1], axis=0), element_offset=k * P * HID),
                        [wg[L].r, idxg.r], [wgt.r], dma=True)
                    b.op("pool", lambda k=k, wut=wut, blk=blk: G.indirect_dma_start(
                        out=wut[:, k, :], out_offset=None, in_=wuL,
                        in_offset=bass.IndirectOffsetOnAxis(ap=idxg[:, blk:blk + 1], axis=0), element_offset=k * P * HID),
                        [wu[L].r, idxg.r], [wut.r], dma=True)
                for k in range(4):
                    b.op("pool", lambda k=k, wdt=wdt, blk=blk: G.indirect_dma_start(
                        out=wdt[:, k, :], out_offset=None, in_=wdL,
                        in_offset=bass.IndirectOffsetOnAxis(ap=idxd[:, blk:blk + 1], axis=0), element_offset=k * P * D),
                        [wd[L].r, idxd.r], [wdt.r], dma=True)
                xr = xrp.get(); xt = xtp.get()
                dma("sp", xr, xr[:, :], xbuf, xbuf[blk * P:(blk + 1) * P, :])
                pt = psb.get()
                for k in range(8):
                    b.op("pe", lambda k=k: PE.transpose(out=pt[:, k * P:(k + 1) * P], in_=xr[:, k * P:(k + 1) * P], identity=identb[:, :]),
                         [xr.r, identb.r], [pt.r], inc=(k == 7))
                b.op("act", lambda: A.copy(out=xt[:, :, :], in_=pt[:, :].rearrange("p (k s) -> p k s", k=8)), [pt.r], [xt.r])
                hh = hp.get()
                for j in range(4):
                    pg = psf.get()
                    for k in range(8):
                        b.op("pe", lambda k=k, j=j: PE.matmul(pg[:, 0:P], lhsT=wgt[:, k, j * P:(j + 1) * P], rhs=xt[:, k, :],
                                                               start=(k == 0), stop=(k == 7)), [wgt.r, xt.r], [pg.r], inc=False)
                    for k in range(8):
                        b.op("pe", lambda k=k, j=j: PE.matmul(pg[:, P:2 * P], lhsT=wut[:, k, j * P:(j + 1) * P], rhs=xt[:, k, :],
                                                               start=(k == 0), stop=(k == 7)), [wut.r, xt.r], [pg.r], inc=(k == 7))
                    sg = sgp.get()
                    b.op("act", lambda: A.activation(out=sg[:, :], in_=pg[:, 0:P], func=AF.Silu), [pg.r], [sg.r])
                    b.op("dve", lambda j=j: V.tensor_tensor(out=hh[:, j, :], in0=sg[:, :], in1=pg[:, P:2 * P], op=ALU.mult),
                         [sg.r, pg.r], [hh.r])
                yy = yp.get()
                for half in range(2):
                    py = psf.get()
                    for j in range(4):
                        b.op("pe", lambda j=j, half=half: PE.matmul(py[:, :], lhsT=hh[:, j, :], rhs=wdt[:, j, half * 512:(half + 1) * 512],
                                                                     start=(j == 0), stop=(j == 3)), [hh.r, wdt.r], [py.r], inc=(j == 3))
                    if half == 0:
                        b.op("act", lambda: A.copy(out=yy[:, 0:512], in_=py[:, :]), [py.r], [yy.r])
                    else:
                        b.op("dve", lambda: V.tensor_copy(out=yy[:, 512:1024], in_=py[:, :]), [py.r], [yy.r])
                dma("sp", ybuf, ybuf[blk * P:(blk + 1) * P, :], yy, yy[:, :])
        if stage < 6:
            return
        with Scope(b) as st:
            def S_(name, shape, dt=F32):
                return T(st.enter_context(nc.sbuf_tensor(name, list(shape), dt)))
            y0p = Pool([S_("y0_%d" % i, [P, D]) for i in range(2)])
            y1p = Pool([S_("y1_%d" % i, [P, D]) for i in range(2)])
            xp = Pool([S_("xd%d" % i, [P, 8, 512]) for i in range(2)])
            for (c0, n, isctx) in tl:
                xin = xp.get()
                dma("sp", xin, xin[:, :, :n], resd, fm(resd, c0, n))
                for ci in range(n // P):
                    ch = (c0 - tok_base) // P + ci
                    y0 = y0p.get(); y1 = y1p.get()
                    for kk, yt in ((0, y0), (1, y1)):
                        b.op("pool", lambda ch=ch, kk=kk, yt=yt: G.indirect_dma_start(
                            out=yt[:, :], out_offset=None, in_=ybuf[:, :],
                            in_offset=bass.IndirectOffsetOnAxis(ap=m.dest[:, ch, kk:kk + 1], axis=0)),
                            [ybuf.r, m.dest.r], [yt.r], dma=True)
                    b.op("dve", lambda ch=ch, y0=y0: V.tensor_scalar(out=y0[:, :], in0=y0[:, :], scalar1=m.info[:, ch, 2:3], scalar2=None,
                                                                      op0=ALU.mult), [y0.r, m.info.r], [y0.r])
                    b.op("dve", lambda ch=ch, y0=y0, y1=y1: V.scalar_tensor_tensor(out=y0[:, :], in0=y1[:, :], scalar=m.info[:, ch, 3:4],
                                                                                    in1=y0[:, :], op0=ALU.mult, op1=ALU.add),
                         [y0.r, y1.r, m.info.r], [y0.r])
                    for hf in range(2):
                        pt = psf.get()
                        for kq in range(4):
                            k = hf * 4 + kq
                            b.op("pe", lambda k=k, kq=kq, y0=y0, pt=pt: PE.transpose(out=pt[:, kq * P:(kq + 1) * P], in_=y0[:, k * P:(k + 1) * P],
                                                                                     identity=ident[:, :]), [y0.r, ident.r], [pt.r], inc=(kq == 3))
                        for kq in range(4):
                            k = hf * 4 + kq
                            b.op("dve", lambda k=k, kq=kq, pt=pt, ci=ci: V.scalar_tensor_tensor(
                                out=xin[:, k, ci * P:(ci + 1) * P], in0=pt[:, kq * P:(kq + 1) * P], scalar=gt[:, isctx, k:k + 1],
                                in1=xin[:, k, ci * P:(ci + 1) * P], op0=ALU.mult, op1=ALU.add), [pt.r, gt.r, xin.r], [xin.r])
                dma("sp", resd, fm(resd, c0, n), xin, xin[:, :, :n])

    GT = 2

    def moe_dense(L, tl):
        moe_begin(m, L, sum(t[1] for t in tl))
        wgL = wg[L]; wuL = wu[L]; wdL = wd[L]
        with Scope(b) as st:
            def S_(name, shape, dt=F32):
                return T(st.enter_context(nc.sbuf_tensor("%s_L%d" % (name, L), list(shape), dt)))
            xin = S_("mx", [P, 8, 512]); sqb = S_("msq", [P, 8, 512]); tb = xin
            h2 = [S_("mh2_%d" % i, [P, 8, 512], BF16) for i in range(GT)]
            yac = [S_("myac%d" % i, [P, 8, 512]) for i in range(GT)]
            for t_ in yac:
                t_.res = [Res() for _ in range(8)]
            wtT = [S_("mwtT%d" % i, [32, 512]) for i in range(GT)]
            wtc = S_("mwtc", [P, 4, 32])
            sel = S_("msel", [32, NEXP * P])
            dma("sp", sel, sel[:, :], sel_d, sel_d[:, :])
            wgp = Pool([S_("mwg%d" % i, [P, 8, HID], BF16) for i in range(2)])
            wup = Pool([S_("mwu%d" % i, [P, 8, HID], BF16) for i in range(2)])
            wdp = Pool([S_("mwd%d" % i, [P, 4, D], BF16) for i in range(2)])
            wrow = S_("mwrow", [P, 512]); sgP = Pool([S_("msg%d" % i, [P, 512]) for i in range(2)]); ttP = Pool([S_("mtt%d" % i, [P, 512]) for i in range(2)])
            hsp = Pool([S_("mhs%d" % i, [P, 4, 512], BF16) for i in range(2)])
            for t_ in hsp.tiles:
                t_.res = [Res() for _ in range(4)]
            for g0 in range(0, len(tl), GT):
                grp = tl[g0:g0 + GT]
                for i, (c0, n, isctx) in enumerate(grp):
                    dma("sp", xin, xin[:, :, :n], resd, fm(resd, c0, n))
                    rmsnorm_mod(xin, n, isctx, g2, sh2, tb, sqb, [sqb, h2[i]])
                    for ci in range(n // P):
                        moe_route_chunk(m, sqb, ci * P, c0 // P + ci, wt=(wtc, wtc[:, ci, :]))
                    pT = psf.get()
                    for ci in range(n // P):
                        b.op("pe", lambda ci=ci: PE.transpose(out=pT[0:32, ci * P:(ci + 1) * P], in_=wtc[:, ci, :], identity=ident[:, :]),
                             [wtc.r, ident.r], [pT.r], inc=(ci == n // P - 1))
                    b.op("act", lambda i=i, n=n: A.copy(out=wtT[i][:, :n], in_=pT[0:32, :n]), [pT.r], [wtT[i].r])
                    b.op("pool", lambda i=i: G.memset(yac[i][:, :, :], 0.0), [], yac[i].res)
                for e in range(NEXP):
                    wgt = wgp.get(); wut = wup.get(); wdt = wdp.get()
                    b.op("pool", lambda: G.dma_start(out=wgt[:, :, :], in_=wgL[e * D:(e + 1) * D, :].rearrange("(k p) n -> p k n", p=P)),
                         [wgL.r], [wgt.r], dma=True)
                    b.op("pool", lambda: G.dma_start(out=wut[:, :, :], in_=wuL[e * D:(e + 1) * D, :].rearrange("(k p) n -> p k n", p=P)),
                         [wuL.r], [wut.r], dma=True)
                    b.op("pool", lambda: G.dma_start(out=wdt[:, :, :], in_=wdL[e * HID:(e + 1) * HID, :].rearrange("(k p) n -> p k n", p=P)),
                         [wdL.r], [wdt.r], dma=True)
                    for i, (c0, n, isctx) in enumerate(grp):
                        pw = psf.get()
                        b.op("pe", lambda i=i, n=n: PE.matmul(pw[:, :n], lhsT=sel[:, e * P:(e + 1) * P], rhs=wtT[i][:, :n], start=True, stop=True),
                             [sel.r, wtT[i].r], [pw.r])
                        b.op("act", lambda n=n: A.copy(out=wrow[:, :n], in_=pw[:, :n]), [pw.r], [wrow.r])
                        hs = hsp.get()
                        for j in range(4):
                            sg = sgP.get(); tt = ttP.get()
                            pg = psf.get(); pu = psf.get()
                            for k in range(8):
                                b.op("pe", lambda k=k, j=j, i=i, n=n: PE.matmul(pg[:, :n], lhsT=wgt[:, k, j * P:(j + 1) * P], rhs=h2[i][:, k, :n],
                                                                               start=(k == 0), stop=(k == 7)), [wgt.r, h2[i].r], [pg.r], inc=(k == 7))
                            for k in range(8):
                                b.op("pe", lambda k=k, j=j, i=i, n=n: PE.matmul(pu[:, :n], lhsT=wut[:, k, j * P:(j + 1) * P], rhs=h2[i][:, k, :n],
                                                                               start=(k == 0), stop=(k == 7)), [wut.r, h2[i].r], [pu.r], inc=(k == 7))
                            b.op("act", lambda n=n: A.activation(out=sg[:, :n], in_=pg[:, :n], func=AF.Silu), [pg.r], [sg.r])
                            b.op("dve", lambda n=n: V.tensor_tensor(out=tt[:, :n], in0=sg[:, :n], in1=pu[:, :n], op=ALU.mult), [sg.r, pu.r], [tt.r])
                            b.op("dve", lambda n=n, j=j: V.tensor_tensor(out=hs[:, j, :n], in0=tt[:, :n], in1=wrow[:, :n], op=ALU.mult),
                                 [tt.r, wrow.r], [hs.res[j]])
                        for o in range(8):
                            py = psf.get()
                            for j in range(4):
                                b.op("pe", lambda o=o, j=j, n=n: PE.matmul(py[:, :n], lhsT=wdt[:, j, o * P:(o + 1) * P], rhs=hs[:, j, :n],
                                                                          start=(j == 0), stop=(j == 3)), [wdt.r, hs.res[j]], [py.r], inc=(j == 3))
                            b.op("dve", lambda o=o, i=i, n=n: V.tensor_tensor(out=yac[i][:, o, :n], in0=yac[i][:, o, :n], in1=py[:, :n], op=ALU.add),
                                 [py.r, yac[i].res[o]], [yac[i].res[o]])
                for i, (c0, n, isctx) in enumerate(grp):
                    dma("sp", xin, xin[:, :, :n], resd, fm(resd, c0, n))
                    for o in range(8):
                        b.op("dve", lambda o=o, i=i, n=n, isctx=isctx: V.scalar_tensor_tensor(
                            out=xin[:, o, :n], in0=yac[i][:, o, :n], scalar=gt2[:, isctx, o:o + 1], in1=xin[:, o, :n],
                            op0=ALU.mult, op1=ALU.add), [yac[i].res[o], gt2.r, xin.r], [xin.r])
                    dma("sp", resd, fm(resd, c0, n), xin, xin[:, :, :n])

    def layer2(L):
        layer_scalars(L)
        tl = tiles(True)
        with Scope(b) as st:
            def S_(name, shape, dt=F32):
                return T(st.enter_context(nc.sbuf_tensor(name, list(shape), dt)))
            win = S_("lwin", [P, 8, 2 * LW], BF16)
            b.op("pool", lambda: G.dma_start(out=win[:, :, :], in_=lru_w_in[:, :].rearrange("(k p) n -> p k n", p=P)), [lru_w_in.r], [win.r], dma=True)
            xin = S_("lxin", [P, 8, 512]); sqb = S_("lsqb", [P, 8, 512])
            hT = S_("lhT", [P, 8, 512], BF16)
            gsb = S_("lgsb", [P, 10, 512], BF16); zsb = S_("lzsb", [P, 10, 512])
            t1 = S_("lt1", [P, 512]); t2 = S_("lt2", [P, 512])
            for (c0, n, isctx) in tl:
                dma("sp", xin, xin[:, :, :n], resd, fm(resd, c0, n))
                rmsnorm_mod(xin, n, isctx, g1, sh1, xin, sqb, [hT])
                for o in range(20):
                    if o < 10 and isctx:
                        continue
                    pz = psf.get()
                    for k in range(8):
                        b.op("pe", lambda o=o, k=k: PE.matmul(pz[:, :n], lhsT=win[:, k, o * P:(o + 1) * P], rhs=hT[:, k, :n],
                                                               start=(k == 0), stop=(k == 7)), [win.r, hT.r], [pz.r], inc=(k == 7))
                    if o < 10:
                        gelu_tanh(gsb[:, o, :n], gsb, pz[:, :n], pz, ((t1[:, :n], t1), (t2[:, :n], t2)), None)
                    else:
                        b.op("act", lambda o=o: A.copy(out=zsb[:, o - 10, :n], in_=pz[:, :n]), [pz.r], [zsb.r])
                if not isctx:
                    dma("sp", lru_g, lru_g[:, c0:c0 + n].rearrange("(k p) t -> p k t", p=P), gsb, gsb[:, :, :n])
                dma("sp", lru_zx, lru_zx[:, c0:c0 + n].rearrange("(k p) t -> p k t", p=P), zsb, zsb[:, :, :n])
        with Scope(b) as st:
            def S_(name, shape, dt=F32):
                return T(st.enter_context(nc.sbuf_tensor(name, list(shape), dt)))
            wa = S_("lwa", [P, 2, 10, P]); wx = S_("lwx", [P, 2, 10, P]); cw = S_("lcw", [P, 10, 4]); vec = S_("lvec", [P, 7, 10])
            nsp = S_("lnsp", [P, 2, 10])
            dma("sp", wa, wa[:, :, :, :], lru_wa, lru_wa[:, :, :, :]); dma("sp", wx, wx[:, :, :, :], lru_wx, lru_wx[:, :, :, :])
            dma("sp", cw, cw[:, :, :], lru_cw, lru_cw[:, :, :]); dma("sp", vec, vec[:, :, :], lru_vec, lru_vec[:, :, :])
            b.op("act", lambda: A.activation(out=nsp[:, :, :], in_=vec[:, 5:7, :], func=AF.Exp, scale=-1.0), [vec.r], [nsp.r])
            b.op("act", lambda: A.activation(out=nsp[:, :, :], in_=nsp[:, :, :], func=AF.Ln, bias=onec[:, 0:1], scale=1.0), [nsp.r, onec.r], [nsp.r])
            b.op("dve", lambda: V.tensor_scalar(out=nsp[:, :, :], in0=nsp[:, :, :], scalar1=-8.0, scalar2=None, op0=ALU.mult), [nsp.r], [nsp.r])
            zp = Pool([S_("lzh%d" % i, [P, 516]) for i in range(2)])
            xr = S_("lxr", [P, 512]); rr = S_("lrr", [P, 512]); ii = S_("lii", [P, 512]); aa = S_("laa", [P, 512]); uu = S_("luu", [P, 512])
            hh = S_("lhh", [P, 512]); hfp = Pool([S_("lhf%d" % i, [P, 512]) for i in range(2)])
            gtp = Pool([S_("lgt%d" % i, [P, 512], BF16) for i in range(2)]); gout = S_("lgo", [P, 512], BF16)
            carry = S_("lcar", [P, 1])
            lat_tl = [t for t in tl if not t[2]]
            for ct in range(10):
                for d in range(2):
                    order = tl if d == 0 else ([t for t in tl if t[2]] + lat_tl[::-1])
                    b.op("dve", lambda: V.memset(carry[:, :], 0.0), [], [carry.r])
                    for (c0, n, isctx) in order:
                        lo = 0 if isctx else NCTX
                        hi = NCTX if isctx else NT
                        zh = zp.get()
                        a0 = max(c0 - 2, lo); a1 = min(c0 + n + 1, hi)
                        if a0 > c0 - 2 or a1 < c0 + n + 1:
                            b.op("pool", lambda zh=zh: G.memset(zh[:, :], 0.0), [], [zh.r])
                        dma("sp", zh, zh[:, a0 - (c0 - 2):a1 - (c0 - 2)], lru_zx, lru_zx[ct * P:(ct + 1) * P, a0:a1])
                        b.op("dve", lambda zh=zh: V.tensor_scalar(out=xr[:, :n], in0=zh[:, 0:n], scalar1=cw[:, ct, 0:1], scalar2=vec[:, 0, ct:ct + 1],
                                                                  op0=ALU.mult, op1=ALU.add), [zh.r, cw.r, vec.r], [xr.r])
                        for j in range(1, 4):
                            b.op("dve", lambda zh=zh, j=j: V.scalar_tensor_tensor(out=xr[:, :n], in0=zh[:, j:j + n], scalar=cw[:, ct, j:j + 1], in1=xr[:, :n],
                                                                                  op0=ALU.mult, op1=ALU.add), [zh.r, cw.r, xr.r], [xr.r])
                        pr = psf.get(); pi = psf.get()
                        b.op("pe", lambda: PE.matmul(pr[:, :n], lhsT=wa[:, d, ct, :], rhs=xr[:, :n], start=True, stop=True), [wa.r, xr.r], [pr.r])
                        b.op("pe", lambda: PE.matmul(pi[:, :n], lhsT=wx[:, d, ct, :], rhs=xr[:, :n], start=True, stop=True), [wx.r, xr.r], [pi.r])
                        b.op("act", lambda: A.activation(out=rr[:, :n], in_=pr[:, :n], func=AF.Sigmoid, bias=vec[:, 1 + d, ct:ct + 1], scale=1.0),
                             [pr.r, vec.r], [rr.r])
                        b.op("act", lambda: A.activation(out=ii[:, :n], in_=pi[:, :n], func=AF.Sigmoid, bias=vec[:, 3 + d, ct:ct + 1], scale=1.0),
                             [pi.r, vec.r], [ii.r])
                        b.op("act", lambda: A.activation(out=aa[:, :n], in_=rr[:, :n], func=AF.Exp, scale=nsp[:, d, ct:ct + 1]), [rr.r, nsp.r], [aa.r])
                        b.op("dve", lambda: V.tensor_tensor(out=uu[:, :n], in0=aa[:, :n], in1=aa[:, :n], op=ALU.mult), [aa.r], [uu.r])
                        b.op("act", lambda: A.activation(out=uu[:, :n], in_=uu[:, :n], func=AF.Sqrt, bias=onec[:, 0:1], scale=-1.0), [uu.r, onec.r], [uu.r])
                        b.op("dve", lambda: V.tensor_tensor(out=ii[:, :n], in0=ii[:, :n], in1=xr[:, :n], op=ALU.mult), [ii.r, xr.r], [ii.r])
                        b.op("dve", lambda: V.tensor_tensor(out=uu[:, :n], in0=uu[:, :n], in1=ii[:, :n], op=ALU.mult), [uu.r, ii.r], [uu.r])

                        def r2(t):
                            ap = t[:, 0:n]
                            return AP(ap.tensor, ap.offset + (n - 1), [[ap.ap[0][0], P], [-1, n]])
                        if d == 0:
                            hf = hfp.get()
                            b.op("dve", lambda hf=hf: V.tensor_tensor_scan(out=hf[:, :n], data0=aa[:, :n], data1=uu[:, :n], initial=carry[:, 0:1],
                                                                           op0=ALU.mult, op1=ALU.add), [aa.r, uu.r, carry.r], [hf.r])
                            b.op("dve", lambda hf=hf: V.tensor_copy(out=carry[:, :], in_=hf[:, n - 1:n]), [hf.r], [carry.r])
                            if not isctx:
                                dma("sp", lru_hf, lru_hf[ct * P:(ct + 1) * P, c0:c0 + n], hf, hf[:, :n])
                        else:
                            b.op("dve", lambda: V.tensor_tensor_scan(out=r2(hh), data0=r2(aa), data1=r2(uu), initial=carry[:, 0:1],
                                                                      op0=ALU.mult, op1=ALU.add), [aa.r, uu.r, carry.r], [hh.r])
                            b.op("dve", lambda: V.tensor_copy(out=carry[:, :], in_=hh[:, 0:1]), [hh.r], [carry.r])
                            if not isctx:
                                hf = hfp.get(); gt_ = gtp.get()
                                dma("sp", hf, hf[:, :n], lru_hf, lru_hf[ct * P:(ct + 1) * P, c0:c0 + n])
                                dma("sp", gt_, gt_[:, :n], lru_g, lru_g[ct * P:(ct + 1) * P, c0:c0 + n])
                                b.op("dve", lambda hf=hf: V.tensor_tensor(out=hh[:, :n], in0=hh[:, :n], in1=hf[:, :n], op=ALU.add), [hh.r, hf.r], [hh.r])
                                b.op("dve", lambda gt_=gt_: V.tensor_tensor(out=gout[:, :n], in0=hh[:, :n], in1=gt_[:, :n], op=ALU.mult), [hh.r, gt_.r], [gout.r])
                                dma("sp", lru_gated, lru_gated[ct * P:(ct + 1) * P, c0:c0 + n], gout, gout[:, :n])
        with Scope(b) as st:
            def S_(name, shape, dt=F32):
                return T(st.enter_context(nc.sbuf_tensor(name, list(shape), dt)))
            wout = S_("lwout", [P, 10, D], BF16)
            b.op("pool", lambda: G.dma_start(out=wout[:, :, :], in_=lru_w_out[:, :].rearrange("(k p) n -> p k n", p=P)), [lru_w_out.r], [wout.r], dma=True)
            xp = Pool([S_("lcx%d" % i, [P, 8, 512]) for i in range(2)])
            gp = Pool([S_("lcg%d" % i, [P, 10, 512], BF16) for i in range(2)])
            for (c0, n, isctx) in tiles(False):
                xin = xp.get(); gg = gp.get()
                dma("sp", xin, xin[:, :, :n], resd, fm(resd, c0, n))
                dma("sp", gg, gg[:, :, :n], lru_gated, lru_gated[:, c0:c0 + n].rearrange("(k p) t -> p k t", p=P))
                for o in range(8):
                    py = psf.get()
                    for k in range(10):
                        b.op("pe", lambda o=o, k=k: PE.matmul(py[:, :n], lhsT=wout[:, k, o * P:(o + 1) * P], rhs=gg[:, k, :n],
                                                               start=(k == 0), stop=(k == 9)), [wout.r, gg.r], [py.r], inc=(k == 9))
                    b.op("dve", lambda o=o: V.scalar_tensor_tensor(out=xin[:, o, :n], in0=py[:, :n], scalar=gt1[:, 0, o:o + 1],
                                                                    in1=xin[:, o, :n], op0=ALU.mult, op1=ALU.add), [py.r, gt1.r, xin.r], [xin.r])
                dma("sp", resd, fm(resd, c0, n), xin, xin[:, :, :n])
        return tiles(False)

    def layer3(L):
        layer_scalars(L)
        tl = tiles(False)
        with Scope(b) as st:
            def S_(name, shape, dt=F32):
                return T(st.enter_context(nc.sbuf_tensor(name, list(shape), dt)))
            win = S_("fwin", [P, 8, D], BF16)
            b.op("pool", lambda: G.dma_start(out=win[:, :, :], in_=fn_w_in[:, :].rearrange("(k p) n -> p k n", p=P)), [fn_w_in.r], [win.r], dma=True)
            cs = S_("fcs", [P, 2, 512])
            dma("sp", cs, cs[:, :, :], fn_cs, fn_cs[:, :, :])
            xin = S_("fxin", [P, 8, 512]); sqb = S_("fsqb", [P, 8, 512]); hT = S_("fhT", [P, 8, 512], BF16)
            zs = S_("fzs", [P, 8, 512]); uvp = Pool([S_("fuv%d" % i, [P, 2048]) for i in range(2)])
            for (c0, n, isctx) in tl:
                dma("sp", xin, xin[:, :, :n], resd, fm(resd, c0, n))
                rmsnorm_mod(xin, n, 0, g1, sh1, xin, sqb, [hT])
                for o in range(8):
                    pz = psf.get()
                    for k in range(8):
                        b.op("pe", lambda o=o, k=k: PE.matmul(pz[:, :n], lhsT=win[:, k, o * P:(o + 1) * P], rhs=hT[:, k, :n],
                                                               start=(k == 0), stop=(k == 7)), [win.r, hT.r], [pz.r], inc=(k == 7))
                    b.op("act", lambda o=o: A.copy(out=zs[:, o, :n], in_=pz[:, :n]), [pz.r], [zs.r])
                for ci in range(n // P):
                    uv = uvp.get()
                    for g in range(4):
                        pu = psf.get()
                        for kk in range(2):
                            b.op("pe", lambda g=g, kk=kk, ci=ci: PE.matmul(pu[:, :], lhsT=zs[:, 2 * g + kk, ci * P:(ci + 1) * P], rhs=cs[:, kk, :],
                                                                            start=(kk == 0), stop=(kk == 1)), [zs.r, cs.r], [pu.r], inc=(kk == 1))
                        if g % 2 == 0:
                            b.op("act", lambda g=g, uv=uv: A.copy(out=uv[:, g * 512:(g + 1) * 512], in_=pu[:, :]), [pu.r], [uv.r])
                        else:
                            b.op("dve", lambda g=g, uv=uv: V.tensor_copy(out=uv[:, g * 512:(g + 1) * 512], in_=pu[:, :]), [pu.r], [uv.r])
                    t0 = c0 - NCTX + ci * P
                    dma("sp", fn_uv, fn_uv[t0:t0 + P, :], uv, uv[:, :])
        with Scope(b) as st:
            def S_(name, shape, dt=F32):
                return T(st.enter_context(nc.sbuf_tensor(name, list(shape), dt)))
            dft = S_("fdft", [P, 3, P]); tw = S_("ftw", [P, 2, P])
            dma("sp", dft, dft[:, :, :], fn_dft, fn_dft[:, :, :]); dma("sp", tw, tw[:, :, :], fn_tw, fn_tw[:, :, :])
            inp_ = Pool([S_("fin%d" % i, [P, 512]) for i in range(3)])
            outp = Pool([S_("fout%d" % i, [P, 512]) for i in range(3)])
            tmp = S_("ftmp", [P, 512])
            uv3 = fn_uv[:, :].rearrange("(t1 t2) c -> t1 t2 c", t2=P)
            for t2 in range(P):
                for g in range(4):
                    xi = inp_.get(); bo = outp.get()
                    dma("sp", xi, xi[:, :], fn_uv, uv3[:, t2, g * 512:(g + 1) * 512])
                    pa = psf.get()
                    b.op("pe", lambda: PE.matmul(pa[:, 0:256], lhsT=dft[:, 0, :], rhs=xi[:, 0:256], start=True, stop=False), [dft.r, xi.r], [pa.r], inc=False)
                    b.op("pe", lambda: PE.matmul(pa[:, 0:256], lhsT=dft[:, 2, :], rhs=xi[:, 256:512], start=False, stop=True), [dft.r, xi.r], [pa.r], inc=False)
                    b.op("pe", lambda: PE.matmul(pa[:, 256:512], lhsT=dft[:, 1, :], rhs=xi[:, 0:256], start=True, stop=False), [dft.r, xi.r], [pa.r], inc=False)
                    b.op("pe", lambda: PE.matmul(pa[:, 256:512], lhsT=dft[:, 0, :], rhs=xi[:, 256:512], start=False, stop=True), [dft.r, xi.r], [pa.r])
                    b.op("dve", lambda: V.tensor_scalar(out=tmp[:, 0:256], in0=pa[:, 256:512], scalar1=tw[:, 1, t2:t2 + 1], scalar2=None, op0=ALU.mult),
                         [pa.r, tw.r], [tmp.r])
                    b.op("dve", lambda: V.scalar_tensor_tensor(out=bo[:, 0:256], in0=pa[:, 0:256], scalar=tw[:, 0, t2:t2 + 1], in1=tmp[:, 0:256],
                                                                op0=ALU.mult, op1=ALU.subtract), [pa.r, tw.r, tmp.r], [bo.r])
                    b.op("dve", lambda: V.tensor_scalar(out=tmp[:, 256:512], in0=pa[:, 256:512], scalar1=tw[:, 0, t2:t2 + 1], scalar2=None, op0=ALU.mult),
                         [pa.r, tw.r], [tmp.r])
                    b.op("dve", lambda: V.scalar_tensor_tensor(out=bo[:, 256:512], in0=pa[:, 0:256], scalar=tw[:, 1, t2:t2 + 1], in1=tmp[:, 256:512],
                                                                op0=ALU.mult, op1=ALU.add), [pa.r, tw.r, tmp.r], [bo.r])
                    dma("sp", fn_b, fn_b[t2, :, g * 512:(g + 1) * 512], bo, bo[:, :])
            rp = Pool([S_("frr%d" % i, [P, D]) for i in range(2)])
            r3 = fn_r[:, :].rearrange("(k2 k1) c -> k2 k1 c", k1=P)
            for k1 in range(P):
                ro = rp.get()
                for g in range(4):
                    xi = inp_.get()
                    dma("sp", xi, xi[:, :], fn_b, fn_b[:, k1, g * 512:(g + 1) * 512])
                    pr = psf.get()
                    b.op("pe", lambda: PE.matmul(pr[:, 0:256], lhsT=dft[:, 0, :], rhs=xi[:, 0:256], start=True, stop=False), [dft.r, xi.r], [pr.r], inc=False)
                    b.op("pe", lambda: PE.matmul(pr[:, 0:256], lhsT=dft[:, 2, :], rhs=xi[:, 256:512], start=False, stop=True), [dft.r, xi.r], [pr.r])
                    b.op("act", lambda g=g, ro=ro: A.activation(out=ro[:, g * 256:(g + 1) * 256], in_=pr[:, 0:256], func=AF.Copy, scale=1.0 / 2048.0),
                         [pr.r], [ro.r])
                dma("sp", fn_r, r3[:, k1, :], ro, ro[:, :])
        with Scope(b) as st:
            def S_(name, shape, dt=F32):
                return T(st.enter_context(nc.sbuf_tensor(name, list(shape), dt)))
            wout = S_("fwout", [P, 8, D], BF16)
            b.op("pool", lambda: G.dma_start(out=wout[:, :, :], in_=fn_w_out[:, :].rearrange("(k p) n -> p k n", p=P)), [fn_w_out.r], [wout.r], dma=True)
            xp = Pool([S_("fdx%d" % i, [P, 8, 512]) for i in range(2)])
            rtp = Pool([S_("frt%d" % i, [P, D]) for i in range(2)])
            rT = S_("frT", [P, 8, 512], BF16)
            for (c0, n, isctx) in tl:
                xin = xp.get()
                dma("sp", xin, xin[:, :, :n], resd, fm(resd, c0, n))
                for ci in range(n // P):
                    rt = rtp.get()
                    t0 = c0 - NCTX + ci * P
                    dma("sp", rt, rt[:, :], fn_r, fn_r[t0:t0 + P, :])
                    for hf in range(2):
                        pt = psf.get()
                        for kq in range(4):
                            k = hf * 4 + kq
                            b.op("pe", lambda k=k, kq=kq, rt=rt: PE.transpose(out=pt[:, kq * P:(kq + 1) * P], in_=rt[:, k * P:(k + 1) * P], identity=ident[:, :]),
                                 [rt.r, ident.r], [pt.r], inc=(kq == 3))
                        b.op("act", lambda hf=hf, ci=ci: A.copy(out=rT[:, hf * 4:(hf + 1) * 4, ci * P:(ci + 1) * P],
                                                                 in_=pt[:, :].rearrange("p (k t) -> p k t", k=4)), [pt.r], [rT.r])
                for o in range(8):
                    py = psf.get()
                    for k in range(8):
                        b.op("pe", lambda o=o, k=k: PE.matmul(py[:, :n], lhsT=wout[:, k, o * P:(o + 1) * P], rhs=rT[:, k, :n],
                                                               start=(k == 0), stop=(k == 7)), [wout.r, rT.r], [py.r], inc=(k == 7))
                    b.op("dve", lambda o=o: V.scalar_tensor_tensor(out=xin[:, o, :n], in0=py[:, :n], scalar=gt1[:, 0, o:o + 1],
                                                                    in1=xin[:, o, :n], op0=ALU.mult, op1=ALU.add), [py.r, gt1.r, xin.r], [xin.r])
                dma("sp", resd, fm(resd, c0, n), xin, xin[:, :, :n])
        return tl

    def layer1(L):
        layer_scalars(L)
        tl = tiles(True)
        MLI = 3088
        with Scope(b) as st:
            def S_(name, shape, dt=F32):
                return T(st.enter_context(nc.sbuf_tensor(name, list(shape), dt)))
            win = S_("mwin", [P, 8, MLI], BF16)
            b.op("pool", lambda: G.dma_start(out=win[:, :, :], in_=ml_w_in[:, :].rearrange("(k p) n -> p k n", p=P)), [ml_w_in.r], [win.r], dma=True)
            gb = S_("mgb", [4, 4]); ngb = S_("mngb", [4, 4])
            dma("sp", gb, gb[:, :], ml_gb, ml_gb[:, :])
            b.op("dve", lambda: V.tensor_scalar(out=ngb[:, :], in0=gb[:, :], scalar1=-1.0, scalar2=None, op0=ALU.mult), [gb.r], [ngb.r])
            xin = S_("mxin", [P, 8, 512]); sqb = S_("msqb", [P, 8, 512]); hT = S_("mhT", [P, 8, 512], BF16)
            zq = S_("mzq", [P, 8, 512]); so = S_("mso", [P, 8, 512], BF16)
            vp = Pool([S_("mvv%d" % i, [P, D], BF16) for i in range(2)])
            gr = S_("mgr", [4, 4, 512])
            for (c0, n, isctx) in tl:
                dma("sp", xin, xin[:, :, :n], resd, fm(resd, c0, n))
                rmsnorm_mod(xin, n, isctx, g1, sh1, xin, sqb, [hT])
                for o in range(8):
                    pz = psf.get()
                    for k in range(8):
                        b.op("pe", lambda o=o, k=k: PE.matmul(pz[:, :n], lhsT=win[:, k, o * P:(o + 1) * P], rhs=hT[:, k, :n],
                                                               start=(k == 0), stop=(k == 7)), [win.r, hT.r], [pz.r], inc=(k == 7))
                    b.op("act", lambda o=o: A.copy(out=zq[:, o, :n], in_=pz[:, :n]), [pz.r], [zq.r])
                dma("sp", ml_zqk, fm(ml_zqk, c0, n), zq, zq[:, :, :n])
                for o in range(8):
                    pz = psf.get()
                    for k in range(8):
                        b.op("pe", lambda o=o, k=k: PE.matmul(pz[:, :n], lhsT=win[:, k, 2048 + o * P:2048 + (o + 1) * P], rhs=hT[:, k, :n],
                                                               start=(k == 0), stop=(k == 7)), [win.r, hT.r], [pz.r], inc=(k == 7))
                    b.op("act", lambda o=o: A.activation(out=so[:, o, :n], in_=pz[:, :n], func=AF.Sigmoid), [pz.r], [so.r])
                dma("sp", ml_so, fm(ml_so, c0, n), so, so[:, :, :n])
                for ci in range(n // P):
                    vv = vp.get()
                    for hf in range(2):
                        pv = psf.get()
                        for k in range(8):
                            b.op("pe", lambda k=k, hf=hf, ci=ci: PE.matmul(pv[:, :], lhsT=hT[:, k, ci * P:(ci + 1) * P],
                                                                            rhs=win[:, k, 1024 + hf * 512:1024 + (hf + 1) * 512],
                                                                            start=(k == 0), stop=(k == 7)), [win.r, hT.r], [pv.r], inc=(k == 7))
                        b.op("act", lambda hf=hf, vv=vv: A.copy(out=vv[:, hf * 512:(hf + 1) * 512], in_=pv[:, :]), [pv.r], [vv.r])
                    dma("sp", ml_v, ml_v[c0 + ci * P:c0 + (ci + 1) * P, :], vv, vv[:, :])
                for q in range(4):
                    pgt = psf.get()
                    for k in range(8):
                        b.op("pe", lambda q=q, k=k: PE.matmul(pgt[0:4, :n], lhsT=win[:, k, 3072 + 4 * q:3072 + 4 * q + 4], rhs=hT[:, k, :n],
                                                               start=(k == 0), stop=(k == 7)), [win.r, hT.r], [pgt.r], inc=(k == 7))
                    if q % 2 == 0:
                        b.op("act", lambda q=q: A.activation(out=gr[:, q, :n], in_=pgt[0:4, :n], func=AF.Identity, bias=gb[:, q:q + 1], scale=1.0),
                             [pgt.r, gb.r], [gr.r])
                    else:
                        b.op("act", lambda q=q: A.activation(out=gr[:, q, :n], in_=pgt[0:4, :n], func=AF.Exp, bias=ngb[:, q:q + 1], scale=-1.0),
                             [pgt.r, ngb.r], [gr.r])
                        b.op("act", lambda q=q: A.activation(out=gr[:, q, :n], in_=gr[:, q, :n], func=AF.Ln, bias=onec[0:4, 0:1], scale=1.0),
                             [gr.r, onec.r], [gr.r])
                        b.op("dve", lambda q=q: V.tensor_scalar(out=gr[:, q, :n], in0=gr[:, q, :n], scalar1=-1.0, scalar2=None, op0=ALU.mult), [gr.r], [gr.r])
                dma("sp", ml_g, ml_g[:, :, c0:c0 + n], gr, gr[:, :, :n])
        with Scope(b) as st:
            def S_(name, shape, dt=F32):
                return T(st.enter_context(nc.sbuf_tensor(name, list(shape), dt)))
            cw = S_("mcw", [P, 8, 4]); cb = S_("mcb", [P, 8])
            dma("sp", cw, cw[:, :, :], ml_cw, ml_cw[:, :, :]); dma("sp", cb, cb[:, :], ml_cb, ml_cb[:, :])
            zp = Pool([S_("mzh%d" % i, [P, 516]) for i in range(2)])
            xr = S_("mxr", [P, 512]); qo = Pool([S_("mqo%d" % i, [P, 512], BF16) for i in range(2)])
            for ft in range(8):
                for (c0, n, isctx) in tl:
                    lo = 0 if isctx else NCTX
                    hi = NCTX if isctx else NT
                    zh = zp.get()
                    a0 = max(c0 - 2, lo); a1 = min(c0 + n + 1, hi)
                    if a0 > c0 - 2 or a1 < c0 + n + 1:
                        b.op("pool", lambda zh=zh: G.memset(zh[:, :], 0.0), [], [zh.r])
                    dma("sp", zh, zh[:, a0 - (c0 - 2):a1 - (c0 - 2)], ml_zqk, ml_zqk[ft * P:(ft + 1) * P, a0:a1])
                    b.op("dve", lambda zh=zh: V.tensor_scalar(out=xr[:, :n], in0=zh[:, 0:n], scalar1=cw[:, ft, 0:1], scalar2=cb[:, ft:ft + 1],
                                                              op0=ALU.mult, op1=ALU.add), [zh.r, cw.r, cb.r], [xr.r])
                    for j in range(1, 4):
                        b.op("dve", lambda zh=zh, j=j: V.scalar_tensor_tensor(out=xr[:, :n], in0=zh[:, j:j + n], scalar=cw[:, ft, j:j + 1], in1=xr[:, :n],
                                                                              op0=ALU.mult, op1=ALU.add), [zh.r, cw.r, xr.r], [xr.r])
                    qq = qo.get()
                    b.op("act", lambda: A.activation(out=xr[:, :n], in_=xr[:, :n], func=AF.Silu), [xr.r], [xr.r])
                    sc_ = (128.0 ** -0.5) if ft < 4 else 1.0
                    b.op("dve", lambda qq=qq: V.tensor_scalar(out=qq[:, :n], in0=xr[:, :n], scalar1=sc_, scalar2=None, op0=ALU.mult), [xr.r], [qq.r])
                    dst = ml_q if ft < 4 else ml_k
                    f4 = ft % 4
                    dma("sp", dst, dst[f4 * P:(f4 + 1) * P, c0:c0 + n], qq, qq[:, :n])
        with Scope(b) as st:
            def S_(name, shape, dt=F32):
                return T(st.enter_context(nc.sbuf_tensor(name, list(shape), dt)))
            msk = S_("mmsk", [P, 2, P]); sel4 = S_("msel4", [4, 512]); ng = S_("mng", [P, 8])
            dma("sp", msk, msk[:, :, :], ml_mask, ml_mask[:, :, :]); dma("sp", sel4, sel4[:, :], sel_d, sel_d[0:4, 0:512])
            dma("sp", ng, ng[:, :], ml_ng, ml_ng[:, :])
            Cst = S_("mC", [P, 4, 256]); Cb = S_("mCb", [P, 4, 256], BF16); nst = S_("mn", [P, 4, P]); nb = S_("mnb", [P, 4, P], BF16)
            qTp = Pool([S_("mqT%d" % i, [P, 4, P], BF16) for i in range(2)]); kTp = Pool([S_("mkT%d" % i, [P, 4, P], BF16) for i in range(2)])
            vcp = Pool([S_("mvc%d" % i, [P, D], BF16) for i in range(2)]); grp_ = Pool([S_("mgr%d" % i, [4, 2, P]) for i in range(2)])
            rb = S_("mrb", [4, P]); rc = S_("mrc", [4, P]); rk = S_("mrk", [4, P]); Rb = S_("mRb", [4, 257]); cols = S_("mcols", [P, 8])
            bcP = Pool([S_("mbc%d" % i, [P, 257]) for i in range(2)]); DmP = Pool([S_("mDm%d" % i, [P, P]) for i in range(2)])
            StP = Pool([S_("mSt%d" % i, [P, P], BF16) for i in range(2)]); t1P = Pool([S_("mt1%d" % i, [P, P]) for i in range(2)])
            numP = Pool([S_("mnum%d" % i, [P, 2, P]) for i in range(2)]); denP = Pool([S_("mden%d" % i, [P, P]) for i in range(2)])
            kwP = Pool([S_("mkw%d" % i, [P, P], BF16) for i in range(2)])
            hop = Pool([S_("mho%d" % i, [P, 8, P]) for i in range(2)]); hfp = Pool([S_("mhf%d" % i, [P, 8, P]) for i in range(2)])
            sop = Pool([S_("msoc%d" % i, [P, 8, P], BF16) for i in range(2)]); gop = Pool([S_("mgo%d" % i, [P, 8, P], BF16) for i in range(2)])
            sq2 = S_("msq2", [P, 2, P]); rstd = S_("mrstd", [P, P])
            nchk = NT // P
            for d in range(2):
                order = list(range(nchk)) if d == 0 else ([1, 0] + list(range(nchk - 1, 1, -1)))
                b.op("dve", lambda: V.memset(Cst[:, :, :], 0.0), [], [Cst.r]); b.op("dve", lambda: V.memset(Cb[:, :, :], 0.0), [], [Cb.r])
                b.op("dve", lambda: V.memset(nst[:, :, :], 0.0), [], [nst.r]); b.op("dve", lambda: V.memset(nb[:, :, :], 0.0), [], [nb.r])

                def dv(t):
                    ap = t[:, 0:P]
                    if d == 0:
                        return ap
                    return AP(ap.tensor, ap.offset + (P - 1), [[ap.ap[0][0], 4], [-1, P]])
                for j in order:
                    c0 = j * P
                    isctx = 1 if j < 2 else 0
                    qT = qTp.get(); kT = kTp.get(); vc = vcp.get(); gq = grp_.get()
                    dma("sp", qT, qT[:, :, :], ml_q, ml_q[:, c0:c0 + P].rearrange("(h p) t -> p h t", p=P))
                    dma("sp", kT, kT[:, :, :], ml_k, ml_k[:, c0:c0 + P].rearrange("(h p) t -> p h t", p=P))
                    dma("sp", vc, vc[:, :], ml_v, ml_v[c0:c0 + P, :])
                    dma("sp", gq, gq[:, :, :], ml_g, ml_g[:, 2 * d:2 * d + 2, c0:c0 + P])
                    gidx = P - 1 if d == 0 else 0
                    lfap = gq[:, 1, :]
                    if d == 1:
                        lfap = AP(lfap.tensor, lfap.offset + (P - 1), [[lfap.ap[0][0], 4], [-1, P]])
                    b.op("dve", lambda lfap=lfap: V.tensor_tensor_scan(out=dv(rb), data0=ones[0:4, 0:P], data1=lfap,
                                                                       initial=0.0, op0=ALU.mult, op1=ALU.add), [ones.r, gq.r], [rb.r])
                    b.op("dve", lambda gq=gq: V.tensor_tensor(out=rc[:, :], in0=gq[:, 0, :], in1=rb[:, :], op=ALU.subtract), [gq.r, rb.r], [rc.r])
                    b.op("act", lambda: A.activation(out=rk[:, :], in_=rc[:, :], func=AF.Exp, bias=rb[:, gidx:gidx + 1], scale=1.0), [rc.r, rb.r], [rk.r])
                    b.op("dve", lambda: V.tensor_copy(out=Rb[:, 0:P], in_=rb[:, :]), [rb.r], [Rb.r])
                    b.op("act", lambda: A.activation(out=Rb[:, P:2 * P], in_=rb[:, :], func=AF.Exp), [rb.r], [Rb.r])
                    b.op("act", lambda: A.activation(out=Rb[:, 256:257], in_=rb[:, gidx:gidx + 1], func=AF.Exp), [rb.r], [Rb.r])
                    pT = psf.get()
                    b.op("pe", lambda: PE.transpose(out=pT[:, 0:4], in_=rc[:, :], identity=ident[0:4, 0:4]), [rc.r, ident.r], [pT.r], inc=False)
                    b.op("pe", lambda: PE.transpose(out=pT[:, 4:8], in_=rk[:, :], identity=ident[0:4, 0:4]), [rk.r, ident.r], [pT.r])
                    b.op("dve", lambda: V.tensor_copy(out=cols[:, :], in_=pT[:, 0:8]), [pT.r], [cols.r])
                    ho = hop.get()
                    for h in range(4):
                        bc = bcP.get(); Dm = DmP.get(); St = StP.get(); t1 = t1P.get(); num = numP.get(); den = denP.get(); kw = kwP.get()
                        pbc = psf.get()
                        b.op("pe", lambda h=h: PE.matmul(pbc[:, 0:257], lhsT=sel4[:, h * P:(h + 1) * P], rhs=Rb[:, :], start=True, stop=True),
                             [sel4.r, Rb.r], [pbc.r])
                        b.op("act", lambda: A.copy(out=bc[:, :], in_=pbc[:, 0:257]), [pbc.r], [bc.r])
                        pst = psf.get()
                        b.op("pe", lambda h=h: PE.matmul(pst[:, 0:P], lhsT=kT[:, h, :], rhs=qT[:, h, :], start=True, stop=True), [kT.r, qT.r], [pst.r])
                        b.op("act", lambda h=h: A.activation(out=Dm[:, :], in_=bc[:, 0:P], func=AF.Exp, bias=cols[:, h:h + 1], scale=1.0),
                             [bc.r, cols.r], [Dm.r])
                        b.op("dve", lambda: V.tensor_tensor(out=Dm[:, :], in0=Dm[:, :], in1=msk[:, d, :], op=ALU.mult), [Dm.r, msk.r], [Dm.r])
                        b.op("dve", lambda: V.tensor_tensor(out=St[:, :], in0=Dm[:, :], in1=pst[:, 0:P], op=ALU.mult), [Dm.r, pst.r], [St.r])
                        for vt in range(2):
                            pn = psf.get()
                            b.op("pe", lambda h=h, vt=vt: PE.matmul(pn[:, 0:P], lhsT=vc[:, h * 256 + vt * P:h * 256 + (vt + 1) * P], rhs=St[:, :],
                                                                     start=True, stop=True), [vc.r, St.r], [pn.r], inc=False)
                            b.op("pe", lambda h=h, vt=vt: PE.matmul(pn[:, P:2 * P], lhsT=Cb[:, h, vt * P:(vt + 1) * P], rhs=qT[:, h, :],
                                                                     start=True, stop=True), [Cb.r, qT.r], [pn.r])
                            b.op("dve", lambda: V.tensor_tensor(out=t1[:, :], in0=pn[:, P:2 * P], in1=bc[:, P:2 * P], op=ALU.mult), [pn.r, bc.r], [t1.r])
                            b.op("dve", lambda vt=vt: V.tensor_tensor(out=num[:, vt, :], in0=t1[:, :], in1=pn[:, 0:P], op=ALU.add), [t1.r, pn.r], [num.r])
                        pd = psf.get()
                        b.op("pe", lambda: PE.matmul(pd[:, 0:P], lhsT=onesb[:, :], rhs=St[:, :], start=True, stop=True), [onesb.r, St.r], [pd.r], inc=False)
                        b.op("pe", lambda h=h: PE.matmul(pd[:, P:2 * P], lhsT=nb[:, h, :], rhs=qT[:, h, :], start=True, stop=True), [nb.r, qT.r], [pd.r])
                        b.op("dve", lambda: V.tensor_tensor(out=den[:, :], in0=pd[:, P:2 * P], in1=bc[:, P:2 * P], op=ALU.mult), [pd.r, bc.r], [den.r])
                        b.op("dve", lambda: V.tensor_tensor(out=den[:, :], in0=den[:, :], in1=pd[:, 0:P], op=ALU.add), [den.r, pd.r], [den.r])
                        b.op("dve", lambda: V.tensor_scalar(out=t1[:, :], in0=den[:, :], scalar1=-1.0, scalar2=None, op0=ALU.mult), [den.r], [t1.r])
                        b.op("dve", lambda: V.tensor_tensor(out=den[:, :], in0=den[:, :], in1=t1[:, :], op=ALU.max), [den.r, t1.r], [den.r])
                        b.op("dve", lambda: V.tensor_scalar(out=den[:, :], in0=den[:, :], scalar1=1.0, scalar2=None, op0=ALU.max), [den.r], [den.r])
                        b.op("dve", lambda: V.reciprocal(out=den[:, :], in_=den[:, :]), [den.r], [den.r])
                        for vt in range(2):
                            b.op("dve", lambda h=h, vt=vt: V.tensor_tensor(out=ho[:, 2 * h + vt, :], in0=num[:, vt, :], in1=den[:, :], op=ALU.mult),
                                 [num.r, den.r], [ho.r])
                        ptk = psb.get()
                        b.op("pe", lambda h=h: PE.transpose(out=ptk[:, 0:P], in_=kT[:, h, :], identity=identb[:, :]), [kT.r, identb.r], [ptk.r])
                        b.op("dve", lambda h=h: V.tensor_scalar(out=kw[:, :], in0=ptk[:, 0:P], scalar1=cols[:, 4 + h:5 + h], scalar2=None, op0=ALU.mult),
                             [ptk.r, cols.r], [kw.r])
                        pC = psf.get()
                        b.op("pe", lambda h=h: PE.matmul(pC[:, 0:256], lhsT=kw[:, :], rhs=vc[:, h * 256:(h + 1) * 256], start=True, stop=True),
                             [kw.r, vc.r], [pC.r], inc=False)
                        b.op("pe", lambda: PE.matmul(pC[:, 256:384], lhsT=kw[:, :], rhs=onesb[:, :], start=True, stop=True), [kw.r, onesb.r], [pC.r])
                        b.op("dve", lambda h=h: V.scalar_tensor_tensor(out=Cst[:, h, :], in0=Cst[:, h, :], scalar=bc[:, 256:257], in1=pC[:, 0:256],
                                                                        op0=ALU.mult, op1=ALU.add), [Cst.r, bc.r, pC.r], [Cst.r])
                        b.op("dve", lambda h=h: V.scalar_tensor_tensor(out=nst[:, h, :], in0=nst[:, h, :], scalar=bc[:, 256:257], in1=pC[:, 256:384],
                                                                        op0=ALU.mult, op1=ALU.add), [nst.r, bc.r, pC.r], [nst.r])
                        b.op("act", lambda h=h: A.copy(out=Cb[:, h, :], in_=Cst[:, h, :]), [Cst.r], [Cb.r])
                        b.op("act", lambda h=h: A.copy(out=nb[:, h, :], in_=nst[:, h, :]), [nst.r], [nb.r])
                    if d == 0:
                        dma("sp", ml_hf, ml_hf[:, c0:c0 + P].rearrange("(k p) t -> p k t", p=P), ho, ho[:, :, :])
                    else:
                        hf = hfp.get(); soc = sop.get(); go = gop.get()
                        dma("sp", hf, hf[:, :, :], ml_hf, ml_hf[:, c0:c0 + P].rearrange("(k p) t -> p k t", p=P))
                        dma("sp", soc, soc[:, :, :], ml_so, ml_so[:, c0:c0 + P].rearrange("(k p) t -> p k t", p=P))
                        b.op("dve", lambda: V.tensor_tensor(out=ho[:, :, :], in0=ho[:, :, :], in1=hf[:, :, :], op=ALU.add), [ho.r, hf.r], [ho.r])
                        for h in range(4):
                            b.op("act", lambda h=h: A.activation(out=sq2[:, :, :], in_=ho[:, 2 * h:2 * h + 2, :], func=AF.Square), [ho.r], [sq2.r])
                            pss = psf.get()
                            for vt in range(2):
                                b.op("pe", lambda vt=vt: PE.matmul(pss[:, 0:P], lhsT=ones[:, :], rhs=sq2[:, vt, :], start=(vt == 0), stop=(vt == 1)),
                                     [ones.r, sq2.r], [pss.r], inc=(vt == 1))
                            b.op("act", lambda: A.activation(out=rstd[:, :], in_=pss[:, 0:P], func=AF.Sqrt, bias=epsc[:, 0:1], scale=1.0 / 256.0),
                                 [pss.r, epsc.r], [rstd.r])
                            b.op("dve", lambda: V.reciprocal(out=rstd[:, :], in_=rstd[:, :]), [rstd.r], [rstd.r])
                            for vt in range(2):
                                kk = 2 * h + vt
                                b.op("dve", lambda kk=kk: V.scalar_tensor_tensor(out=ho[:, kk, :], in0=ho[:, kk, :], scalar=ng[:, kk:kk + 1], in1=rstd[:, :],
                                                                                 op0=ALU.mult, op1=ALU.mult), [ho.r, ng.r, rstd.r], [ho.r])
                        b.op("dve", lambda: V.tensor_tensor(out=go[:, :, :], in0=ho[:, :, :], in1=soc[:, :, :], op=ALU.mult), [ho.r, soc.r], [go.r])
                        dma("sp", ml_gated, ml_gated[:, c0:c0 + P].rearrange("(k p) t -> p k t", p=P), go, go[:, :, :])
        with Scope(b) as st:
            def S_(name, shape, dt=F32):
                return T(st.enter_context(nc.sbuf_tensor(name, list(shape), dt)))
            wout = S_("mwout", [P, 8, D], BF16)
            b.op("pool", lambda: G.dma_start(out=wout[:, :, :], in_=ml_w_out[:, :].rearrange("(k p) n -> p k n", p=P)), [ml_w_out.r], [wout.r], dma=True)
            xp = Pool([S_("mdx%d" % i, [P, 8, 512]) for i in range(2)])
            gp = Pool([S_("mdg%d" % i, [P, 8, 512], BF16) for i in range(2)])
            for (c0, n, isctx) in tl:
                xin = xp.get(); gg = gp.get()
                dma("sp", xin, xin[:, :, :n], resd, fm(resd, c0, n))
                dma("sp", gg, gg[:, :, :n], ml_gated, fm(ml_gated, c0, n))
                for o in range(8):
                    py = psf.get()
                    for k in range(8):
                        b.op("pe", lambda o=o, k=k: PE.matmul(py[:, :n], lhsT=wout[:, k, o * P:(o + 1) * P], rhs=gg[:, k, :n],
                                                               start=(k == 0), stop=(k == 7)), [wout.r, gg.r], [py.r], inc=(k == 7))
                    b.op("dve", lambda o=o: V.scalar_tensor_tensor(out=xin[:, o, :n], in0=py[:, :n], scalar=gt1[:, isctx, o:o + 1],
                                                                    in1=xin[:, o, :n], op0=ALU.mult, op1=ALU.add), [py.r, gt1.r, xin.r], [xin.r])
                dma("sp", resd, fm(resd, c0, n), xin, xin[:, :, :n])
        return tl

    def layer0(L):
        layer_scalars(L)
        tl = tiles(True)
        with Scope(b) as st:
            def S_(name, shape, dt=F32):
                return T(st.enter_context(nc.sbuf_tensor(name, list(shape), dt)))
            moe_begin(m, L, sum(t[1] for t in tl))
            win = S_("cmwin", [P, 8, 2 * D], BF16)
            wout = S_("cmwout", [P, 8, D], BF16)
            wsT = S_("cmwsT", [P, 4, P], BF16)
            vg = S_("cmvg", [P, D]); bs = S_("cmbs", [P, 4, 512])
            b.op("pool", lambda: G.dma_start(out=win[:, :, :], in_=cm_w_in[:, :].rearrange("(k p) n -> p k n", p=P)), [cm_w_in.r], [win.r], dma=True)
            b.op("pool", lambda: G.dma_start(out=wout[:, :, :], in_=cm_w_out[:, :].rearrange("(k p) n -> p k n", p=P)), [cm_w_out.r], [wout.r], dma=True)
            b.op("pool", lambda: G.dma_start(out=wsT[:, :, :], in_=cm_wsT[:, :, :]), [cm_wsT.r], [wsT.r], dma=True)
            dma("sp", vg, vg[:, :], cm_vg_rep, cm_vg_rep[:, :])
            dma("sp", bs, bs[:, :, :], cm_bs_rep, cm_bs_rep[:, :, :])
            xp = Pool([S_("xin%d" % i, [P, 8, 512]) for i in range(2)])
            sqb = S_("sqb", [P, 8, 512]); tb = S_("tb", [P, 8, 512])
            hT = S_("hT", [P, 8, 512], BF16)
            u = S_("u", [P, 8, 512], BF16)
            vt = S_("vt", [P, 1024]); vn = S_("vn", [P, 4, 1024], BF16)
            gated = S_("gated", [P, 8, 512], BF16)
            h2b = S_("h2b", [P, 8, 512], BF16)
            rowsp = Pool([S_("rows%d" % i, [P, D], BF16) for i in range(2)])
            t1 = S_("gsc1", [P, 512]); t2 = S_("gsc2", [P, 512])
            vss = S_("vss", [P, 2]); junk = S_("junk", [P, 1024])
            for (c0, n, isctx) in tl:
                xin = xp.get()
                dma("sp", xin, xin[:, :, :n], resd, fm(resd, c0, n))
                rmsnorm_mod(xin, n, isctx, g1, sh1, tb, sqb, [hT])
                for o in range(8):
                    pu = psf.get()
                    for k in range(8):
                        b.op("pe", lambda o=o, k=k: PE.matmul(pu[:, :n], lhsT=win[:, k, o * P:(o + 1) * P], rhs=hT[:, k, :n],
                                                               start=(k == 0), stop=(k == 7)), [win.r, hT.r], [pu.r], inc=(k == 7))
                    gelu_tanh(u[:, o, :n], u, pu[:, :n], pu, ((t1[:, :n], t1), (t2[:, :n], t2)), None)
                for ci in range(n // P):
                    for hf in range(2):
                        pv = psf.get()
                        for k in range(8):
                            b.op("pe", lambda k=k, hf=hf, ci=ci: PE.matmul(pv[:, :], lhsT=hT[:, k, ci * P:(ci + 1) * P],
                                                                            rhs=win[:, k, D + hf * 512:D + (hf + 1) * 512],
                                                                            start=(k == 0), stop=(k == 7)), [win.r, hT.r], [pv.r], inc=(k == 7))
                        gelu_tanh(vt[:, hf * 512:(hf + 1) * 512], vt, pv[:, :], pv, ((t1[:, :], t1), (t2[:, :], t2)), None)
                    b.op("act", lambda: A.activation(out=junk[:, :], in_=vt[:, :], func=AF.Square), [vt.r], [junk.r])
                    b.op("dve", lambda: V.reduce_sum(out=vss[:, 0:1], in_=junk[:, :], axis=AX.X), [junk.r], [vss.r])
                    b.op("act", lambda: A.activation(out=vss[:, 1:2], in_=vss[:, 0:1], func=AF.Sqrt, bias=epsc[:, 0:1], scale=1.0 / D),
                         [vss.r, epsc.r], [vss.r])
                    b.op("dve", lambda: V.reciprocal(out=vss[:, 1:2], in_=vss[:, 1:2]), [vss.r], [vss.r])
                    b.op("dve", lambda ci=ci: V.scalar_tensor_tensor(out=vn[:, ci, :], in0=vt[:, :], scalar=vss[:, 1:2], in1=vg[:, :],
                                                                      op0=ALU.mult, op1=ALU.mult), [vt.r, vss.r, vg.r], [vn.r], ss=True)
                for c in range(8):
                    psm = psf.get()
                    for ci in range(n // P):
                        b.op("pe", lambda c=c, ci=ci: PE.matmul(psm[:, ci * P:(ci + 1) * P], lhsT=vn[:, ci, c * P:(c + 1) * P], rhs=wsT[:, c // 2, :],
                                                                 start=True, stop=True), [vn.r, wsT.r], [psm.r], inc=(ci == n // P - 1))
                    b.op("dve", lambda c=c: V.tensor_tensor(out=t1[:, :n], in0=psm[:, :n], in1=bs[:, c // 2, :n], op=ALU.add), [psm.r, bs.r], [t1.r])
                    b.op("dve", lambda c=c: V.tensor_tensor(out=gated[:, c, :n], in0=t1[:, :n], in1=u[:, c, :n], op=ALU.mult), [t1.r, u.r], [gated.r])
                for o in range(8):
                    py = psf.get()
                    for k in range(8):
                        b.op("pe", lambda o=o, k=k: PE.matmul(py[:, :n], lhsT=wout[:, k, o * P:(o + 1) * P], rhs=gated[:, k, :n],
                                                               start=(k == 0), stop=(k == 7)), [wout.r, gated.r], [py.r], inc=(k == 7))
                    b.op("dve", lambda o=o: V.scalar_tensor_tensor(out=xin[:, o, :n], in0=py[:, :n], scalar=gt1[:, isctx, o:o + 1],
                                                                    in1=xin[:, o, :n], op0=ALU.mult, op1=ALU.add), [py.r, gt1.r, xin.r], [xin.r])
                dma("sp", resd, fm(resd, c0, n), xin, xin[:, :, :n])
                continue
                rmsnorm_mod(xin, n, isctx, g2, sh2, tb, sqb, [sqb, h2b])
                for ci in range(n // P):
                    ch = c0 // P + ci
                    moe_route_chunk(m, sqb, ci * P, ch)
                    moe_rows_chunk(m, h2b, ci * P, c0 + ci * P, rowsp.get())
            if dump_res:
                dbg.extend([("modall", modall, modall[:, :, :, :], [P, DEPTH, 48, 2], F32), ("g1", g1, g1[:, :, :], [P, 2, 8], F32),
                            ("hT", hT, hT[:, :, :], [P, 8, 512], BF16), ("u", u, u[:, :, :], [P, 8, 512], BF16),
                            ("vn", vn, vn[:, :, :], [P, 4, 1024], BF16), ("gated", gated, gated[:, :, :], [P, 8, 512], BF16),
                            ("tb", tb, tb[:, :, :], [P, 8, 512], F32), ("sqb", sqb, sqb[:, :, :], [P, 8, 512], F32),
                            ("sc", sc, sc[:, :, :], [P, 8, 2], F32),
                            ("vt", vt, vt[:, :], [P, 1024], F32), ("junk", junk, junk[:, :], [P, 1024], F32), ("vss", vss, vss[:, :], [P, 2], F32),
                            ("vg", vg, vg[:, :], [P, 1024], F32)])
                emit_dbg()
        return tl

    for L in layers:
        if L == 0:
            tl = layer0(L)
            if stage >= 5:
                moe_dense(L, tl)
        elif L == 1:
            tl = layer1(L)
            if stage >= 5:
                moe_dense(L, tl)
        elif L == 3:
            tl = layer3(L)
            if stage >= 5:
                moe_dense(L, tl)
        elif L == 2:
            tl = layer2(L)
            if stage >= 5:
                moe_dense(L, tl)
        else:
            raise NotImplementedError("mixer for layer %d not implemented yet" % L)

    with Scope(b) as st:
        fa = Pool([T(st.enter_context(nc.sbuf_tensor("fa%d" % i, [P, 8, 512], F32))) for i in range(2)])
        if out_res and not dump_res:
            for c0 in range(0, NT, 512):
                n = min(512, NT - c0)
                a = fa.get()
                dma("sp", a, a[:, :, :n], resd, fm(resd, c0, n))
                dma("sp", res_out, fm(res_out, c0, n), a, a[:, :, :n])
        if dump_res:
            dma("sp", info_out, info_out[:, :, :], m.info, m.info[:, :, :])
            dma("sp", dest_out, dest_out[:, :, :], m.dest, m.dest[:, :, :])
            dma("sp", carry_out, carry_out[:, :], m.carry, m.carry[:, :])
            for c0 in range(0, NT, 512):
                n = min(512, NT - c0)
                a = fa.get()
                dma("sp", a, a[:, :, :n], resd, fm(resd, c0, n))
                dma("sp", res_out, fm(res_out, c0, n), a, a[:, :, :n])
        if final:
            fsq = T(st.enter_context(nc.sbuf_tensor("fsq", [P, 8, 512], F32)))
            ftb = T(st.enter_context(nc.sbuf_tensor("ftb", [P, 8, 512], F32)))
            fsc = T(st.enter_context(nc.sbuf_tensor("fsc", [P, 2, 8], F32)))
            fzero = T(st.enter_context(nc.sbuf_tensor("fzero", [P, 2, 8], F32)))
            b.op("dve", lambda: V.memset(fzero[:, :, :], 0.0), [], [fzero.r])
            b.op("dve", lambda: V.tensor_copy(out=fsc[:, 0, :], in_=nfin[:, :]), [nfin.r], [fsc.r])
            for c0 in range(0, SEQ, 512):
                a = fa.get()
                dma("sp", a, a[:, :, :], resd, fm(resd, NCTX + c0, 512))
                rmsnorm_mod(a, 512, 0, fsc, fzero, ftb, fsq, [fsq])
                dma("sp", outT, fm(outT, c0, 512), fsq, fsq[:, :, :])
    b.finish("sp")
    return nc, b


def _pos_embedding_T():
    rows = SEQ // 64
    r = np.repeat(np.arange(rows, dtype=np.float32), 64)
    col = np.tile(np.arange(64, dtype=np.float32), rows)
    q = D // 4
    freq = np.exp(np.float32(-math.log(10000.0)) * np.arange(q, dtype=np.float32) / np.float32(q)).astype(np.float32)
    ar = r[:, None] * freq
    ac = col[:, None] * freq
    pe = np.concatenate([np.sin(ar), np.cos(ar), np.sin(ac), np.cos(ac)], axis=-1).astype(np.float32)
    return np.ascontiguousarray(pe.T)


def _fp(v, n):
    return np.ascontiguousarray(np.asarray(v, np.float32).reshape(n, P).T)


def host_prep(inp):
    f = lambda a: np.ascontiguousarray(np.asarray(a, np.float32))
    m = {}
    m["xT"] = f(inp["x"][0].T)
    m["posT"] = _pos_embedding_T()
    m["ctxT"] = f(inp["ctx"][0].T)
    m["cT"] = f(np.stack([_fp(inp["c"][0], 8), _fp(inp["c_ctx"], 8)], axis=-1))
    for l in range(DEPTH):
        m["ada_w%d" % l] = f(inp["ada_w"][l])
    m["ada_bT"] = f(np.stack([_fp(inp["ada_b"][l], 48) for l in range(DEPTH)]))
    m["nmixT"] = f(np.stack([_fp(inp["norm_mix_g"][l], 8) for l in range(DEPTH)]))
    m["nffnT"] = f(np.stack([_fp(inp["norm_ffn_g"][l], 8) for l in range(DEPTH)]))
    m["nfinT"] = _fp(inp["final_norm_g"], 8)
    m["rw"] = f(np.concatenate([inp["router_group_w"], inp["router_expert_w"]], axis=-1))
    rb = np.concatenate([inp["router_group_b"], inp["router_expert_b"]], axis=-1)
    m["rbrep"] = f(np.broadcast_to(rb[:, None, :], (DEPTH, P, 36)))
    for l in range(DEPTH):
        m["wg%d" % l] = f(inp["expert_w_gate"][l]).reshape(NEXP * D, HID)
        m["wu%d" % l] = f(inp["expert_w_up"][l]).reshape(NEXP * D, HID)
        m["wd%d" % l] = f(inp["expert_w_down"][l]).reshape(NEXP * HID, D)
    m["cm_w_in"] = f(inp["cm_w_in"][0])
    m["cm_vg_rep"] = f(np.broadcast_to(inp["cm_v_norm_g"][0][None, :], (P, D)))
    m["cm_wsT"] = f(np.transpose(inp["cm_w_s"][0], (2, 0, 1)))
    m["cm_bs_rep"] = f(np.broadcast_to(np.tile(inp["cm_b_s"][0], (1, 4))[None], (P, 4, 512)))
    m["cm_w_out"] = f(inp["cm_w_out"][0])
    m["lru_w_in"] = f(inp["lru_w_in"][0]); m["lru_w_out"] = f(inp["lru_w_out"][0])
    m["lru_wa"] = f(np.transpose(inp["lru_w_a"][0], (2, 0, 1, 3)))
    m["lru_wx"] = f(np.transpose(inp["lru_w_x"][0], (2, 0, 1, 3)))
    m["lru_cw"] = f(inp["lru_conv_w"][0].T.reshape(10, P, 4).transpose(1, 0, 2))
    vecs = [inp["lru_conv_b"][0], inp["lru_b_a"][0][0], inp["lru_b_a"][0][1], inp["lru_b_x"][0][0], inp["lru_b_x"][0][1],
            inp["lru_lambda"][0][0], inp["lru_lambda"][0][1]]
    m["lru_vec"] = f(np.stack([_fp(v, 10) for v in vecs], axis=1))
    m["fn_w_in"] = f(inp["fn_w_in"][0]); m["fn_w_out"] = f(inp["fn_w_out"][0])
    cc = np.arange(256, dtype=np.float64)
    ph = 2 * np.pi * np.outer(cc, cc) / 256.0
    csm = np.concatenate([np.cos(ph), np.sin(ph)], axis=1).astype(np.float32)
    m["fn_cs"] = f(csm.reshape(2, P, 512).transpose(1, 0, 2))
    kk = np.arange(P, dtype=np.float64)
    p128 = 2 * np.pi * np.outer(kk, kk) / 128.0
    m["fn_dft"] = f(np.stack([np.cos(p128), np.sin(p128), -np.sin(p128)], axis=1))
    ptw = 2 * np.pi * np.outer(kk, kk) / float(SEQ)
    m["fn_tw"] = f(np.stack([np.cos(ptw), np.sin(ptw)], axis=1))
    m["ml_w_in"] = f(inp["ml_w_in"][0]); m["ml_w_out"] = f(inp["ml_w_out"][0])
    m["ml_gb"] = f(inp["ml_gate_b"][0].reshape(4, 4).T)
    m["ml_cw"] = f(inp["ml_conv_w"][0].T.reshape(8, P, 4).transpose(1, 0, 2))
    m["ml_cb"] = _fp(inp["ml_conv_b"][0], 8)
    m["ml_mask"] = f(np.stack([np.triu(np.ones((P, P), np.float32)), np.tril(np.ones((P, P), np.float32))], axis=1))
    m["ml_ng"] = _fp(inp["ml_norm_g"][0], 8)
    m["ident"] = np.eye(P, dtype=np.float32)
    m["ustrict"] = np.triu(np.ones((P, P), np.float32), 1)
    m["iota32"] = f(np.broadcast_to(np.arange(32, dtype=np.float32)[None], (P, 32)))
    nbmax = (2 * NT + NEXP * 127 + 127) // 128
    m["blkpos"] = f(np.broadcast_to((np.arange(nbmax, dtype=np.float32) * 128)[None], (P, nbmax)))
    m["piota"] = np.arange(P, dtype=np.float32)[:, None].copy()
    selm = np.zeros((32, NEXP, P), np.float32)
    selm[np.arange(32), np.arange(32), :] = 1.0
    m["sel"] = selm.reshape(32, NEXP * P)
    return m


def kernel(**inputs):
    m = host_prep(inputs)
    nc, b = build(layers=(0, 1, 2, 3), in_res=False, final=True)
    im = {k: v for k, v in m.items() if k in b.in_names}
    r = run_bass_kernel_spmd(nc, [im], core_ids=[0]).results[0]
    return np.ascontiguousarray(r["outT"].T)[None].astype(np.float32)
```
